# Optimizing a Trainium2 kernel written in Bass

```python
import jax, jax.numpy as jnp
from jax import lax
import numpy as np

D_MODEL = 1024
BATCH = 2
SEQ = 16384
DEPTH = 1

A_HEADS = 8
A_KV_HEADS = 2
A_HEAD_DIM = 64
WINDOW = 128
BLOCK = 128
B_HEADS = 8
B_NOPE_DIM = 64
B_ROPE_DIM = 32
B_V_DIM = 64
B_Q_RANK = 768
B_KV_RANK = 256
ROPE_THETA = 10000.0
N_GROUPS = 4
EXPERTS_PER_GROUP = 8
N_EXPERTS = N_GROUPS * EXPERTS_PER_GROUP
EXPERT_FF = 256
TOP_K_IN_GROUP = 2
LN_EPS = 1e-5
RMS_EPS = 1e-6
DEEPNORM_ALPHA = (2 * DEPTH) ** 0.25
DEEPNORM_BETA = (8 * DEPTH) ** -0.25

A_Q_COLS = A_HEADS * A_HEAD_DIM
A_KV_COLS = A_KV_HEADS * A_HEAD_DIM
A_WIDTH = A_HEADS * A_HEAD_DIM
B_WIDTH = B_HEADS * B_V_DIM
MIX_WIDTH = A_WIDTH + B_WIDTH
IN_SPLITS = (A_Q_COLS,
             A_Q_COLS + A_KV_COLS,
             A_Q_COLS + 2 * A_KV_COLS,
             A_Q_COLS + 2 * A_KV_COLS + B_Q_RANK,
             A_Q_COLS + 2 * A_KV_COLS + B_Q_RANK + B_KV_RANK)
IN_COLS = A_Q_COLS + 2 * A_KV_COLS + B_Q_RANK + B_KV_RANK + B_ROPE_DIM
NEG_BIG = -1e30

kernel_name = "hybrid_swa_mla_hiermoe_deepnorm"


def layer_norm(x, g, b):
    xf = x.astype(jnp.float32)
    mu = jnp.mean(xf, -1, keepdims=True)
    xc = xf - mu
    var = jnp.mean(xc * xc, -1, keepdims=True)
    return (xc * lax.rsqrt(var + LN_EPS) * g.astype(jnp.float32) + b.astype(jnp.float32)).astype(x.dtype)


def rms_norm(x, g):
    xf = x.astype(jnp.float32)
    return (xf * lax.rsqrt(jnp.mean(xf * xf, -1, keepdims=True) + RMS_EPS) * g.astype(jnp.float32)).astype(x.dtype)


def rope_cos_sin(positions, dim):
    inv = 1.0 / (ROPE_THETA ** (jnp.arange(0, dim, 2, dtype=jnp.float32) / dim))
    ang = positions.astype(jnp.float32)[..., None] * inv
    return jnp.cos(ang), jnp.sin(ang)


def apply_rope(x, cos, sin):
    x1, x2 = jnp.split(x.astype(jnp.float32), 2, axis=-1)
    c = cos[:, :, None, :]
    s = sin[:, :, None, :]
    return jnp.concatenate([x1 * c - x2 * s, x1 * s + x2 * c], axis=-1).astype(x.dtype)


def windowed_gqa_sink(q, k, v, sink):
    b, s, h, d = q.shape
    kvh = k.shape[2]
    g = h // kvh
    nb = s // BLOCK
    qb = q.reshape(b, nb, BLOCK, kvh, g, d)

    def bands(t):
        tp = jnp.pad(t, ((0, 0), (BLOCK, BLOCK), (0, 0), (0, 0))).reshape(b, nb + 2, BLOCK, kvh, d)
        return jnp.concatenate([tp[:, :-2], tp[:, 1:-1], tp[:, 2:]], axis=2)

    kb = bands(k)
    vb = bands(v)
    scores = jnp.einsum('bnqkgd,bnskd->bnkgqs', qb, kb,
                        preferred_element_type=jnp.float32) * (d ** -0.5)
    qpos = jnp.arange(nb)[:, None] * BLOCK + jnp.arange(BLOCK)[None, :]
    kpos = jnp.arange(nb)[:, None] * BLOCK - BLOCK + jnp.arange(3 * BLOCK)[None, :]
    kp = kpos[:, None, :]
    valid = (kp >= 0) & (kp < s) & (jnp.abs(kp - qpos[:, :, None]) <= WINDOW)
    scores = jnp.where(valid[None, :, None, None], scores, NEG_BIG)
    sk = sink.astype(jnp.float32).reshape(1, 1, kvh, g, 1, 1)
    m = jnp.maximum(jnp.max(scores, -1, keepdims=True), sk)
    p = jnp.exp(scores - m)
    p = p / (jnp.sum(p, -1, keepdims=True) + jnp.exp(sk - m))
    out = jnp.einsum('bnkgqs,bnskd->bnqkgd', p.astype(v.dtype), vb)
    return out.reshape(b, s, h * d)


def mla_attention(q_nope, q_rope, k_nope, k_rope, v):
    b, s, h, dn = q_nope.shape
    dv = v.shape[-1]
    nb = s // BLOCK
    scale = (B_NOPE_DIM + B_ROPE_DIM) ** -0.5
    qn_blocks = q_nope.reshape(b, nb, BLOCK, h, dn).transpose(1, 0, 2, 3, 4)
    qr_blocks = q_rope.reshape(b, nb, BLOCK, h, q_rope.shape[-1]).transpose(1, 0, 2, 3, 4)

    def one_block(args):
        qn, qr = args
        sc = (jnp.einsum('bqhd,bshd->bhqs', qn, k_nope, preferred_element_type=jnp.float32)
              + jnp.einsum('bqhr,bsr->bhqs', qr, k_rope, preferred_element_type=jnp.float32)) * scale
        p = jax.nn.softmax(sc, axis=-1)
        return jnp.einsum('bhqs,bshd->bqhd', p.astype(v.dtype), v)

    out = lax.map(one_block, (qn_blocks, qr_blocks))
    return out.transpose(1, 0, 2, 3, 4).reshape(b, s, h * dv)


def hierarchical_moe(x, w_group, b_group, w_expert, b_expert, w_gate, w_up, w_down):
    b, s, d = x.shape
    t = x.reshape(b * s, d)
    gl = (t @ w_group).astype(jnp.float32) + b_group.astype(jnp.float32)
    pg = jax.nn.softmax(gl, axis=-1)
    _, g_idx = lax.top_k(gl, 1)
    p_group = jnp.take_along_axis(pg, g_idx, axis=-1)
    el = ((t @ w_expert).astype(jnp.float32) + b_expert.astype(jnp.float32)).reshape(-1, N_GROUPS, EXPERTS_PER_GROUP)
    el_sel = jnp.take_along_axis(el, g_idx[:, :, None], axis=1)[:, 0]
    top_v, top_i = lax.top_k(el_sel, TOP_K_IN_GROUP)
    w = p_group * jax.nn.softmax(top_v, axis=-1)
    e_idx = g_idx * EXPERTS_PER_GROUP + top_i
    combine = jnp.sum(jax.nn.one_hot(e_idx, N_EXPERTS, dtype=jnp.float32) * w[..., None], axis=1)
    y = jnp.zeros(t.shape, jnp.float32)
    for e in range(N_EXPERTS):
        hid = jax.nn.silu(t @ w_gate[e]) * (t @ w_up[e])
        y = y + combine[:, e:e + 1] * (hid @ w_down[e]).astype(jnp.float32)
    return y.astype(x.dtype).reshape(b, s, d)


def setup_inputs(seed: int = 0) -> dict:
    key = jax.random.key(seed)
    ks = jax.random.split(key, 26)
    L, D = DEPTH, D_MODEL
    f32 = jnp.float32

    def nrm(k, shape, scale):
        return jax.random.normal(k, shape, f32) * scale

    offs = jax.random.randint(ks[1], (BATCH, 1), 0, 4096, dtype=jnp.int32)
    positions = jnp.arange(SEQ, dtype=jnp.int32)[None, :] + offs
    return {
        "x": nrm(ks[0], (BATCH, SEQ, D), 1.0),
        "positions": positions,
        "ln_emb_g": 1.0 + nrm(ks[2], (D,), 0.02),
        "ln_emb_b": nrm(ks[3], (D,), 0.02),
        "w_in": nrm(ks[4], (L, D, IN_COLS), D ** -0.5),
        "a_sink": nrm(ks[5], (L, A_HEADS), 1.0),
        "q_a_norm_g": 1.0 + nrm(ks[6], (L, B_Q_RANK), 0.02),
        "w_q_b": nrm(ks[7], (L, B_Q_RANK, B_HEADS * (B_NOPE_DIM + B_ROPE_DIM)), B_Q_RANK ** -0.5),
        "kv_a_norm_g": 1.0 + nrm(ks[8], (L, B_KV_RANK), 0.02),
        "w_kv_b": nrm(ks[9], (L, B_KV_RANK, B_HEADS * (B_NOPE_DIM + B_V_DIM)), B_KV_RANK ** -0.5),
        "out_norm_a_g": 1.0 + nrm(ks[10], (L, A_WIDTH), 0.02),
        "out_norm_b_g": 1.0 + nrm(ks[11], (L, B_WIDTH), 0.02),
        "w_out": nrm(ks[12], (L, MIX_WIDTH, D), MIX_WIDTH ** -0.5 * DEEPNORM_BETA),
        "ln_attn_g": 1.0 + nrm(ks[13], (L, D), 0.02),
        "ln_attn_b": nrm(ks[14], (L, D), 0.02),
        "w_group": nrm(ks[15], (L, D, N_GROUPS), D ** -0.5),
        "b_group": nrm(ks[16], (L, N_GROUPS), 0.01),
        "w_expert": nrm(ks[17], (L, D, N_EXPERTS), D ** -0.5),
        "b_expert": nrm(ks[18], (L, N_EXPERTS), 0.01),
        "w_gate": nrm(ks[19], (L, N_EXPERTS, D, EXPERT_FF), D ** -0.5),
        "w_up": nrm(ks[20], (L, N_EXPERTS, D, EXPERT_FF), D ** -0.5),
        "w_down": nrm(ks[21], (L, N_EXPERTS, EXPERT_FF, D), EXPERT_FF ** -0.5 * DEEPNORM_BETA),
        "ln_ffn_g": 1.0 + nrm(ks[22], (L, D), 0.02),
        "ln_ffn_b": nrm(ks[23], (L, D), 0.02),
    }


def reference(x, positions, ln_emb_g, ln_emb_b, w_in, a_sink, q_a_norm_g, w_q_b, kv_a_norm_g, w_kv_b,
              out_norm_a_g, out_norm_b_g, w_out, ln_attn_g, ln_attn_b, w_group, b_group, w_expert,
              b_expert, w_gate, w_up, w_down, ln_ffn_g, ln_ffn_b):
    b, s, _ = x.shape
    h = layer_norm(x, ln_emb_g, ln_emb_b)
    cos_a, sin_a = rope_cos_sin(positions, A_HEAD_DIM)
    cos_b, sin_b = rope_cos_sin(positions, B_ROPE_DIM)
    for l in range(DEPTH):
        proj = h @ w_in[l]
        qa, ka, va, cq, ckv, kr = jnp.split(proj, IN_SPLITS, axis=-1)
        qa = apply_rope(qa.reshape(b, s, A_HEADS, A_HEAD_DIM), cos_a, sin_a)
        ka = apply_rope(ka.reshape(b, s, A_KV_HEADS, A_HEAD_DIM), cos_a, sin_a)
        va = va.reshape(b, s, A_KV_HEADS, A_HEAD_DIM)
        out_a = windowed_gqa_sink(qa, ka, va, a_sink[l])
        qb = (rms_norm(cq, q_a_norm_g[l]) @ w_q_b[l]).reshape(b, s, B_HEADS, B_NOPE_DIM + B_ROPE_DIM)
        q_nope = qb[..., :B_NOPE_DIM]
        q_rope = apply_rope(qb[..., B_NOPE_DIM:], cos_b, sin_b)
        kv = (rms_norm(ckv, kv_a_norm_g[l]) @ w_kv_b[l]).reshape(b, s, B_HEADS, B_NOPE_DIM + B_V_DIM)
        k_nope = kv[..., :B_NOPE_DIM]
        v_b = kv[..., B_NOPE_DIM:]
        k_rope = apply_rope(kr[:, :, None, :], cos_b, sin_b)[:, :, 0]
        out_b = mla_attention(q_nope, q_rope, k_nope, k_rope, v_b)
        mixed = jnp.concatenate([rms_norm(out_a, out_norm_a_g[l]), rms_norm(out_b, out_norm_b_g[l])], axis=-1) @ w_out[l]
        h = layer_norm(DEEPNORM_ALPHA * h + mixed, ln_attn_g[l], ln_attn_b[l])
        ffn = hierarchical_moe(h, w_group[l], b_group[l], w_expert[l], b_expert[l], w_gate[l], w_up[l], w_down[l])
        h = layer_norm(DEEPNORM_ALPHA * h + ffn, ln_ffn_g[l], ln_ffn_b[l])
    return h
```

```python
import math
import numpy as np
import ml_dtypes
from contextlib import ExitStack
import concourse.bass as bass
import concourse.mybir as mybir
from concourse.bass_utils import run_bass_kernel_spmd

F32 = mybir.dt.float32
BF16 = mybir.dt.bfloat16
I32 = mybir.dt.int32
AF = mybir.ActivationFunctionType
ALU = mybir.AluOpType
AX = mybir.AxisListType

LN_EPS = 1e-5
RMS_EPS = 1e-6
ALPHA = 2.0 ** 0.25
NEGB = -30000.0
TWO_PI = 2.0 * math.pi
C1 = 6.28125
C2 = float(np.float32(TWO_PI - C1))
C3 = float(TWO_PI - C1 - float(np.float32(TWO_PI - C1)))
MAGIC = 12582912.0
PI_LIM = 3.1415925

VOFF = {}
_o = 0
for _n, _l in [("ln_emb_g", 1024), ("ln_emb_b", 1024), ("q_g", 768), ("kv_g", 256), ("oa_g", 512),
               ("ob_g", 512), ("ln_attn_g", 1024), ("ln_attn_b", 1024), ("ln_ffn_g", 1024),
               ("ln_ffn_b", 1024), ("sink", 8), ("b_router", 36), ("invA", 32), ("invB", 16)]:
    VOFF[_n] = (_o, _l)
    _o += _l
NVEC = _o


class TT:
    def __init__(s, h, name):
        s.h = h
        s.t = Tok(name)

    def __getitem__(s, k):
        return s.h[k]

    @property
    def ap(s):
        return s.h[:]


def build(NOWN, NOTH, stage="ABC", dbg=False):
    NJ = NOWN + NOTH + 2
    NK = NOWN + NOTH
    NW = NOWN + 2
    NQ = NOWN * 128
    nc = bass.Bass("TRN2", target_bir_lowering=False)
    S = Sched()
    es = ExitStack()

    def din(name, shape, dt=F32):
        return nc.dram_tensor(name, list(shape), dt, kind="ExternalInput").ap()

    def dscr(name, shape, dt):
        return nc.dram_tensor(name, list(shape), dt, kind="Internal").ap()

    def dout(name, shape, dt=F32):
        return nc.dram_tensor(name, list(shape), dt, kind="ExternalOutput").ap()

    xr = din("xr", [NJ * 128, 1024])
    posr = din("posr", [128, NJ], I32)
    w_in = din("w_in", [1024, 1824])
    w_q_b = din("w_q_b", [768, 768])
    w_kv_b = din("w_kv_b", [256, 1024])
    w_out = din("w_out", [1024, 1024])
    w_router = din("w_router", [1024, 36])
    w_gate = din("w_gate", [32, 1024, 256])
    w_up = din("w_up", [32, 1024, 256])
    w_down = din("w_down", [32, 256, 1024])
    vecs = din("vecs", [1, NVEC])
    cst_f = din("cst_f", [128, 128])
    cst_b = din("cst_b", [128, 128 + 4 * 512], BF16)
    y_out = dout("y", [NQ, 1024])

    KnT_s = dscr("KnT_s", [512, NK * 128], BF16)
    KrT_s = dscr("KrT_s", [32, NK * 128], BF16)
    V_s = dscr("V_s", [8, 128, NK * 72], BF16)
    h_s = dscr("h_s", [NQ, 1024], F32)
    mixa_s = dscr("mixa_s", [NQ, 512], BF16)
    outb_s = dscr("outb_s", [NQ, 512], F32)
    QT_s = dscr("QT_s", [97, 8, NQ], BF16)
    tk_QT = Tok("QT_s")
    tk_h = Tok("h_s")
    tk_mixa = Tok("mixa_s")
    tk_outb = Tok("outb_s")
    tk_KnT = Tok("KnT_s")
    tk_KrT = Tok("KrT_s")
    tk_V = Tok("V_s")

    dbg_outs = {}

    def sb(name, shape, dt, stack=None):
        h = (stack or es).enter_context(nc.sbuf_tensor(name, list(shape), dt))
        return TT(h, name)

    def OP(eng, meth, R, W, *a, **kw):
        return S.add(eng, lambda e: getattr(e, meth)(*a, **kw), [x.t if isinstance(x, TT) else x for x in R],
                     [x.t if isinstance(x, TT) else x for x in W])

    def DMA(q, out, in_, R, W):
        return S.add(q, lambda e: e.dma_start(out=out, in_=in_), [x.t if isinstance(x, TT) else x for x in R],
                     [x.t if isinstance(x, TT) else x for x in W], dma=True)

    def MM(out, lhsT, rhs, start, stop, R, W):
        return OP("pe", "matmul", R, W, out, lhsT, rhs, start=start, stop=stop)

    def TR(out, in_, ident, R, W):
        return OP("pe", "transpose", R, W, out, in_, ident)

    def ACT(out, in_, func, R, W, **kw):
        return OP("act", "activation", R, W, out, in_, func, **kw)

    def TTo(eng, out, in0, in1, op, R, W):
        return OP(eng, "tensor_tensor", R, W, out, in0, in1, op)

    def TSo(eng, out, in0, s1, s2, op0, op1, R, W):
        if op1 is None:
            return OP(eng, "tensor_scalar", R, W, out, in0, s1, None, op0)
        return OP(eng, "tensor_scalar", R, W, out, in0, s1, s2, op0, op1)

    def STT(eng, out, in0, sc, in1, op0, op1, R, W):
        return OP(eng, "scalar_tensor_tensor", R, W, out, in0, sc, in1, op0, op1)

    def CP(eng, out, in_, R, W):
        if eng == "act":
            return OP("act", "copy", R, W, out, in_)
        return OP(eng, "tensor_copy", R, W, out, in_)

    EPSC = {}

    def RSQ(dst, src, addc, R, W):
        col = EPSC[addc]
        ACT(dst, src, AF.Ln, list(R) + [epst], W, bias=epst[:, col:col + 1])
        ACT(dst, dst, AF.Exp, W, W, scale=-0.5)

    def RED(out, in_, op, R, W, axis=AX.X):
        return OP("dve", "tensor_reduce", R, W, out, in_, axis, op)

    PB = []
    PP = []
    for b in range(4):
        h = es.enter_context(nc.psum_tensor("pp%d" % b, [128, 1024], F32))
        PP.append(h)
        for half in range(2):
            PB.append(TT(h[:, half * 512:(half + 1) * 512], "pb%d" % (2 * b + half)))
            PB[-1].t.x = True

    def pbf(b):
        return PB[b].ap.bitcast(BF16)

    identf = sb("identf", [128, 128], F32)
    cstb = sb("cstb", [128, 128 + 2048], BF16)
    onesf = sb("onesf", [128, 128], F32)
    DMA("sp", identf.ap, cst_f, [], [identf])
    DMA("sp", cstb.ap, cst_b, [], [cstb])
    OP("pool", "memset", [], [onesf], onesf.ap, 1.0)
    identb = cstb[:, 0:128]
    epst = sb("epst", [128, 4], F32)
    for ci, ev in enumerate([LN_EPS, 256.0 * RMS_EPS, 768.0 * RMS_EPS, 512.0 * RMS_EPS]):
        EPSC[ev] = ci
        OP("pool", "memset", [], [epst], epst[:, ci:ci + 1], ev)

    def maskap(i):
        return cstb[:, 128 + i * 512:128 + (i + 1) * 512]

    def vload(dst, name, eng="sp"):
        o, l = VOFF[name]
        DMA(eng, dst.ap, vecs[0:1, o:o + l].to_broadcast([128, l]), [], [dst])

    esT = ExitStack()
    qn2b = sb("qn2b", [128, NOWN, 8], F32, esT)
    kmax2 = sb("kmax2", [128, 8], F32, esT)

    esA = ExitStack()
    A = lambda name, shape, dt: sb(name, shape, dt, esA)
    winb = A("winb", [128, 8, 1824], BF16)
    wqb = A("wqb", [128, 6, 768], BF16)
    wkvb = A("wkvb", [128, 2, 1024], BF16)
    wkb = A("wkb", [128, 2, 512], BF16)
    G_emb = A("G_emb", [128, 1024], F32)
    B_emb = A("B_emb", [128, 1024], F32)
    Gq = A("Gq", [128, 768], F32)
    Gkv = A("Gkv", [128, 256], F32)
    Ga = A("Ga", [128, 512], F32)
    sinkB = A("sinkB", [128, 8], F32)
    invA = A("invA", [128, 32], F32)
    invB = A("invB", [128, 16], F32)
    posi = A("posi", [128, NJ], I32)
    posf = A("posf", [128, NJ], F32)
    tabB = A("tabB", [128, NJ, 2, 16], F32)
    tabA = A("tabA", [128, NW, 2, 32], F32)
    esP = ExitStack()
    tabk = sb("tabk", [128, max(NJ * 32, NW * 64)], F32, esP)
    stg = [sb("stg%d" % i, [128, 1824], F32, esP) for i in range(2)]

    vload(G_emb, "ln_emb_g")
    vload(B_emb, "ln_emb_b")
    vload(Gq, "q_g")
    vload(Gkv, "kv_g")
    vload(Ga, "oa_g")
    vload(sinkB, "sink")
    vload(invA, "invA")
    vload(invB, "invB")
    DMA("sp", posi.ap, posr, [], [posi])
    TSo("dve", Gq.ap, Gq.ap, math.sqrt(768.0), None, ALU.mult, None, [Gq], [Gq])
    TSo("dve", Gkv.ap, Gkv.ap, 16.0, None, ALU.mult, None, [Gkv], [Gkv])
    TSo("dve", Ga.ap, Ga.ap, math.sqrt(512.0), None, ALU.mult, None, [Ga], [Ga])
    CP("dve", posf.ap, posi.ap, [posi], [posf])

    def make_table(tab, n, half, inv):
        tv = tab.ap
        kv = tabk[:, 0:n * 2 * half].rearrange("p (n t f) -> p n t f", t=2, f=half)
        pb_ = posf[:, 0:n].unsqueeze(2).to_broadcast([128, n, half])
        ib_ = inv.ap.unsqueeze(1).to_broadcast([128, n, half])
        TTo("dve", tv[:, :, 0, :], pb_, ib_, ALU.mult, [posf, inv], [tab])
        TSo("dve", tv[:, :, 1, :], tv[:, :, 0, :], math.pi / 2, None, ALU.add, None, [tab], [tab])
        TSo("dve", kv, tv, 1.0 / TWO_PI, MAGIC, ALU.mult, ALU.add, [tab], [tabk])
        TSo("dve", kv, kv, -MAGIC, None, ALU.add, None, [tabk], [tabk])
        STT("dve", tv, kv, -C1, tv, ALU.mult, ALU.add, [tabk, tab], [tab])
        STT("dve", tv, kv, -C2, tv, ALU.mult, ALU.add, [tabk, tab], [tab])
        STT("dve", tv, kv, -C3, tv, ALU.mult, ALU.add, [tabk, tab], [tab])
        TSo("dve", tv, tv, PI_LIM, -PI_LIM, ALU.min, ALU.max, [tab], [tab])
        ACT(tv, tv, AF.Sin, [tab], [tab])

    import os
    SKIP = os.environ.get("SKIP", "").split(",")
    if "tables" not in SKIP:
        make_table(tabB, NJ, 16, invB)
        make_table(tabA, NW, 32, invA)

    ncast = [0]

    def cast(out, in_, R, W, scale=None):
        eng = ("dve", "pool")[ncast[0] % 2]
        ncast[0] += 1
        if scale is None:
            CP(eng, out, in_, R, W)
        else:
            TSo(eng, out, in_, scale, None, ALU.mult, None, R, W)

    si = 0
    for c in range(8 if "weights" not in SKIP else 0):
        st_ = stg[si % 2]
        si += 1
        DMA("sp", st_[:, 0:1824], w_in[c * 128:(c + 1) * 128, :], [], [st_])
        cast(winb[:, c, 0:512], st_[:, 0:512], [st_], [winb], scale=0.125)
        cast(winb[:, c, 512:1824], st_[:, 512:1824], [st_], [winb])
    for c in range(6 if "weights" not in SKIP else 0):
        st_ = stg[si % 2]
        si += 1
        DMA("sp", st_[:, 0:768], w_q_b[c * 128:(c + 1) * 128, :], [], [st_])
        cast(wqb[:, c, :], st_[:, 0:768], [st_], [wqb], scale=96.0 ** -0.5)
    for c in range(2 if "weights" not in SKIP else 0):
        st_ = stg[si % 2]
        si += 1
        DMA("sp", st_[:, 0:1024], w_kv_b[c * 128:(c + 1) * 128, :], [], [st_])
        cast(wkvb[:, c, :], st_[:, 0:1024], [st_], [wkvb])
        cast(wkb[:, c, :].rearrange("p (h d) -> p h d", d=64),
             st_[:, 0:1024].rearrange("p (h x) -> p h x", x=128)[:, :, 0:64], [st_], [wkb])

    STOPAT = os.environ.get("STOPAT", "")
    if STOPAT == "P":
        d_tabB = dout("d_tabB", [128, NJ * 32])
        DMA("sp", d_tabB, tabB.ap.rearrange("p n t f -> p (n t f)"), [tabB], [Tok()])
        S.barrier()
        S.emit(nc, es, final=True)
        return nc
    S.barrier()
    S.emit(nc, es)
    esP.close()
    xt = [A("xt%d" % i, [128, 1024], F32) for i in range(2)]
    hf = [A("hf%d" % i, [128, 1024], F32) for i in range(2)]
    hb = [A("hb%d" % i, [128, 1024], BF16) for i in range(2)]
    hT = [A("hT%d" % i, [128, 8, 128], BF16) for i in range(2)]
    stt = [A("stt%d" % i, [128, 16], F32) for i in range(2)]
    kvb = [A("kvb%d" % i, [128, 288], BF16) for i in range(2)]
    sml = [A("sml%d" % i, [128, 32], F32) for i in range(2)]
    latT = [A("latT%d" % i, [128, 2, 128], BF16) for i in range(2)]
    KTbuf = [A("KTbuf%d" % i, [128, 4, 512], BF16) for i in range(2)]
    KrTbuf = [A("KrTbuf%d" % i, [32, 512], BF16) for i in range(2)]
    Vbuf = [A("Vbuf%d" % i, [128, 8, 4, 72], BF16) for i in range(2)]
    kn2 = A("kn2", [128, 8], F32)
    cqn = A("cqn", [128, 768], BF16)
    cqT = A("cqT", [128, 6, 128], BF16)
    qtok = A("qtok", [128, 8, 96], BF16)
    qst = [A("qst%d" % i, [96, 8, 128], BF16) for i in range(2)]
    KaT = A("KaT", [65, 2, 8 * 128], BF16)
    Va = A("Va", [128, 8, 2, 72], BF16)
    kmaxa = A("kmaxa", [128, NW, 2], F32)
    katok = A("katok", [128, 128], BF16)
    qatok = [A("qatok%d" % i, [128, 8, 65], BF16) for i in range(6)]
    qn2a = [A("qn2a%d" % i, [128, 8], F32) for i in range(6)]
    PTa = [A("PTa%d" % i, [128, 512], BF16) for i in range(2)]
    Osb = A("Osb", [65, 512], F32)
    Osb_default = Osb
    mixa = [A("mixa%d" % i, [128, 512], BF16) for i in range(2)]
    wsm = A("wsm", [128, 64], F32)
    wsm_default = wsm

    for v in Vbuf:
        OP("pool", "memset", [], [v], v.ap, 0.0)
        OP("pool", "memset", [], [v], v[:, :, :, 64:65], 1.0)
    OP("pool", "memset", [], [Va], Va.ap, 0.0)
    OP("pool", "memset", [], [Va], Va[:, :, :, 64:65], 1.0)
    OP("pool", "memset", [], [KaT], KaT[64:65, :, :], 1.0)
    OP("pool", "memset", [], [kmax2], kmax2.ap, 0.0)

    def rope(src, H, half, cos, sin, dst, R, W, tmps=None, scl=None):
        n = H * 2 * half
        tA_, tB_ = tmps
        tA = tA_[:, 0:n].rearrange("p (h t f) -> p h t f", t=2, f=half)
        tB = tB_[:, 0:n].rearrange("p (h t f) -> p h t f", t=2, f=half)
        cb = cos.unsqueeze(1).unsqueeze(1).to_broadcast([128, H, 2, half])
        sb_ = sin.unsqueeze(1).to_broadcast([128, H, half])
        if scl is None:
            TTo("dve", tA, src, cb, ALU.mult, R, [tA_])
            TTo("dve", tB[:, :, 0, :], src[:, :, 1, :], sb_, ALU.mult, R, [tB_])
            TTo("dve", tB[:, :, 1, :], src[:, :, 0, :], sb_, ALU.mult, R, [tB_])
        else:
            cb3 = cos.unsqueeze(1).to_broadcast([128, H, half])
            STT("dve", tA[:, :, 0, :], src[:, :, 0, :], scl, cb3, ALU.mult, ALU.mult, R, [tA_])
            STT("dve", tA[:, :, 1, :], src[:, :, 1, :], scl, cb3, ALU.mult, ALU.mult, R, [tA_])
            STT("dve", tB[:, :, 0, :], src[:, :, 1, :], scl, sb_, ALU.mult, ALU.mult, R, [tB_])
            STT("dve", tB[:, :, 1, :], src[:, :, 0, :], scl, sb_, ALU.mult, ALU.mult, R, [tB_])
        TTo("dve", dst[:, :, 0, :], tA[:, :, 0, :], tB[:, :, 0, :], ALU.subtract, [tA_, tB_], W)
        TTo("dve", dst[:, :, 1, :], tA[:, :, 1, :], tB[:, :, 1, :], ALU.add, [tA_, tB_], W)

    def pmax_bcast(src_ap, src_tok, ncol, dst_ap, dst_tok, bank, wsm=None):
        if wsm is None:
            wsm = wsm_default
        pb_ = PB[bank]
        TR(pb_[0:ncol, 0:128], src_ap, identf.ap, [src_tok, identf], [pb_])
        RED(wsm[0:ncol, 0:1], pb_[0:ncol, 0:128], ALU.max, [pb_], [wsm])
        TSo("dve", wsm[0:ncol, 8:8 + ncol], identf[0:ncol, 0:ncol], wsm[0:ncol, 0:1], None, ALU.mult, None,
            [wsm, identf], [wsm])
        MM(pb_[:, 256:256 + ncol], onesf[0:ncol, :], wsm[0:ncol, 8:8 + ncol], True, True, [onesf, wsm], [pb_])
        CP("dve", dst_ap, pb_[:, 256:256 + ncol], [pb_], [dst_tok])

    evi = [0]

    def evac(out, in_, R, W):
        eng = ("act", "dve")[evi[0] % 2]
        evi[0] += 1
        CP(eng, out, in_, R, W)

    wsm9 = [A("wsm9_%d" % i, [128, 64], F32) for i in range(2)]
    QaT2 = [A("QaT2_%d" % i, [65, 1024], BF16) for i in range(2)]
    oa2 = [A("oa2_%d" % i, [128, 8, 64], F32) for i in range(2)]
    sq9 = A("sq9", [128, 512], F32)
    for w9 in wsm9:
        OP("pool", "memset", [], [w9], w9.ap, 0.0)

    def wa_a(n):
        w = n + 1
        sq_ = n % 6
        qa_ = qatok[sq_]
        wsm = wsm9[n % 2]
        TTo("dve", wsm[:, 16:18], kmaxa[:, w - 1, :], kmaxa[:, w, :], ALU.max, [kmaxa], [wsm])
        TTo("dve", wsm[:, 16:18], wsm[:, 16:18], kmaxa[:, w + 1, :], ALU.max, [kmaxa, wsm], [wsm])
        TTo("dve", wsm[:, 20:28].rearrange("p (g f) -> p g f", f=4),
            qn2a[sq_].ap.rearrange("p (g f) -> p g f", f=4),
            wsm[:, 16:18].unsqueeze(2).to_broadcast([128, 2, 4]), ALU.mult, [qn2a[sq_], wsm], [wsm])
        ACT(wsm[:, 20:28], wsm[:, 20:28], AF.Ln, [wsm], [wsm])
        ACT(wsm[:, 20:28], wsm[:, 20:28], AF.Exp, [wsm], [wsm], scale=0.5)
        TSo("dve", qa_[:, :, 64:65], wsm[:, 20:28].unsqueeze(2), -1.0, None, ALU.mult, None, [wsm], [qa_])
        TTo("dve", wsm[:, 28:36].unsqueeze(2), qa_[:, :, 64:65], sinkB.ap.unsqueeze(2), ALU.add, [qa_, sinkB], [wsm])
        ACT(wsm[:, 36:44], wsm[:, 28:36], AF.Exp, [wsm], [wsm])
        for h in range(8):
            TR(pbf(0)[0:65, h * 128:(h + 1) * 128], qa_[:, h, 0:65], identb, [qa_, cstb], [PB[0]])
        evac(QaT2[n % 2][:, :], pbf(0)[0:65, :], [PB[0]], [QaT2[n % 2]])

    PTa2 = [A("PTa2_%d" % i, [128, 512], BF16) for i in range(2)]
    Osb2 = A("Osb2", [65, 512], F32)

    def capture(fn):
        rec = []
        orig = S.add
        S.add = lambda *a, **kw: rec.append((a, kw))
        try:
            fn()
        finally:
            S.add = orig
        return rec

    def interleave2(fa, fb):
        ra = capture(fa) if fa is not None else []
        rb = capture(fb) if fb is not None else []
        i = 0
        while i < len(ra) or i < len(rb):
            if i < len(ra):
                S.add(*ra[i][0], **ra[i][1])
            if i < len(rb):
                S.add(*rb[i][0], **rb[i][1])
            i += 1

    def wa_g(n, g, bset=0):
        w = n + 1
        wsm = wsm9[n % 2]
        QaT = QaT2[n % 2]
        oa = oa2[n % 2]
        first_tile = (n == 0)
        last_tile = (n == NOWN - 1)
        if bset == 0:
            bo, bsl, bt, PTx, Osb = PB[5], (PB[6], PB[7]), PB[4], PTa, Osb_default
        else:
            bo, bsl, bt, PTx, Osb = PB[1], (PB[2], PB[3]), PB[0], PTa2, Osb2
        kts = [w - 1, w, w + 1]
        for ki, kt in enumerate(kts):
            bs = bsl[ki % 2]
            pt = PTx[ki % 2]
            MM(bs.ap, KaT[0:65, g, (kt % 8) * 128:(kt % 8 + 1) * 128], QaT[:, g * 512:(g + 1) * 512], True, kt == w,
               [KaT, QaT], [bs])
            if kt != w:
                if kt < w:
                    mi = 2 if first_tile else 0
                else:
                    mi = 3 if last_tile else 1
                MM(bs.ap, identb, maskap(mi), False, True, [cstb], [bs])
            ACT(pt.ap, bs.ap, AF.Exp, [bs], [pt])
            MM(bo[0:65, :], Va[:, kt % 8, g, 0:65], pt.ap, ki == 0, ki == 2, [Va, pt], [bo])
        CP("dve", Osb.ap, bo[0:65, :], [bo], [Osb])
        btv = bt[:, 0:260].rearrange("p (i x) -> p i x", x=65)
        for i in range(4):
            TR(bt[:, i * 65:(i + 1) * 65], Osb[:, i * 128:(i + 1) * 128], identf[0:65, 0:65], [Osb, identf], [bt])
        TTo("dve", wsm[:, 44:48].unsqueeze(2), btv[:, :, 64:65], wsm[:, 36 + g * 4:40 + g * 4].unsqueeze(2),
            ALU.add, [bt, wsm], [wsm])
        OP("dve", "reciprocal", [wsm], [wsm], wsm[:, 48:52], wsm[:, 44:48])
        TTo("dve", oa[:, g * 4:(g + 1) * 4, :], btv[:, :, 0:64],
            wsm[:, 48:52].unsqueeze(2).to_broadcast([128, 4, 64]), ALU.mult, [bt, wsm], [oa])

    def wa_pair(j):
        fa = (lambda: wa_g(j - 2, 1, 0)) if has_wa(j) else None
        fb = (lambda: wa_g(j - 1, 0, 1)) if has_wa(j + 1) else None
        interleave2(fa, fb)

    def wa_d(n):
        wsm = wsm9[n % 2]
        oa = oa2[n % 2]
        oaf = oa.ap.rearrange("p h d -> p (h d)")
        ACT(sq9.ap, oaf, AF.Square, [oa], [sq9])
        RED(wsm[:, 52:53], sq9.ap, ALU.add, [sq9], [wsm])
        RSQ(wsm[:, 53:54], wsm[:, 52:53], 512.0 * RMS_EPS, [wsm], [wsm])
        mx = mixa[n % 2]
        STT("dve", mx.ap, oaf, wsm[:, 53:54], Ga.ap, ALU.mult, ALU.mult, [oa, wsm, Ga], [mx])
        DMA("sp", mixa_s[n * 128:(n + 1) * 128, :], mx.ap, [mx], [tk_mixa])

    sq3 = [A("sq3_%d" % i, [128, 288], F32) for i in range(2)]
    sq5 = A("sq5", [128, 512], F32)
    sq7 = A("sq7", [128, 128], F32)
    sq8 = A("sq8", [128, 768], F32)
    rt3 = [(A("rt3a%d" % i, [128, 32], F32), A("rt3b%d" % i, [128, 32], F32)) for i in range(2)]
    rt7 = (A("rt7a", [128, 128], F32), A("rt7b", [128, 128], F32))
    rt8a = (A("rt8aa", [128, 512], F32), A("rt8ab", [128, 512], F32))
    rt8d = (A("rt8da", [128, 128], F32), A("rt8db", [128, 128], F32))
    sm8s = [A("sm8_%d" % i, [128, 8], F32) for i in range(2)]
    kvf = [A("kvf%d" % i, [128, 288], F32) for i in range(4)]
    xn = [A("xn%d" % i, [128, 1024], F32) for i in range(2)]
    kr2all = A("kr2all", [128, NJ], F32)
    ssqkv = A("ssqkv", [128, NJ], F32)
    wsm7 = A("wsm7", [128, 64], F32)
    keyidx = {}
    _kt = 0
    for j in range(NJ):
        if j not in (0, NOWN + 1):
            keyidx[j] = _kt
            _kt += 1

    def proj(hT_, bank, ncols, col0):
        for c in range(8):
            MM(PB[bank][:, 0:ncols], hT_[:, c, :], winb[:, c, col0:col0 + ncols], c == 0, c == 7,
               [hT_, winb], [PB[bank]])

    is_key_ = lambda j: j not in (0, NOWN + 1)
    is_win_ = lambda j: j <= NOWN + 1
    is_own_ = lambda j: 1 <= j <= NOWN

    def sA0(j):
        s = j % 2
        x_, st_ = xt[s], stt[s]
        if j == 0:
            DMA("pool", x_.ap, xr[0:128, :], [], [x_])
        if j + 1 < NJ:
            DMA("pool", xt[(j + 1) % 2].ap, xr[(j + 1) * 128:(j + 2) * 128, :], [], [xt[(j + 1) % 2]])
        OP("dve", "bn_stats", [x_], [st_], st_[:, 0:6], x_[:, 0:512])
        OP("dve", "bn_stats", [x_], [st_], st_[:, 6:12], x_[:, 512:1024])
        OP("dve", "bn_aggr", [st_], [st_], st_[:, 12:14], st_[:, 0:12])
        TSo("dve", st_[:, 14:15], st_[:, 13:14], LN_EPS, None, ALU.add, None, [st_], [st_])

    def sA1(j):
        s = j % 2
        x_, st_, xn_ = xt[s], stt[s], xn[s]
        ACT(st_[:, 14:15], st_[:, 14:15], AF.Ln, [st_], [st_])
        ACT(st_[:, 14:15], st_[:, 14:15], AF.Exp, [st_], [st_], scale=-0.5)
        ACT(st_[:, 15:16], st_[:, 12:13], AF.Identity, [st_], [st_], scale=st_[:, 14:15])
        OP("act", "mul", [st_], [st_], st_[:, 15:16], st_[:, 15:16], -1.0)
        ACT(xn_.ap, x_.ap, AF.Identity, [x_, st_], [xn_], bias=st_[:, 15:16], scale=st_[:, 14:15])

    def sA2(j):
        xn_ = xn[j % 2]
        TTo("pool", xn_.ap, xn_.ap, G_emb.ap, ALU.mult, [xn_, G_emb], [xn_])

    def sA3(j):
        s = j % 2
        xn_, hf_, hb_ = xn[s], hf[s], hb[s]
        if is_own_(j):
            TTo("dve", hf_.ap, xn_.ap, B_emb.ap, ALU.add, [xn_, B_emb], [hf_])
        else:
            TTo("dve", hb_.ap, xn_.ap, B_emb.ap, ALU.add, [xn_, B_emb], [hb_])

    def sA4(j):
        s = j % 2
        hf_, hb_ = hf[s], hb[s]
        DMA("sp", h_s[(j - 1) * 128:j * 128, :], hf_.ap, [hf_], [tk_h])
        CP("act", hb_.ap, hf_.ap, [hf_], [hb_])

    def sA5(j):
        hb_ = hb[j % 2]
        for c in range(8):
            TR(pbf(0)[:, c * 128:(c + 1) * 128], hb_[:, c * 128:(c + 1) * 128], identb, [hb_, cstb], [PB[0]])

    def sA6(j):
        CP(("act", "dve")[j % 2], hT[j % 2].ap, pbf(0).rearrange("p (c t) -> p c t", t=128), [PB[0]], [hT[j % 2]])

    def sA7(j):
        proj(hT[j % 2], 1, 288, 1536)

    def sA8(j):
        kf = kvf[j % 4]
        p1 = PB[1]
        CP("act", kf.ap, p1[:, 0:288], [p1], [kf])
        ACT(sq3[j % 2].ap, kf.ap, AF.Square, [kf], [sq3[j % 2]])

    def sA9(j):
        kf = kvf[j % 4]
        n = 32
        tA_, tB_ = rt3[j % 2]
        src = kf[:, 256:288].rearrange("p (h t f) -> p h t f", h=1, t=2)
        tA = tA_[:, 0:n].rearrange("p (h t f) -> p h t f", t=2, f=16)
        tB = tB_[:, 0:n].rearrange("p (h t f) -> p h t f", t=2, f=16)
        cos, sin = tabB[:, j, 1, :], tabB[:, j, 0, :]
        cb = cos.unsqueeze(1).unsqueeze(1).to_broadcast([128, 1, 2, 16])
        sb_ = sin.unsqueeze(1).to_broadcast([128, 1, 16])
        TTo("dve", tA, src, cb, ALU.mult, [kf, tabB], [tA_])
        TTo("dve", tB[:, :, 0, :], src[:, :, 1, :], sb_, ALU.mult, [kf, tabB], [tB_])
        TTo("dve", tB[:, :, 1, :], src[:, :, 0, :], sb_, ALU.mult, [kf, tabB], [tB_])
        RED(ssqkv[:, j:j + 1], sq3[j % 2][:, 0:256], ALU.add, [sq3[j % 2]], [ssqkv])
        RED(kr2all[:, j:j + 1], sq3[j % 2][:, 256:288], ALU.add, [sq3[j % 2]], [kr2all])
        TSo("dve", ssqkv[:, j:j + 1], ssqkv[:, j:j + 1], 256.0 * RMS_EPS, None, ALU.add, None, [ssqkv], [ssqkv])

    def sA10(j):
        kv_ = kvb[j % 2]
        tA_, tB_ = rt3[j % 2]
        tA = tA_[:, 0:32].rearrange("p (h t f) -> p h t f", t=2, f=16)
        tB = tB_[:, 0:32].rearrange("p (h t f) -> p h t f", t=2, f=16)
        dst = kv_[:, 256:288].rearrange("p (h t f) -> p h t f", h=1, t=2)
        ACT(ssqkv[:, j:j + 1], ssqkv[:, j:j + 1], AF.Ln, [ssqkv], [ssqkv])
        ACT(ssqkv[:, j:j + 1], ssqkv[:, j:j + 1], AF.Exp, [ssqkv], [ssqkv], scale=-0.5)
        TTo("pool", dst[:, :, 0, :], tA[:, :, 0, :], tB[:, :, 0, :], ALU.subtract, [tA_, tB_], [kv_])
        TTo("pool", dst[:, :, 1, :], tA[:, :, 1, :], tB[:, :, 1, :], ALU.add, [tA_, tB_], [kv_])

    def sA11(j):
        kf, kv_ = kvf[j % 4], kvb[j % 2]
        STT("dve", kv_[:, 0:256], kf[:, 0:256], ssqkv[:, j:j + 1], Gkv.ap, ALU.mult, ALU.mult, [kf, ssqkv, Gkv], [kv_])

    def sA12(j):
        kv_ = kvb[j % 2]
        for c in range(2):
            TR(pbf(2)[:, c * 128:(c + 1) * 128], kv_[:, c * 128:(c + 1) * 128], identb, [kv_, cstb], [PB[2]])
        TR(pbf(2)[0:32, 256:384], kv_[:, 256:288], identb, [kv_, cstb], [PB[2]])

    def sA13(j):
        kt = keyidx[j]
        g, q = (kt // 4) % 2, kt % 4
        lt_ = latT[j % 2]
        CP("act", lt_.ap, pbf(2)[:, 0:256].rearrange("p (c t) -> p c t", t=128), [PB[2]], [lt_])
        CP("act", KrTbuf[g][:, q * 128:(q + 1) * 128], pbf(2)[0:32, 256:384], [PB[2]], [KrTbuf[g]])

    def sA14(j):
        lt_ = latT[j % 2]
        for half in range(2):
            for c in range(2):
                MM(PB[3 + half].ap, lt_[:, c, :], wkvb[:, c, half * 512:(half + 1) * 512], c == 0, c == 1,
                   [lt_, wkvb], [PB[3 + half]])
        p5 = PB[5]
        for pair in range(4):
            for c in range(2):
                MM(p5[:, pair * 128:(pair + 1) * 128], wkb[:, c, pair * 128:(pair + 1) * 128], lt_[:, c, :],
                   c == 0, c == 1, [wkb, lt_], [p5])

    def sA15(j):
        kt = keyidx[j]
        g, q = (kt // 4) % 2, kt % 4
        for half in range(2):
            pv = PB[3 + half].ap.rearrange("p (h x) -> p h x", x=128)
            CP("dve", Vbuf[g][:, half * 4:(half + 1) * 4, q, 0:64], pv[:, :, 64:128], [PB[3 + half]], [Vbuf[g]])
            ACT(sq5.ap.rearrange("p (h d) -> p h d", d=64)[:, half * 4:(half + 1) * 4, :], pv[:, :, 0:64],
                AF.Square, [PB[3 + half]], [sq5])
        p5 = PB[5]
        CP(("dve", "act")[j % 2], KTbuf[g][:, :, q * 128:(q + 1) * 128], p5.ap.rearrange("p (a t) -> p a t", t=128),
           [p5], [KTbuf[g]])

    def sA16(j):
        kt = keyidx[j]
        g, q = (kt // 4) % 2, kt % 4
        RED(kn2.ap, sq5.ap.rearrange("p (h d) -> p h d", d=64), ALU.add, [sq5], [kn2])
        STT("dve", kmax2.ap, kn2.ap, kr2all[:, j:j + 1], kmax2.ap, ALU.add, ALU.max, [kn2, kr2all, kmax2], [kmax2])
        if q == 3 or kt == NK - 1:
            kt0 = kt - q
            nq = q + 1
            DMA("sp", KnT_s.rearrange("(a p) t -> p a t", p=128)[:, :, kt0 * 128:(kt0 + nq) * 128],
                KTbuf[g][:, :, 0:nq * 128], [KTbuf[g]], [tk_KnT])
            DMA("sp", KrT_s[:, kt0 * 128:(kt0 + nq) * 128], KrTbuf[g][:, 0:nq * 128], [KrTbuf[g]], [tk_KrT])
            DMA("sp", V_s.rearrange("h p f -> p h f")[:, :, kt0 * 72:(kt0 + nq) * 72],
                Vbuf[g][:, :, 0:nq, :].rearrange("p h q x -> p h (q x)"), [Vbuf[g]], [tk_V])

    def st7(j):
        s = j % 2
        w = j
        hT_ = hT[s]
        proj(hT_, 6, 256, 512)
        p6 = PB[6]
        rope(p6[:, 0:128].rearrange("p (h t f) -> p h t f", h=2, t=2), 2, 32, tabA[:, w, 1, :], tabA[:, w, 0, :],
             katok.ap.rearrange("p (h t f) -> p h t f", h=2, t=2), [p6, tabA], [katok], rt7)
        ACT(sq7.ap, p6[:, 0:128], AF.Square, [p6], [sq7])
        CP("act", Va[:, w % 8, :, 0:64], p6[:, 128:256].rearrange("p (h d) -> p h d", d=64), [p6], [Va])
        RED(wsm7[:, 56:58], sq7.ap.rearrange("p (h d) -> p h d", d=64), ALU.add, [sq7], [wsm7])
        pmax_bcast(wsm7[:, 56:58], wsm7.t, 2, kmaxa[:, w, :], kmaxa.t, 7, wsm7)
        for g_ in range(2):
            TR(pbf(2)[0:64, g_ * 128:(g_ + 1) * 128], katok[:, g_ * 64:(g_ + 1) * 64], identb, [katok, cstb], [PB[2]])
        evac(KaT[0:64, :, (w % 8) * 128:(w % 8 + 1) * 128], pbf(2)[0:64, 0:256].rearrange("p (g t) -> p g t", t=128),
             [PB[2]], [KaT])

    def st8a(j):
        s = j % 2
        n = j - 1
        sq_ = n % 6
        proj(hT[s], 3, 512, 0)
        p7 = PB[3]
        rope(p7.ap.rearrange("p (h t f) -> p h t f", h=8, t=2), 8, 32, tabA[:, j, 1, :], tabA[:, j, 0, :],
             qatok[sq_][:, :, 0:64].rearrange("p h (t f) -> p h t f", t=2), [p7, tabA], [qatok[sq_]], rt8a)
        ACT(sq8[:, 0:512], p7.ap, AF.Square, [p7], [sq8])
        RED(qn2a[sq_].ap, sq8[:, 0:512].rearrange("p (h d) -> p h d", d=64), ALU.add, [sq8], [qn2a[sq_]])

    def st8b(j):
        s = j % 2
        sm8 = sm8s[s]
        proj(hT[s], 6, 384, 768)
        proj(hT[s], 7, 384, 1152)
        ACT(sq8[:, 0:384], PB[6][:, 0:384], AF.Square, [PB[6]], [sq8])
        ACT(sq8[:, 384:768], PB[7][:, 0:384], AF.Square, [PB[7]], [sq8])
        STT("dve", cqn[:, 0:384], PB[6][:, 0:384], 1.0, Gq[:, 0:384], ALU.mult, ALU.mult, [PB[6], Gq], [cqn])
        STT("dve", cqn[:, 384:768], PB[7][:, 0:384], 1.0, Gq[:, 384:768], ALU.mult, ALU.mult, [PB[7], Gq], [cqn])
        RED(sm8[:, 4:5], sq8[:, 0:768], ALU.add, [sq8], [sm8])
        RSQ(sm8[:, 5:6], sm8[:, 4:5], 768.0 * RMS_EPS, [sm8], [sm8])

    def st8c(j):
        for c in range(6):
            TR(pbf(0)[:, c * 128:(c + 1) * 128], cqn[:, c * 128:(c + 1) * 128], identb, [cqn, cstb], [PB[0]])
        evac(cqT.ap, pbf(0)[:, 0:768].rearrange("p (c t) -> p c t", t=128), [PB[0]], [cqT])

    def st8d(j):
        n = j - 1
        sm8 = sm8s[j % 2]
        for half in range(2):
            for c in range(6):
                MM(PB[6 + half][:, 0:384], cqT[:, c, :], wqb[:, c, half * 384:(half + 1) * 384], c == 0, c == 5,
                   [cqT, wqb], [PB[6 + half]])
        for half in range(2):
            pq = PB[6 + half][:, 0:384].rearrange("p (h x) -> p h x", x=96)
            OP("act", "activation", [PB[6 + half], sm8], [qtok], qtok[:, half * 4:(half + 1) * 4, 0:64], pq[:, :, 0:64],
               AF.Identity, scale=sm8[:, 5:6])
            ACT(sq8[:, 0:768].rearrange("p (h x) -> p h x", x=96)[:, half * 4:(half + 1) * 4, :], pq, AF.Square,
                [PB[6 + half], sm8], [sq8], scale=sm8[:, 5:6])
            rope(pq[:, :, 64:96].rearrange("p h (t f) -> p h t f", t=2), 4, 16, tabB[:, j, 1, :], tabB[:, j, 0, :],
                 qtok[:, half * 4:(half + 1) * 4, 64:96].rearrange("p h (t f) -> p h t f", t=2),
                 [PB[6 + half], tabB, sm8], [qtok], rt8d, scl=sm8[:, 5:6])
        RED(qn2b[:, n, :], sq8[:, 0:768].rearrange("p (h x) -> p h x", x=96), ALU.add, [sq8], [qn2b])

    def st8e(j):
        n = j - 1
        for h in range(8):
            TR(pbf(0)[0:96, h * 128:(h + 1) * 128], qtok[:, h, :], identb, [qtok, cstb], [PB[0]])
        evac(qst[n % 2].ap, pbf(0)[0:96, :].rearrange("p (h t) -> p h t", t=128), [PB[0]], [qst[n % 2]])
        DMA("sp", QT_s[0:96, :, n * 128:(n + 1) * 128], qst[n % 2].ap, [qst[n % 2]], [tk_QT])

    has_wa = lambda j: 2 <= j <= NOWN + 1 and "wattn" not in SKIP
    def m2(f, g_):
        def h(j):
            f(j)
            g_(j)
        return h
    def interleaveN(fns):
        recs = [capture(f) for f in fns if f is not None]
        i = 0
        while any(i < len(r) for r in recs):
            for r in recs:
                if i < len(r):
                    S.add(*r[i][0], **r[i][1])
            i += 1

    def grp(*members):
        def h(j):
            interleaveN([(lambda f=f: f(j)) for f, p in members if p(j)])
        return h, (lambda j: any(p(j) for _, p in members))

    ALL = lambda j: True
    g12 = grp((sA16, is_key_), (lambda j: wa_d(j - 2), has_wa))
    g10 = grp((m2(sA12, sA13), is_key_), (st8e, is_own_))
    g9 = grp((sA11, is_key_), (st8d, is_own_), (lambda j: wa_a(j - 2), has_wa))
    g8 = grp((sA10, is_key_), (st8c, is_own_))
    g7 = grp((sA9, is_key_), (st8b, is_own_))
    g6 = grp((m2(sA7, sA8), is_key_), (st7, is_win_), (st8a, is_own_))
    stages = [
        (0, sA0, ALL),
        (1, sA1, ALL),
        (2, sA2, ALL),
        (3, sA3, ALL),
        (4, sA4, is_own_),
        (5, m2(sA5, sA6), ALL),
        (6, g6[0], g6[1]),
        (7, g7[0], g7[1]),
        (8, g8[0], g8[1]),
        (9, g9[0], g9[1]),
        (10, g10[0], g10[1]),
        (11, m2(sA14, sA15), is_key_),
        (11, wa_pair, lambda j: has_wa(j) or has_wa(j + 1)),
        (12, g12[0], g12[1]),
    ]
    maxoff = max(o for o, _, _ in stages)
    for step in range(NJ + maxoff):
        for off, fn, pred in sorted(stages, key=lambda x: -x[0]):
            j = step - off
            if 0 <= j < NJ and pred(j):
                fn(j)
        if STOPAT == "S%d" % step:
            S.barrier()
            S.emit(nc, es, final=True)
            return nc


    if dbg:
        d_tabB = dout("d_tabB", [128, NJ * 32])
        DMA("sp", d_tabB, tabB.ap.rearrange("p n t f -> p (n t f)"), [tabB], [Tok()])
    S.barrier()
    S.emit(nc, es)
    esA.close()
    esX = ExitStack()
    nmT = sb("nmT", [8, NQ], BF16, esX)
    nm = sb("nm", [128, NOWN, 8], BF16, esX)
    wsm = sb("wsm2", [128, 64], F32, esX)
    sqs = sb("sqs2", [128, NOWN * 8], F32, esX)
    pmax_bcast(kmax2.ap, kmax2.t, 8, wsm[:, 56:64], wsm.t, 7, wsm)
    for n in range(NOWN):
        TTo("dve", sqs[:, n * 8:(n + 1) * 8], qn2b[:, n, :], wsm[:, 56:64], ALU.mult, [qn2b, wsm], [sqs])
    ACT(sqs[:, 0:NOWN * 8], sqs[:, 0:NOWN * 8], AF.Ln, [sqs], [sqs])
    ACT(sqs[:, 0:NOWN * 8], sqs[:, 0:NOWN * 8], AF.Exp, [sqs], [sqs], scale=0.5)
    TSo("dve", nm.ap.rearrange("p n h -> p (n h)"), sqs[:, 0:NOWN * 8], -1.0, None, ALU.mult, None, [sqs], [nm])
    for n0 in range(0, NOWN, 8):
        nn = min(8, NOWN - n0)
        for i in range(nn):
            TR(pbf(0)[0:8, i * 128:(i + 1) * 128], nm[:, n0 + i, :], identb, [nm, cstb], [PB[0]])
        evac(nmT[:, n0 * 128:(n0 + nn) * 128], pbf(0)[0:8, 0:nn * 128], [PB[0]], [nmT])
    DMA("sp", QT_s[96], nmT.ap, [nmT], [tk_QT])

    if dbg:
        d_QT = dout("d_QT", [97, 8 * NQ], BF16)
        DMA("sp", d_QT, QT_s.rearrange("p h n -> p (h n)"), [tk_QT], [Tok()])
        d_KnT = dout("d_KnT", [512, NK * 128], BF16)
        DMA("sp", d_KnT, KnT_s, [tk_KnT], [Tok()])
        d_KrT = dout("d_KrT", [32, NK * 128], BF16)
        DMA("sp", d_KrT, KrT_s, [tk_KrT], [Tok()])
        d_V = dout("d_V", [8 * 128, NK * 72], BF16)
        DMA("sp", d_V, V_s.rearrange("h p f -> (h p) f"), [tk_V], [Tok()])
        if "wattn" not in SKIP:
            d_mixa = dout("d_mixa", [NQ, 512], BF16)
            DMA("sp", d_mixa, mixa_s, [tk_mixa], [Tok()])
        d_h = dout("d_h", [NQ, 1024], F32)
        DMA("sp", d_h, h_s, [tk_h], [Tok()])

    S.barrier()
    S.emit(nc, es, final=(stage == "A"))
    esX.close()
    esT.close()
    print("phase A ops", len(S.ops), "standalone waits", S.nwait)

    if stage == "A":
        es.close()
        return nc

    esB = ExitStack()
    Bt = lambda name, shape, dt: sb(name, shape, dt, esB)
    KT = [Bt("KT%d" % i, [97, NK * 128], BF16) for i in range(2)]
    Vh = [Bt("Vh%d" % i, [128, NK, 72], BF16) for i in range(2)]
    QTh = [Bt("QTh%d" % i, [97, NQ], BF16) for i in range(2)]
    OsbB = [Bt("OsbB%d" % i, [65, 512], F32) for i in range(2)]
    obt = [Bt("obt%d" % i, [128, 4, 64], F32) for i in range(2)]
    wsB = [Bt("wsB%d" % i, [128, 8], F32) for i in range(2)]
    for kt_ in KT:
        OP("pool", "memset", [], [kt_], kt_[64:97, :], 1.0)
    NQB = NOWN // 4

    def load_head(h):
        hb_ = h % 2
        half = (NK * 128) // 2
        DMA("sp", KT[hb_][0:64, 0:half], KnT_s[h * 64:(h + 1) * 64, 0:half], [tk_KnT], [KT[hb_]])
        DMA("pool", KT[hb_][0:64, half:], KnT_s[h * 64:(h + 1) * 64, half:], [tk_KnT], [KT[hb_]])
        DMA("sp", KT[hb_][64:96, :], KrT_s, [tk_KrT], [KT[hb_]])
        DMA("pool", Vh[hb_].ap.rearrange("p c x -> p (c x)"), V_s[h], [tk_V], [Vh[hb_]])
        DMA("sp", QTh[hb_].ap, QT_s[:, h, :], [tk_QT], [QTh[hb_]])

    PT = [Bt("PT%d" % i, [128, 512], BF16) for i in range(4)]
    units = [(h, qb, c) for h in range(8) for qb in range(NQB) for c in range(NK)]
    NU = len(units)
    LA = 2
    pending = []

    def epilogue(h, qb, blk):
        ob_ = PB[4 + blk % 2]
        os_ = OsbB[blk % 2]
        bt = PB[6 + blk % 2]
        ot = obt[blk % 2]
        ws = wsB[blk % 2]
        CP("dve", os_.ap, ob_[0:65, :], [ob_], [os_])

        def part2():
            btv = bt[:, 0:260].rearrange("p (i x) -> p i x", x=65)
            for i in range(4):
                TR(bt[:, i * 65:(i + 1) * 65], os_[:, i * 128:(i + 1) * 128], identf[0:65, 0:65], [os_, identf], [bt])
            OP("dve", "reciprocal", [bt], [ws], ws[:, 0:4].unsqueeze(2), btv[:, :, 64:65])
            TTo("dve", ot.ap, btv[:, :, 0:64], ws[:, 0:4].unsqueeze(2).to_broadcast([128, 4, 64]), ALU.mult,
                [bt, ws], [ot])
            DMA("pool", outb_s[qb * 512:(qb + 1) * 512, h * 64:(h + 1) * 64].rearrange("(i p) d -> p i d", p=128),
                ot.ap, [ot], [tk_outb])
        return part2

    for i in range(NU + LA):
        while pending and pending[0][0] <= i:
            pending.pop(0)[1]()
        if i < NU:
            h, qb, c = units[i]
            if qb == 0 and c == 0 and h == 0:
                load_head(0)
            hb_ = h % 2
            bs = PB[i % 3]
            MM(bs.ap, KT[hb_][0:97, c * 128:(c + 1) * 128], QTh[hb_][0:97, qb * 512:(qb + 1) * 512], True, True,
               [KT[hb_], QTh[hb_]], [bs])
            ACT(PT[i % 4].ap, bs.ap, AF.Exp, [bs], [PT[i % 4]])
        ip = i - LA
        if ip >= 0:
            h, qb, c = units[ip]
            if qb == 0 and c == 0 and h + 1 < 8:
                load_head(h + 1)
            hb_ = h % 2
            blk = h * NQB + qb
            ob_ = PB[4 + blk % 2]
            MM(ob_[0:65, :], Vh[hb_][:, c, 0:65], PT[ip % 4].ap, c == 0, c == NK - 1, [Vh[hb_], PT[ip % 4]], [ob_])
            if c == NK - 1:
                pending.append((i + 6, epilogue(h, qb, blk)))
    for _, f in pending:
        f()

    if dbg:
        d_outb = dout("d_outb", [NQ, 512], F32)
        DMA("sp", d_outb, outb_s, [tk_outb], [Tok()])

    S.barrier()
    S.emit(nc, es, final=(stage == "AB"))
    esB.close()
    print("phase B ops", len(S.ops), "standalone waits", S.nwait)
    if stage == "AB":
        es.close()
        return nc

    NSB = 2 if NOWN >= 8 else 1
    SBT = NOWN // NSB
    NT = SBT * 128
    cmb_s = dscr("cmb_s", [NSB, 32, NT], F32)
    tk_cmb = Tok("cmb_s")
    tk_y = Tok("y")
    esC = ExitStack()
    C = lambda name, shape, dt: sb(name, shape, dt, esC)
    Gob = C("Gob", [128, 512], F32)
    G_at = C("G_at", [128, 1024], F32)
    B_at = C("B_at", [128, 1024], F32)
    G_ff = C("G_ff", [128, 1024], F32)
    B_ff = C("B_ff", [128, 1024], F32)
    brB = C("brB", [128, 36], F32)
    wr_f = C("wr_f", [128, 8, 36], F32)
    yacc = C("yacc", [128, SBT, 1024], F32)
    h2T = C("h2T", [128, 8, NT], BF16)
    cmbT = C("cmbT", [32, NT], F32)
    vload(Gob, "ob_g")
    vload(G_at, "ln_attn_g")
    vload(B_at, "ln_attn_b")
    vload(G_ff, "ln_ffn_g")
    vload(B_ff, "ln_ffn_b")
    vload(brB, "b_router")
    TSo("dve", Gob.ap, Gob.ap, math.sqrt(512.0), None, ALU.mult, None, [Gob], [Gob])
    DMA("sp", wr_f.ap, w_router.rearrange("(c p) n -> p c n", p=128), [], [wr_f])

    def layer_norm_tile(src, st_, Gt, Bt_, dst):
        OP("dve", "bn_stats", [src], [st_], st_[:, 0:6], src[:, 0:512])
        OP("dve", "bn_stats", [src], [st_], st_[:, 6:12], src[:, 512:1024])
        OP("dve", "bn_aggr", [st_], [st_], st_[:, 12:14], st_[:, 0:12])
        RSQ(st_[:, 14:15], st_[:, 13:14], LN_EPS, [st_], [st_])
        STT("dve", st_[:, 15:16], st_[:, 12:13], -1.0, st_[:, 14:15], ALU.mult, ALU.mult, [st_], [st_])
        ACT(src.ap, src.ap, AF.Identity, [src, st_], [src], bias=st_[:, 15:16], scale=st_[:, 14:15])
        TTo("pool", src.ap, src.ap, Gt.ap, ALU.mult, [src, Gt], [src])
        TTo("dve", dst.ap, src.ap, Bt_.ap, ALU.add, [src, Bt_], [dst])

    for sbi in range(NSB):
        es1 = ExitStack()
        mk1 = lambda name, shape, dt: sb("%s_s%d" % (name, sbi), shape, dt, es1)
        woutb = mk1("woutb", [128, 8, 1024], BF16)
        mix = [mk1("mix%d" % i, [128, 1024], BF16) for i in range(2)]
        obf = [mk1("obf%d" % i, [128, 512], F32) for i in range(6)]
        hin = [mk1("hin%d" % i, [128, 1024], F32) for i in range(2)]
        rr = [mk1("rr%d" % i, [128, 1024], F32) for i in range(2)]
        rn = [mk1("rn%d" % i, [128, 1024], F32) for i in range(2)]
        h2 = [mk1("h2_%d" % i, [128, 1024], F32) for i in range(2)]
        h2Tf = mk1("h2Tf", [128, 8, 128], F32)
        mixT = mk1("mixT", [128, 8, 128], BF16)
        sq1 = [mk1("sq1_%d" % i, [128, 512], F32) for i in range(2)]
        sta = [mk1("sta_%d" % i, [128, 4], F32) for i in range(4)]
        stb = [mk1("stb_%d" % i, [128, 16], F32) for i in range(2)]
        rt = [mk1("rt%d" % i, [128, 256], F32) for i in range(4)]
        wst1 = rr
        for c in range(8):
            DMA("sp", wst1[c % 2].ap, w_out[c * 128:(c + 1) * 128, :], [], [wst1[c % 2]])
            CP(("dve", "pool")[c % 2], woutb[:, c, :], wst1[c % 2].ap, [wst1[c % 2]], [woutb])

        def rows_(l):
            n = sbi * SBT + l
            return slice(n * 128, (n + 1) * 128)

        def c0(l):
            DMA("sp", obf[l % 6].ap, outb_s[rows_(l), :], [tk_outb], [obf[l % 6]])

        def c1(l):
            ACT(sq1[l % 2].ap, obf[l % 6].ap, AF.Square, [obf[l % 6]], [sq1[l % 2]])

        def c2(l):
            st_ = sta[l % 4]
            RED(st_[:, 0:1], sq1[l % 2].ap, ALU.add, [sq1[l % 2]], [st_])
            TSo("dve", st_[:, 1:2], st_[:, 0:1], 512.0 * RMS_EPS, None, ALU.add, None, [st_], [st_])

        def c3(l):
            st_ = sta[l % 4]
            ACT(st_[:, 1:2], st_[:, 1:2], AF.Ln, [st_], [st_])
            ACT(st_[:, 1:2], st_[:, 1:2], AF.Exp, [st_], [st_], scale=-0.5)
            DMA("sp", mix[l % 2][:, 0:512], mixa_s[rows_(l), :], [tk_mixa], [mix[l % 2]])

        def c4(l):
            mx = mix[l % 2]
            STT("dve", mx[:, 512:1024], obf[l % 6].ap, sta[l % 4][:, 1:2], Gob.ap, ALU.mult, ALU.mult,
                [obf[l % 6], sta[l % 4], Gob], [mx])
            DMA("pool", hin[l % 2].ap, h_s[rows_(l), :], [tk_h], [hin[l % 2]])

        def c5(l):
            mx = mix[l % 2]
            for c in range(8):
                TR(pbf(0)[:, c * 128:(c + 1) * 128], mx[:, c * 128:(c + 1) * 128], identb, [mx, cstb], [PB[0]])
            CP("act", mixT.ap, pbf(0).rearrange("p (c t) -> p c t", t=128), [PB[0]], [mixT])

        def c6(l):
            hi_, r_ = hin[l % 2], rr[l % 2]
            for half in range(2):
                for c in range(8):
                    MM(PB[1 + half].ap, mixT[:, c, :], woutb[:, c, half * 512:(half + 1) * 512], c == 0, c == 7,
                       [mixT, woutb], [PB[1 + half]])
            for half in range(2):
                cs = slice(half * 512, (half + 1) * 512)
                STT("dve", r_[:, cs], hi_[:, cs], ALPHA, PB[1 + half].ap, ALU.mult, ALU.add, [hi_, PB[1 + half]], [r_])

        def c7(l):
            r_, st_ = rr[l % 2], stb[l % 2]
            OP("dve", "bn_stats", [r_], [st_], st_[:, 0:6], r_[:, 0:512])
            OP("dve", "bn_stats", [r_], [st_], st_[:, 6:12], r_[:, 512:1024])
            OP("dve", "bn_aggr", [st_], [st_], st_[:, 12:14], st_[:, 0:12])
            TSo("dve", st_[:, 14:15], st_[:, 13:14], LN_EPS, None, ALU.add, None, [st_], [st_])

        def c8(l):
            r_, st_, rn_ = rr[l % 2], stb[l % 2], rn[l % 2]
            ACT(st_[:, 14:15], st_[:, 14:15], AF.Ln, [st_], [st_])
            ACT(st_[:, 14:15], st_[:, 14:15], AF.Exp, [st_], [st_], scale=-0.5)
            ACT(st_[:, 15:16], st_[:, 12:13], AF.Identity, [st_], [st_], scale=st_[:, 14:15])
            OP("act", "mul", [st_], [st_], st_[:, 15:16], st_[:, 15:16], -1.0)
            ACT(rn_.ap, r_.ap, AF.Identity, [r_, st_], [rn_], bias=st_[:, 15:16], scale=st_[:, 14:15])

        def c9(l):
            rn_ = rn[l % 2]
            TTo("pool", rn_.ap, rn_.ap, G_at.ap, ALU.mult, [rn_, G_at], [rn_])

        def c10(l):
            TTo("dve", h2[l % 2].ap, rn[l % 2].ap, B_at.ap, ALU.add, [rn[l % 2], B_at], [h2[l % 2]])

        def c11(l):
            h2_ = h2[l % 2]
            OP("act", "mul", [h2_], [yacc], yacc[:, l, :], h2_.ap, ALPHA)
            for c in range(8):
                TR(PB[3 + c // 4][:, (c % 4) * 128:(c % 4 + 1) * 128], h2_[:, c * 128:(c + 1) * 128], identf.ap,
                   [h2_, identf], [PB[3 + c // 4]])
            for hh in range(2):
                pv = PB[3 + hh].ap.rearrange("p (c t) -> p c t", t=128)
                CP("act", h2T[:, hh * 4:(hh + 1) * 4, l * 128:(l + 1) * 128], pv, [PB[3 + hh]], [h2T])
                CP("dve", h2Tf[:, hh * 4:(hh + 1) * 4, :], pv, [PB[3 + hh]], [h2Tf])

        def c12(l):
            rt_ = rt[l % 4]
            for c in range(8):
                MM(PB[5][:, 0:36], h2Tf[:, c, :], wr_f[:, c, :], c == 0, c == 7, [h2Tf, wr_f], [PB[5]])
            TTo("dve", rt_[:, 0:36], PB[5][:, 0:36], brB.ap, ALU.add, [PB[5], brB], [rt_])

        def c13(l):
            rt_ = rt[l % 4]
            RED(rt_[:, 40:41], rt_[:, 0:4], ALU.max, [rt_], [rt_])
            TSo("dve", rt_[:, 44:48], rt_[:, 0:4], rt_[:, 40:41], None, ALU.is_equal, None, [rt_], [rt_])
            TSo("dve", rt_[:, 48:52], rt_[:, 44:48], 1.0, 1e30, ALU.subtract, ALU.mult, [rt_], [rt_])
            elm = rt_[:, 64:96]
            TTo("dve", elm.rearrange("p (g j) -> p g j", j=8), rt_[:, 4:36].rearrange("p (g j) -> p g j", j=8),
                rt_[:, 48:52].unsqueeze(2).to_broadcast([128, 4, 8]), ALU.add, [rt_], [rt_])
            RED(rt_[:, 41:42], elm, ALU.max, [rt_], [rt_])
            TSo("dve", rt_[:, 96:128], elm, rt_[:, 41:42], None, ALU.is_equal, None, [rt_], [rt_])
            STT("dve", rt_[:, 128:160], rt_[:, 96:128], -1e30, elm, ALU.mult, ALU.add, [rt_], [rt_])
            RED(rt_[:, 42:43], rt_[:, 128:160], ALU.max, [rt_], [rt_])
            TSo("dve", rt_[:, 160:192], rt_[:, 128:160], rt_[:, 42:43], None, ALU.is_equal, None, [rt_], [rt_])
            TSo("dve", rt_[:, 43:44], rt_[:, 40:41], -1.0, None, ALU.mult, None, [rt_], [rt_])
            TTo("dve", rt_[:, 57:58], rt_[:, 42:43], rt_[:, 41:42], ALU.subtract, [rt_], [rt_])

        def c14(l):
            rt_ = rt[l % 4]
            ACT(rt_[:, 52:56], rt_[:, 0:4], AF.Exp, [rt_], [rt_], bias=rt_[:, 43:44], scale=1.0)
            ACT(rt_[:, 58:59], rt_[:, 57:58], AF.Exp, [rt_], [rt_])

        def c15(l):
            rt_ = rt[l % 4]
            RED(rt_[:, 56:57], rt_[:, 52:56], ALU.add, [rt_], [rt_])
            STT("dve", rt_[:, 59:60], rt_[:, 58:59], 1.0, rt_[:, 56:57], ALU.add, ALU.mult, [rt_], [rt_])
            OP("dve", "reciprocal", [rt_], [rt_], rt_[:, 60:61], rt_[:, 59:60])
            TTo("dve", rt_[:, 61:62], rt_[:, 60:61], rt_[:, 58:59], ALU.mult, [rt_], [rt_])
            TSo("dve", rt_[:, 192:224], rt_[:, 96:128], rt_[:, 60:61], None, ALU.mult, None, [rt_], [rt_])
            STT("dve", rt_[:, 192:224], rt_[:, 160:192], rt_[:, 61:62], rt_[:, 192:224], ALU.mult, ALU.add,
                [rt_], [rt_])
            TR(PB[6][0:32, 0:128], rt_[:, 192:224], identf.ap, [rt_, identf], [PB[6]])
            CP("dve", cmbT[:, l * 128:(l + 1) * 128], PB[6][0:32, 0:128], [PB[6]], [cmbT])

        cstages = [c0, c1, c2, c3, c4, c5, c6, c7, c8, c9, c10, c11, c12, c13, c14, c15]
        for step in range(SBT + len(cstages) - 1):
            for off in range(len(cstages) - 1, -1, -1):
                l = step - off
                if 0 <= l < SBT:
                    cstages[off](l)

        DMA("sp", cmb_s[sbi], cmbT.ap, [cmbT], [tk_cmb])
        if dbg and sbi == 0:
            d_h2T = dout("d_h2T", [128, 8 * NT], BF16)
            DMA("sp", d_h2T, h2T.ap.rearrange("p c n -> p (c n)"), [h2T], [Tok()])
            d_cmbT = dout("d_cmbT", [32, NT], F32)
            DMA("sp", d_cmbT, cmbT.ap, [cmbT], [Tok()])
        S.barrier()
        S.emit(nc, es)
        es1.close()
        es2 = ExitStack()
        mk2 = lambda name, shape, dt: sb("%s_s%d" % (name, sbi), shape, dt, es2)
        wstg = [mk2("wstg%d" % i, [128, 2048], F32) for i in range(3)]
        wg = [mk2("wg%d" % i, [128, 8, 256], BF16) for i in range(2)]
        wu = [mk2("wu%d" % i, [128, 8, 256], BF16) for i in range(2)]
        wd = [mk2("wd%d" % i, [128, 2, 1024], BF16) for i in range(2)]
        cB = [mk2("cB%d" % i, [128, NT], F32) for i in range(2)]
        sg = mk2("sg", [128, 1024], F32)
        t1 = mk2("t1", [128, 1024], F32)
        hid = [mk2("hid%d" % i, [128, 2, 512], BF16) for i in range(2)]
        NBLK = SBT // 4

        def load_expert(e):
            sl = e % 2
            DMA("sp", wstg[0].ap.rearrange("p (c f) -> p c f", f=256), w_gate[e].rearrange("(c p) f -> p c f", p=128),
                [], [wstg[0]])
            CP("act", wg[sl].ap.rearrange("p c f -> p (c f)"), wstg[0].ap, [wstg[0]], [wg[sl]])
            DMA("sp", wstg[1].ap.rearrange("p (c f) -> p c f", f=256), w_up[e].rearrange("(c p) f -> p c f", p=128),
                [], [wstg[1]])
            CP("act", wu[sl].ap.rearrange("p c f -> p (c f)"), wstg[1].ap, [wstg[1]], [wu[sl]])
            DMA("sp", wstg[2].ap.rearrange("p (c f) -> p c f", f=1024), w_down[e].rearrange("(c p) f -> p c f", p=128),
                [], [wstg[2]])
            CP("act", wd[sl].ap.rearrange("p c f -> p (c f)"), wstg[2].ap, [wstg[2]], [wd[sl]])
            DMA("sp", cB[sl].ap, cmb_s[sbi, e:e + 1, :].to_broadcast([128, NT]), [tk_cmb], [cB[sl]])

        eunits = [(e, b) for e in range(32) for b in range(NBLK)]
        dcount = [0]

        def down_pairs(e, b, ui):
            sl = e % 2
            hd = hid[ui % 2]
            outl = []
            for i in range(4):
                for half in range(2):
                    def f(i=i, half=half):
                        bank = PB[4 + dcount[0] % 4]
                        dcount[0] += 1
                        for ff in range(2):
                            MM(bank.ap, hd[:, ff, i * 128:(i + 1) * 128], wd[sl][:, ff, half * 512:(half + 1) * 512],
                               ff == 0, ff == 1, [hd, wd[sl]], [bank])
                        ya = yacc[:, b * 4 + i, half * 512:(half + 1) * 512]
                        TTo("dve", ya, ya, bank.ap, ALU.add, [yacc, bank], [yacc])
                    outl.append(f)
            return outl

        def gu(e, b, ui, inter):
            sl = e % 2
            T0 = b * 512
            nmm = 0
            for which, wt in ((0, wg), (1, wu)):
                for f in range(2):
                    bank = PB[which * 2 + f]
                    for c in range(8):
                        MM(bank.ap, wt[sl][:, c, f * 128:(f + 1) * 128], h2T[:, c, T0:T0 + 512], c == 0, c == 7,
                           [wt[sl], h2T], [bank])
                        nmm += 1
                        if nmm > 16 and nmm % 2 == 0 and inter:
                            inter.pop(0)()
            hd = hid[ui % 2]
            for f in range(2):
                fs = slice(f * 512, (f + 1) * 512)
                ACT(sg[:, fs], PB[f].ap, AF.Silu, [PB[f]], [sg])
                TTo("dve", t1[:, fs], sg[:, fs], PB[2 + f].ap, ALU.mult, [sg, PB[2 + f]], [t1])
                TTo("pool", hd[:, f, :], t1[:, fs], cB[sl][:, T0:T0 + 512], ALU.mult, [t1, cB[sl]], [hd])

        for ui in range(len(eunits) + 1):
            inter = []
            if ui >= 1:
                e, b = eunits[ui - 1]
                inter = down_pairs(e, b, ui - 1)
            if ui < len(eunits):
                e, b = eunits[ui]
                if b == 0 and e == 0:
                    load_expert(0)
                gu(e, b, ui, inter)
            for f in inter:
                f()
            if ui < len(eunits):
                e, b = eunits[ui]
                if b == 0 and e + 1 < 32:
                    load_expert(e + 1)
        S.barrier()
        S.emit(nc, es)
        es2.close()
        es3 = ExitStack()
        yo = [sb("yo%d_s%d" % (i, sbi), [128, 1024], F32, es3) for i in range(2)]
        yi = [sb("yi%d_s%d" % (i, sbi), [128, 1024], F32, es3) for i in range(2)]
        st3 = [sb("st3_%d_s%d" % (i, sbi), [128, 16], F32, es3) for i in range(2)]
        for l in range(SBT):
            n = sbi * SBT + l
            s = l % 2
            CP("act", yi[s].ap, yacc[:, l, :], [yacc], [yi[s]])
            layer_norm_tile(yi[s], st3[s], G_ff, B_ff, yo[s])
            DMA("sp", y_out[n * 128:(n + 1) * 128, :], yo[s].ap, [yo[s]], [tk_y])
        S.barrier()
        S.emit(nc, es, final=(sbi == NSB - 1))
        es3.close()
    esC.close()
    es.close()
    print("total ops", len(S.ops), "standalone waits", S.nwait)
    return nc

COMPUTE = ("pe", "act", "dve", "pool")
KD = 8
EPOCH = 12000


class Tok:
    __slots__ = ("n", "lw", "rd", "rdma", "x")

    def __init__(s, n="", x=False):
        s.n = n
        s.x = x
        s.lw = -1
        s.rd = {}
        s.rdma = []


class Sched:
    def __init__(s):
        s.ops = []
        s.lastc = {}
        s.bar = None
        s.bar_idx = 0
        s.bar_done = set()
        s.dmas_since_bar = []
        s.emitted = 0
        s.dq = {}
        s.semof = {}
        s.pos = {}
        s.cpos = {e: 0 for e in COMPUTE}
        s.cnt = {e: 0 for e in COMPUTE}
        s.seen = {}
        s.sems = {}
        s.final = {}
        s.nwait = 0

    def add(s, eng, fn, reads=(), writes=(), dma=False):
        i = len(s.ops)
        deps = set()
        xr = [t for t in reads if t.x]
        if xr:
            reads = [t for t in reads if not t.x]
            writes = list(writes) + [t for t in xr if t not in writes]
        for t in reads:
            if t.lw >= 0:
                deps.add(t.lw)
        for t in writes:
            if t.lw >= 0:
                deps.add(t.lw)
            deps.update(t.rd.values())
            deps.update(t.rdma)
        bi = s.bar_idx
        deps = set(d for d in deps if d >= bi)
        if s.bar is not None and eng not in s.bar_done:
            deps.update(s.bar)
            s.bar_done.add(eng)
        s.ops.append([eng, fn, deps, dma])
        for t in reads:
            if dma:
                t.rdma.append(i)
            else:
                t.rd[eng] = i
        for t in writes:
            t.lw = i
            t.rd = {}
            t.rdma = []
        if dma:
            s.dmas_since_bar.append(i)
        else:
            s.lastc[eng] = i
        return i

    def barrier(s):
        b = set(s.dmas_since_bar)
        b.update(s.lastc.values())
        if s.bar is not None and len(s.bar_done) < 5:
            b.update(s.bar)
        s.bar = b
        s.bar_idx = len(s.ops)
        s.bar_done = set()
        s.dmas_since_bar = []

    def emit(s, nc, stack, final=False):
        ops = s.ops
        lo, n = s.emitted, len(ops)
        semof, pos = s.semof, s.pos
        for i in range(lo, n):
            eng, fn, deps, dma = ops[i]
            if dma:
                lst = s.dq.setdefault(eng, [])
                k = len(lst)
                if k >= KD:
                    deps.add(lst[k - KD])
                lst.append(i)
                semof[i] = (("d", eng, k % KD), 16 * (k // KD + 1))
            else:
                pos[i] = s.cpos[eng]
                s.cpos[eng] += 1
        hasdep = set()
        for i in range(lo, n):
            hasdep.update(ops[i][2])
        hasdep.update(s.lastc.values())
        for i in range(lo, n):
            eng, fn, deps, dma = ops[i]
            if not dma and i in hasdep:
                c = s.cnt[eng]
                s.cnt[eng] = c + 1
                semof[i] = (("c", eng, c // EPOCH), c % EPOCH + 1)
        waits = {}
        for i in range(lo, n):
            eng, fn, deps, dma = ops[i]
            need = {}
            for d in deps:
                de, _, _, ddma = ops[d]
                if not ddma and de == eng and not dma:
                    if eng == "pe":
                        continue
                    if pos[i] - pos[d] > 3:
                        continue
                key, val = semof[d]
                if need.get(key, 0) < val:
                    need[key] = val
            sn = s.seen.setdefault(eng, {})
            w = []
            for key, val in need.items():
                if sn.get(key, 0) >= val:
                    continue
                sn[key] = val
                w.append((key, val))
            waits[i] = w
        sems = s.sems
        for i in range(lo, n):
            x = semof.get(i)
            if x is None:
                continue
            if x[0] not in sems:
                sems[x[0]] = stack.enter_context(nc.semaphore("s_%s_%s_%d" % x[0]))
            if x[0][0] == "d" and s.final.get(x[0], 0) < x[1]:
                s.final[x[0]] = x[1]
        with nc.Block() as block:
            def run(engname, eobj, tail=False):
                for i in range(lo, n):
                    o = ops[i]
                    if o[0] != engname:
                        continue
                    w = waits[i]
                    for key, val in w[:-1]:
                        eobj.wait_ge(sems[key], val)
                        s.nwait += 1
                    ins = o[1](eobj)
                    if w:
                        ins._wait_ge(sems[w[-1][0]], w[-1][1])
                    x = semof.get(i)
                    if x is not None:
                        ins.then_inc(sems[x[0]], 16 if x[0][0] == "d" else 1)
                    o[1] = None
                if tail:
                    for key, val in s.final.items():
                        eobj.wait_ge(sems[key], val)

            @block.sync
            def _(e):
                run("sp", e, tail=final)

            @block.scalar
            def _(e):
                run("act", e)

            @block.vector
            def _(e):
                run("dve", e)

            @block.gpsimd
            def _(e):
                run("pool", e)

            @block.tensor
            def _(e):
                run("pe", e)
        s.emitted = n


N_CORES = 8
NOWN_FULL = 32
NOTH_FULL = 96
_NC_CACHE = {}


def _host_prep(inputs, c):
    bf = ml_dtypes.bfloat16
    NOWN, NOTH = NOWN_FULL, NOTH_FULL
    seq_tiles = NOWN + NOTH
    b = c // 4
    qd = c % 4
    first = qd == 0
    last = qd == 3
    own0 = qd * NOWN
    x = inputs["x"][b]
    pos = inputs["positions"][b]
    own = list(range(own0, own0 + NOWN))
    others = [t for t in range(seq_tiles) if t not in own]
    P = own0 - 1 if not first else own0
    N = own0 + NOWN if not last else own0 + NOWN - 1
    order = [P] + own + [N] + others
    xr = np.concatenate([x[t * 128:(t + 1) * 128] for t in order], 0)
    posr = np.ascontiguousarray(np.stack([pos[t * 128:(t + 1) * 128] for t in order], 1).astype(np.int32))
    jj = np.arange(128)[:, None]
    ii = np.arange(128)[None, :]
    mP = np.where(jj >= ii, 0.0, NEGB).astype(np.float32)
    mN = np.where(jj <= ii, 0.0, NEGB).astype(np.float32)
    full = np.full((128, 128), NEGB, np.float32)
    ms = [mP, mN, full if first else mP, full if last else mN]
    cst_b = np.concatenate([np.eye(128, dtype=np.float32)] + [np.tile(m, (1, 4)) for m in ms], 1).astype(bf)
    return dict(xr=xr, posr=posr, cst_b=cst_b)


def kernel(**inputs):
    inputs = {k: np.asarray(v) for k, v in inputs.items()}
    g = lambda k: np.ascontiguousarray(inputs[k], dtype=np.float32)
    invA = (1.0 / (np.float32(10000.0) ** (np.arange(0, 64, 2, dtype=np.float32) / np.float32(64)))).astype(np.float32)
    invB = (1.0 / (np.float32(10000.0) ** (np.arange(0, 32, 2, dtype=np.float32) / np.float32(32)))).astype(np.float32)
    parts = dict(ln_emb_g=g("ln_emb_g"), ln_emb_b=g("ln_emb_b"), q_g=g("q_a_norm_g")[0], kv_g=g("kv_a_norm_g")[0],
                 oa_g=g("out_norm_a_g")[0], ob_g=g("out_norm_b_g")[0], ln_attn_g=g("ln_attn_g")[0],
                 ln_attn_b=g("ln_attn_b")[0], ln_ffn_g=g("ln_ffn_g")[0], ln_ffn_b=g("ln_ffn_b")[0], sink=g("a_sink")[0],
                 b_router=np.concatenate([g("b_group")[0], g("b_expert")[0]]), invA=invA, invB=invB)
    vecs = np.zeros((1, NVEC), np.float32)
    for k, (o, l) in VOFF.items():
        vecs[0, o:o + l] = parts[k]
    shared = dict(w_in=g("w_in")[0], w_q_b=g("w_q_b")[0], w_kv_b=g("w_kv_b")[0], w_out=g("w_out")[0],
                  w_router=np.ascontiguousarray(np.concatenate([g("w_group")[0], g("w_expert")[0]], 1)),
                  w_gate=g("w_gate")[0], w_up=g("w_up")[0], w_down=g("w_down")[0], vecs=vecs,
                  cst_f=np.eye(128, dtype=np.float32))
    in_maps = []
    for c in range(N_CORES):
        m = dict(shared)
        m.update(_host_prep(inputs, c))
        in_maps.append(m)
    if "nc" not in _NC_CACHE:
        _NC_CACHE["nc"] = build(NOWN_FULL, NOTH_FULL, stage="ABC", dbg=False)
    nc = _NC_CACHE["nc"]
    res = run_bass_kernel_spmd(nc, in_maps, core_ids=list(range(N_CORES)))
    B, Sq, D = inputs["x"].shape
    out = np.zeros((B, Sq, D), np.float32)
    for c in range(N_CORES):
        b = c // 4
        r0 = (c % 4) * NOWN_FULL * 128
        out[b, r0:r0 + NOWN_FULL * 128] = np.asarray(res.results[c]["y"], dtype=np.float32)
    return out
```

```python
import math
import numpy as np
import ml_dtypes
from contextlib import ExitStack
import concourse.bass as bass
import concourse.mybir as mybir
from concourse.bass_utils import run_bass_kernel_spmd

F32 = mybir.dt.float32
BF16 = mybir.dt.bfloat16
I32 = mybir.dt.int32
AF = mybir.ActivationFunctionType
ALU = mybir.AluOpType
AX = mybir.AxisListType

LN_EPS = 1e-5
RMS_EPS = 1e-6
ALPHA = 2.0 ** 0.25
NEGB = -30000.0
TWO_PI = 2.0 * math.pi
C1 = 6.28125
C2 = float(np.float32(TWO_PI - C1))
C3 = float(TWO_PI - C1 - float(np.float32(TWO_PI - C1)))
MAGIC = 12582912.0
PI_LIM = 3.1415925

VOFF = {}
_o = 0
for _n, _l in [("ln_emb_g", 1024), ("ln_emb_b", 1024), ("q_g", 768), ("kv_g", 256), ("oa_g", 512),
               ("ob_g", 512), ("ln_attn_g", 1024), ("ln_attn_b", 1024), ("ln_ffn_g", 1024),
               ("ln_ffn_b", 1024), ("sink", 8), ("b_router", 36), ("invA", 32), ("invB", 16)]:
    VOFF[_n] = (_o, _l)
    _o += _l
NVEC = _o


class TT:
    def __init__(s, h, name):
        s.h = h
        s.t = Tok(name)

    def __getitem__(s, k):
        return s.h[k]

    @property
    def ap(s):
        return s.h[:]


def build(NOWN, NOTH, stage="ABC", dbg=False):
    NJ = NOWN + NOTH + 2
    NK = NOWN + NOTH
    NW = NOWN + 2
    NQ = NOWN * 128
    nc = bass.Bass("TRN2", target_bir_lowering=False)
    S = Sched()
    es = ExitStack()

    def din(name, shape, dt=F32):
        return nc.dram_tensor(name, list(shape), dt, kind="ExternalInput").ap()

    def dscr(name, shape, dt):
        return nc.dram_tensor(name, list(shape), dt, kind="Internal").ap()

    def dout(name, shape, dt=F32):
        return nc.dram_tensor(name, list(shape), dt, kind="ExternalOutput").ap()

    xr = din("xr", [NJ * 128, 1024])
    posr = din("posr", [128, NJ], I32)
    w_in = din("w_in", [1024, 1824])
    w_q_b = din("w_q_b", [768, 768])
    w_kv_b = din("w_kv_b", [256, 1024])
    w_out = din("w_out", [1024, 1024])
    w_router = din("w_router", [1024, 36])
    w_gate = din("w_gate", [32, 1024, 256])
    w_up = din("w_up", [32, 1024, 256])
    w_down = din("w_down", [32, 256, 1024])
    vecs = din("vecs", [1, NVEC])
    cst_f = din("cst_f", [128, 128])
    cst_b = din("cst_b", [128, 128 + 4 * 512], BF16)
    y_out = dout("y", [NQ, 1024])

    KnT_s = dscr("KnT_s", [512, NK * 128], BF16)
    KrT_s = dscr("KrT_s", [32, NK * 128], BF16)
    V_s = dscr("V_s", [8, 128, NK * 72], BF16)
    h_s = dscr("h_s", [NQ, 1024], F32)
    mixa_s = dscr("mixa_s", [NQ, 512], BF16)
    outb_s = dscr("outb_s", [NQ, 512], F32)
    QT_s = dscr("QT_s", [97, 8, NQ], BF16)
    tk_QT = Tok("QT_s")
    tk_h = Tok("h_s")
    tk_mixa = Tok("mixa_s")
    tk_outb = Tok("outb_s")
    tk_KnT = Tok("KnT_s")
    tk_KrT = Tok("KrT_s")
    tk_V = Tok("V_s")

    dbg_outs = {}

    def sb(name, shape, dt, stack=None):
        h = (stack or es).enter_context(nc.sbuf_tensor(name, list(shape), dt))
        return TT(h, name)

    def OP(eng, meth, R, W, *a, **kw):
        return S.add(eng, lambda e: getattr(e, meth)(*a, **kw), [x.t if isinstance(x, TT) else x for x in R],
                     [x.t if isinstance(x, TT) else x for x in W])

    def DMA(q, out, in_, R, W):
        return S.add(q, lambda e: e.dma_start(out=out, in_=in_), [x.t if isinstance(x, TT) else x for x in R],
                     [x.t if isinstance(x, TT) else x for x in W], dma=True)

    def MM(out, lhsT, rhs, start, stop, R, W):
        return OP("pe", "matmul", R, W, out, lhsT, rhs, start=start, stop=stop)

    def TR(out, in_, ident, R, W):
        return OP("pe", "transpose", R, W, out, in_, ident)

    def ACT(out, in_, func, R, W, **kw):
        return OP("act", "activation", R, W, out, in_, func, **kw)

    def TTo(eng, out, in0, in1, op, R, W):
        return OP(eng, "tensor_tensor", R, W, out, in0, in1, op)

    def TSo(eng, out, in0, s1, s2, op0, op1, R, W):
        if op1 is None:
            return OP(eng, "tensor_scalar", R, W, out, in0, s1, None, op0)
        return OP(eng, "tensor_scalar", R, W, out, in0, s1, s2, op0, op1)

    def STT(eng, out, in0, sc, in1, op0, op1, R, W):
        return OP(eng, "scalar_tensor_tensor", R, W, out, in0, sc, in1, op0, op1)

    def CP(eng, out, in_, R, W):
        if eng == "act":
            return OP("act", "copy", R, W, out, in_)
        return OP(eng, "tensor_copy", R, W, out, in_)

    EPSC = {}

    def RSQ(dst, src, addc, R, W):
        col = EPSC[addc]
        ACT(dst, src, AF.Ln, list(R) + [epst], W, bias=epst[:, col:col + 1])
        ACT(dst, dst, AF.Exp, W, W, scale=-0.5)

    def RED(out, in_, op, R, W, axis=AX.X):
        return OP("dve", "tensor_reduce", R, W, out, in_, axis, op)

    PB = []
    PP = []
    for b in range(4):
        h = es.enter_context(nc.psum_tensor("pp%d" % b, [128, 1024], F32))
        PP.append(h)
        for half in range(2):
            PB.append(TT(h[:, half * 512:(half + 1) * 512], "pb%d" % (2 * b + half)))
            PB[-1].t.x = True

    def pbf(b):
        return PB[b].ap.bitcast(BF16)

    identf = sb("identf", [128, 128], F32)
    cstb = sb("cstb", [128, 128 + 2048], BF16)
    onesf = sb("onesf", [128, 128], F32)
    DMA("sp", identf.ap, cst_f, [], [identf])
    DMA("sp", cstb.ap, cst_b, [], [cstb])
    OP("pool", "memset", [], [onesf], onesf.ap, 1.0)
    identb = cstb[:, 0:128]
    epst = sb("epst", [128, 4], F32)
    for ci, ev in enumerate([LN_EPS, 256.0 * RMS_EPS, 768.0 * RMS_EPS, 512.0 * RMS_EPS]):
        EPSC[ev] = ci
        OP("pool", "memset", [], [epst], epst[:, ci:ci + 1], ev)

    def maskap(i):
        return cstb[:, 128 + i * 512:128 + (i + 1) * 512]

    def vload(dst, name, eng="sp"):
        o, l = VOFF[name]
        DMA(eng, dst.ap, vecs[0:1, o:o + l].to_broadcast([128, l]), [], [dst])

    esT = ExitStack()
    qn2b = sb("qn2b", [128, NOWN, 8], F32, esT)
    kmax2 = sb("kmax2", [128, 8], F32, esT)

    esA = ExitStack()
    A = lambda name, shape, dt: sb(name, shape, dt, esA)
    winb = A("winb", [128, 8, 1824], BF16)
    wqb = A("wqb", [128, 6, 768], BF16)
    wkvb = A("wkvb", [128, 2, 1024], BF16)
    wkb = A("wkb", [128, 2, 512], BF16)
    G_emb = A("G_emb", [128, 1024], F32)
    B_emb = A("B_emb", [128, 1024], F32)
    Gq = A("Gq", [128, 768], F32)
    Gkv = A("Gkv", [128, 256], F32)
    Ga = A("Ga", [128, 512], F32)
    sinkB = A("sinkB", [128, 8], F32)
    invA = A("invA", [128, 32], F32)
    invB = A("invB", [128, 16], F32)
    posi = A("posi", [128, NJ], I32)
    posf = A("posf", [128, NJ], F32)
    tabB = A("tabB", [128, NJ, 2, 16], F32)
    tabA = A("tabA", [128, NW, 2, 32], F32)
    esP = ExitStack()
    tabk = sb("tabk", [128, max(NJ * 32, NW * 64)], F32, esP)
    stg = [sb("stg%d" % i, [128, 1824], F32, esP) for i in range(2)]

    vload(G_emb, "ln_emb_g")
    vload(B_emb, "ln_emb_b")
    vload(Gq, "q_g")
    vload(Gkv, "kv_g")
    vload(Ga, "oa_g")
    vload(sinkB, "sink")
    vload(invA, "invA")
    vload(invB, "invB")
    DMA("sp", posi.ap, posr, [], [posi])
    TSo("dve", Gq.ap, Gq.ap, math.sqrt(768.0), None, ALU.mult, None, [Gq], [Gq])
    TSo("dve", Gkv.ap, Gkv.ap, 16.0, None, ALU.mult, None, [Gkv], [Gkv])
    TSo("dve", Ga.ap, Ga.ap, math.sqrt(512.0), None, ALU.mult, None, [Ga], [Ga])
    CP("dve", posf.ap, posi.ap, [posi], [posf])

    def make_table(tab, n, half, inv):
        tv = tab.ap
        kv = tabk[:, 0:n * 2 * half].rearrange("p (n t f) -> p n t f", t=2, f=half)
        pb_ = posf[:, 0:n].unsqueeze(2).to_broadcast([128, n, half])
        ib_ = inv.ap.unsqueeze(1).to_broadcast([128, n, half])
        TTo("dve", tv[:, :, 0, :], pb_, ib_, ALU.mult, [posf, inv], [tab])
        TSo("dve", tv[:, :, 1, :], tv[:, :, 0, :], math.pi / 2, None, ALU.add, None, [tab], [tab])
        TSo("dve", kv, tv, 1.0 / TWO_PI, MAGIC, ALU.mult, ALU.add, [tab], [tabk])
        TSo("dve", kv, kv, -MAGIC, None, ALU.add, None, [tabk], [tabk])
        STT("dve", tv, kv, -C1, tv, ALU.mult, ALU.add, [tabk, tab], [tab])
        STT("dve", tv, kv, -C2, tv, ALU.mult, ALU.add, [tabk, tab], [tab])
        STT("dve", tv, kv, -C3, tv, ALU.mult, ALU.add, [tabk, tab], [tab])
        TSo("dve", tv, tv, PI_LIM, -PI_LIM, ALU.min, ALU.max, [tab], [tab])
        ACT(tv, tv, AF.Sin, [tab], [tab])

    import os
    SKIP = os.environ.get("SKIP", "").split(",")
    if "tables" not in SKIP:
        make_table(tabB, NJ, 16, invB)
        make_table(tabA, NW, 32, invA)

    ncast = [0]

    def cast(out, in_, R, W, scale=None):
        eng = ("dve", "pool")[ncast[0] % 2]
        ncast[0] += 1
        if scale is None:
            CP(eng, out, in_, R, W)
        else:
            TSo(eng, out, in_, scale, None, ALU.mult, None, R, W)

    si = 0
    for c in range(8 if "weights" not in SKIP else 0):
        st_ = stg[si % 2]
        si += 1
        DMA("sp", st_[:, 0:1824], w_in[c * 128:(c + 1) * 128, :], [], [st_])
        cast(winb[:, c, 0:512], st_[:, 0:512], [st_], [winb], scale=0.125)
        cast(winb[:, c, 512:1824], st_[:, 512:1824], [st_], [winb])
    for c in range(6 if "weights" not in SKIP else 0):
        st_ = stg[si % 2]
        si += 1
        DMA("sp", st_[:, 0:768], w_q_b[c * 128:(c + 1) * 128, :], [], [st_])
        cast(wqb[:, c, :], st_[:, 0:768], [st_], [wqb], scale=96.0 ** -0.5)
    for c in range(2 if "weights" not in SKIP else 0):
        st_ = stg[si % 2]
        si += 1
        DMA("sp", st_[:, 0:1024], w_kv_b[c * 128:(c + 1) * 128, :], [], [st_])
        cast(wkvb[:, c, :], st_[:, 0:1024], [st_], [wkvb])
        cast(wkb[:, c, :].rearrange("p (h d) -> p h d", d=64),
             st_[:, 0:1024].rearrange("p (h x) -> p h x", x=128)[:, :, 0:64], [st_], [wkb])

    STOPAT = os.environ.get("STOPAT", "")
    if STOPAT == "P":
        d_tabB = dout("d_tabB", [128, NJ * 32])
        DMA("sp", d_tabB, tabB.ap.rearrange("p n t f -> p (n t f)"), [tabB], [Tok()])
        S.barrier()
        S.emit(nc, es, final=True)
        return nc
    S.barrier()
    S.emit(nc, es)
    esP.close()
    xt = [A("xt%d" % i, [128, 1024], F32) for i in range(2)]
    hf = [A("hf%d" % i, [128, 1024], F32) for i in range(2)]
    hb = [A("hb%d" % i, [128, 1024], BF16) for i in range(2)]
    hT = [A("hT%d" % i, [128, 8, 128], BF16) for i in range(2)]
    stt = [A("stt%d" % i, [128, 16], F32) for i in range(2)]
    kvb = [A("kvb%d" % i, [128, 288], BF16) for i in range(2)]
    sml = [A("sml%d" % i, [128, 32], F32) for i in range(2)]
    latT = [A("latT%d" % i, [128, 2, 128], BF16) for i in range(2)]
    KTbuf = [A("KTbuf%d" % i, [128, 4, 512], BF16) for i in range(2)]
    KrTbuf = [A("KrTbuf%d" % i, [32, 512], BF16) for i in range(2)]
    Vbuf = [A("Vbuf%d" % i, [128, 8, 4, 72], BF16) for i in range(2)]
    kn2 = A("kn2", [128, 8], F32)
    cqn = A("cqn", [128, 768], BF16)
    cqT = A("cqT", [128, 6, 128], BF16)
    qtok = A("qtok", [128, 8, 96], BF16)
    qst = [A("qst%d" % i, [96, 8, 128], BF16) for i in range(2)]
    KaT = A("KaT", [65, 2, 8 * 128], BF16)
    Va = A("Va", [128, 8, 2, 72], BF16)
    kmaxa = A("kmaxa", [128, NW, 2], F32)
    katok = A("katok", [128, 128], BF16)
    qatok = [A("qatok%d" % i, [128, 8, 65], BF16) for i in range(6)]
    qn2a = [A("qn2a%d" % i, [128, 8], F32) for i in range(6)]
    PTa = [A("PTa%d" % i, [128, 512], BF16) for i in range(2)]
    Osb = A("Osb", [65, 512], F32)
    Osb_default = Osb
    mixa = [A("mixa%d" % i, [128, 512], BF16) for i in range(2)]
    wsm = A("wsm", [128, 64], F32)
    wsm_default = wsm

    for v in Vbuf:
        OP("pool", "memset", [], [v], v.ap, 0.0)
        OP("pool", "memset", [], [v], v[:, :, :, 64:65], 1.0)
    OP("pool", "memset", [], [Va], Va.ap, 0.0)
    OP("pool", "memset", [], [Va], Va[:, :, :, 64:65], 1.0)
    OP("pool", "memset", [], [KaT], KaT[64:65, :, :], 1.0)
    OP("pool", "memset", [], [kmax2], kmax2.ap, 0.0)

    def rope(src, H, half, cos, sin, dst, R, W, tmps=None, scl=None):
        n = H * 2 * half
        tA_, tB_ = tmps
        tA = tA_[:, 0:n].rearrange("p (h t f) -> p h t f", t=2, f=half)
        tB = tB_[:, 0:n].rearrange("p (h t f) -> p h t f", t=2, f=half)
        cb = cos.unsqueeze(1).unsqueeze(1).to_broadcast([128, H, 2, half])
        sb_ = sin.unsqueeze(1).to_broadcast([128, H, half])
        if scl is None:
            TTo("dve", tA, src, cb, ALU.mult, R, [tA_])
            TTo("dve", tB[:, :, 0, :], src[:, :, 1, :], sb_, ALU.mult, R, [tB_])
            TTo("dve", tB[:, :, 1, :], src[:, :, 0, :], sb_, ALU.mult, R, [tB_])
        else:
            cb3 = cos.unsqueeze(1).to_broadcast([128, H, half])
            STT("dve", tA[:, :, 0, :], src[:, :, 0, :], scl, cb3, ALU.mult, ALU.mult, R, [tA_])
            STT("dve", tA[:, :, 1, :], src[:, :, 1, :], scl, cb3, ALU.mult, ALU.mult, R, [tA_])
            STT("dve", tB[:, :, 0, :], src[:, :, 1, :], scl, sb_, ALU.mult, ALU.mult, R, [tB_])
            STT("dve", tB[:, :, 1, :], src[:, :, 0, :], scl, sb_, ALU.mult, ALU.mult, R, [tB_])
        TTo("dve", dst[:, :, 0, :], tA[:, :, 0, :], tB[:, :, 0, :], ALU.subtract, [tA_, tB_], W)
        TTo("dve", dst[:, :, 1, :], tA[:, :, 1, :], tB[:, :, 1, :], ALU.add, [tA_, tB_], W)

    def pmax_bcast(src_ap, src_tok, ncol, dst_ap, dst_tok, bank, wsm=None):
        if wsm is None:
            wsm = wsm_default
        pb_ = PB[bank]
        TR(pb_[0:ncol, 0:128], src_ap, identf.ap, [src_tok, identf], [pb_])
        RED(wsm[0:ncol, 0:1], pb_[0:ncol, 0:128], ALU.max, [pb_], [wsm])
        TSo("dve", wsm[0:ncol, 8:8 + ncol], identf[0:ncol, 0:ncol], wsm[0:ncol, 0:1], None, ALU.mult, None,
            [wsm, identf], [wsm])
        MM(pb_[:, 256:256 + ncol], onesf[0:ncol, :], wsm[0:ncol, 8:8 + ncol], True, True, [onesf, wsm], [pb_])
        CP("dve", dst_ap, pb_[:, 256:256 + ncol], [pb_], [dst_tok])

    evi = [0]

    def evac(out, in_, R, W):
        eng = ("act", "dve")[evi[0] % 2]
        evi[0] += 1
        CP(eng, out, in_, R, W)

    wsm9 = [A("wsm9_%d" % i, [128, 64], F32) for i in range(2)]
    QaT2 = [A("QaT2_%d" % i, [65, 1024], BF16) for i in range(2)]
    oa2 = [A("oa2_%d" % i, [128, 8, 64], F32) for i in range(2)]
    sq9 = A("sq9", [128, 512], F32)
    for w9 in wsm9:
        OP("pool", "memset", [], [w9], w9.ap, 0.0)

    def wa_a(n):
        w = n + 1
        sq_ = n % 6
        qa_ = qatok[sq_]
        wsm = wsm9[n % 2]
        TTo("dve", wsm[:, 16:18], kmaxa[:, w - 1, :], kmaxa[:, w, :], ALU.max, [kmaxa], [wsm])
        TTo("dve", wsm[:, 16:18], wsm[:, 16:18], kmaxa[:, w + 1, :], ALU.max, [kmaxa, wsm], [wsm])
        TTo("dve", wsm[:, 20:28].rearrange("p (g f) -> p g f", f=4),
            qn2a[sq_].ap.rearrange("p (g f) -> p g f", f=4),
            wsm[:, 16:18].unsqueeze(2).to_broadcast([128, 2, 4]), ALU.mult, [qn2a[sq_], wsm], [wsm])
        ACT(wsm[:, 20:28], wsm[:, 20:28], AF.Ln, [wsm], [wsm])
        ACT(wsm[:, 20:28], wsm[:, 20:28], AF.Exp, [wsm], [wsm], scale=0.5)
        TSo("dve", qa_[:, :, 64:65], wsm[:, 20:28].unsqueeze(2), -1.0, None, ALU.mult, None, [wsm], [qa_])
        TTo("dve", wsm[:, 28:36].unsqueeze(2), qa_[:, :, 64:65], sinkB.ap.unsqueeze(2), ALU.add, [qa_, sinkB], [wsm])
        ACT(wsm[:, 36:44], wsm[:, 28:36], AF.Exp, [wsm], [wsm])
        for h in range(8):
            TR(pbf(0)[0:65, h * 128:(h + 1) * 128], qa_[:, h, 0:65], identb, [qa_, cstb], [PB[0]])
        evac(QaT2[n % 2][:, :], pbf(0)[0:65, :], [PB[0]], [QaT2[n % 2]])

    PTa2 = [A("PTa2_%d" % i, [128, 512], BF16) for i in range(2)]
    Osb2 = A("Osb2", [65, 512], F32)

    def capture(fn):
        rec = []
        orig = S.add
        S.add = lambda *a, **kw: rec.append((a, kw))
        try:
            fn()
        finally:
            S.add = orig
        return rec

    def interleave2(fa, fb):
        ra = capture(fa) if fa is not None else []
        rb = capture(fb) if fb is not None else []
        i = 0
        while i < len(ra) or i < len(rb):
            if i < len(ra):
                S.add(*ra[i][0], **ra[i][1])
            if i < len(rb):
                S.add(*rb[i][0], **rb[i][1])
            i += 1

    def wa_g(n, g, bset=0):
        w = n + 1
        wsm = wsm9[n % 2]
        QaT = QaT2[n % 2]
        oa = oa2[n % 2]
        first_tile = (n == 0)
        last_tile = (n == NOWN - 1)
        if bset == 0:
            bo, bsl, bt, PTx, Osb = PB[5], (PB[6], PB[7]), PB[4], PTa, Osb_default
        else:
            bo, bsl, bt, PTx, Osb = PB[1], (PB[2], PB[3]), PB[0], PTa2, Osb2
        kts = [w - 1, w, w + 1]
        for ki, kt in enumerate(kts):
            bs = bsl[ki % 2]
            pt = PTx[ki % 2]
            MM(bs.ap, KaT[0:65, g, (kt % 8) * 128:(kt % 8 + 1) * 128], QaT[:, g * 512:(g + 1) * 512], True, kt == w,
               [KaT, QaT], [bs])
            if kt != w:
                if kt < w:
                    mi = 2 if first_tile else 0
                else:
                    mi = 3 if last_tile else 1
                MM(bs.ap, identb, maskap(mi), False, True, [cstb], [bs])
            ACT(pt.ap, bs.ap, AF.Exp, [bs], [pt])
            MM(bo[0:65, :], Va[:, kt % 8, g, 0:65], pt.ap, ki == 0, ki == 2, [Va, pt], [bo])
        CP("dve", Osb.ap, bo[0:65, :], [bo], [Osb])
        btv = bt[:, 0:260].rearrange("p (i x) -> p i x", x=65)
        for i in range(4):
            TR(bt[:, i * 65:(i + 1) * 65], Osb[:, i * 128:(i + 1) * 128], identf[0:65, 0:65], [Osb, identf], [bt])
        TTo("dve", wsm[:, 44:48].unsqueeze(2), btv[:, :, 64:65], wsm[:, 36 + g * 4:40 + g * 4].unsqueeze(2),
            ALU.add, [bt, wsm], [wsm])
        OP("dve", "reciprocal", [wsm], [wsm], wsm[:, 48:52], wsm[:, 44:48])
        TTo("dve", oa[:, g * 4:(g + 1) * 4, :], btv[:, :, 0:64],
            wsm[:, 48:52].unsqueeze(2).to_broadcast([128, 4, 64]), ALU.mult, [bt, wsm], [oa])

    def wa_pair(j):
        fa = (lambda: wa_g(j - 2, 1, 0)) if has_wa(j) else None
        fb = (lambda: wa_g(j - 1, 0, 1)) if has_wa(j + 1) else None
        interleave2(fa, fb)

    def wa_d(n):
        wsm = wsm9[n % 2]
        oa = oa2[n % 2]
        oaf = oa.ap.rearrange("p h d -> p (h d)")
        ACT(sq9.ap, oaf, AF.Square, [oa], [sq9])
        RED(wsm[:, 52:53], sq9.ap, ALU.add, [sq9], [wsm])
        RSQ(wsm[:, 53:54], wsm[:, 52:53], 512.0 * RMS_EPS, [wsm], [wsm])
        mx = mixa[n % 2]
        STT("dve", mx.ap, oaf, wsm[:, 53:54], Ga.ap, ALU.mult, ALU.mult, [oa, wsm, Ga], [mx])
        DMA("sp", mixa_s[n * 128:(n + 1) * 128, :], mx.ap, [mx], [tk_mixa])

    sq3 = [A("sq3_%d" % i, [128, 288], F32) for i in range(2)]
    sq5 = A("sq5", [128, 512], F32)
    sq7 = A("sq7", [128, 128], F32)
    sq8 = A("sq8", [128, 768], F32)
    rt3 = [(A("rt3a%d" % i, [128, 32], F32), A("rt3b%d" % i, [128, 32], F32)) for i in range(2)]
    rt7 = (A("rt7a", [128, 128], F32), A("rt7b", [128, 128], F32))
    rt8a = (A("rt8aa", [128, 512], F32), A("rt8ab", [128, 512], F32))
    rt8d = (A("rt8da", [128, 128], F32), A("rt8db", [128, 128], F32))
    sm8s = [A("sm8_%d" % i, [128, 8], F32) for i in range(2)]
    kvf = [A("kvf%d" % i, [128, 288], F32) for i in range(4)]
    xn = [A("xn%d" % i, [128, 1024], F32) for i in range(2)]
    kr2all = A("kr2all", [128, NJ], F32)
    ssqkv = A("ssqkv", [128, NJ], F32)
    wsm7 = A("wsm7", [128, 64], F32)
    keyidx = {}
    _kt = 0
    for j in range(NJ):
        if j not in (0, NOWN + 1):
            keyidx[j] = _kt
            _kt += 1

    def proj(hT_, bank, ncols, col0):
        for c in range(8):
            MM(PB[bank][:, 0:ncols], hT_[:, c, :], winb[:, c, col0:col0 + ncols], c == 0, c == 7,
               [hT_, winb], [PB[bank]])

    is_key_ = lambda j: j not in (0, NOWN + 1)
    is_win_ = lambda j: j <= NOWN + 1
    is_own_ = lambda j: 1 <= j <= NOWN

    def sA0(j):
        s = j % 2
        x_, st_ = xt[s], stt[s]
        if j == 0:
            DMA("pool", x_.ap, xr[0:128, :], [], [x_])
        if j + 1 < NJ:
            DMA("pool", xt[(j + 1) % 2].ap, xr[(j + 1) * 128:(j + 2) * 128, :], [], [xt[(j + 1) % 2]])
        OP("dve", "bn_stats", [x_], [st_], st_[:, 0:6], x_[:, 0:512])
        OP("dve", "bn_stats", [x_], [st_], st_[:, 6:12], x_[:, 512:1024])
        OP("dve", "bn_aggr", [st_], [st_], st_[:, 12:14], st_[:, 0:12])
        TSo("dve", st_[:, 14:15], st_[:, 13:14], LN_EPS, None, ALU.add, None, [st_], [st_])

    def sA1(j):
        s = j % 2
        x_, st_, xn_ = xt[s], stt[s], xn[s]
        ACT(st_[:, 14:15], st_[:, 14:15], AF.Ln, [st_], [st_])
        ACT(st_[:, 14:15], st_[:, 14:15], AF.Exp, [st_], [st_], scale=-0.5)
        ACT(st_[:, 15:16], st_[:, 12:13], AF.Identity, [st_], [st_], scale=st_[:, 14:15])
        OP("act", "mul", [st_], [st_], st_[:, 15:16], st_[:, 15:16], -1.0)
        ACT(xn_.ap, x_.ap, AF.Identity, [x_, st_], [xn_], bias=st_[:, 15:16], scale=st_[:, 14:15])

    def sA2(j):
        xn_ = xn[j % 2]
        TTo("pool", xn_.ap, xn_.ap, G_emb.ap, ALU.mult, [xn_, G_emb], [xn_])

    def sA3(j):
        s = j % 2
        xn_, hf_, hb_ = xn[s], hf[s], hb[s]
        if is_own_(j):
            TTo("dve", hf_.ap, xn_.ap, B_emb.ap, ALU.add, [xn_, B_emb], [hf_])
        else:
            TTo("pool", hb_.ap, xn_.ap, B_emb.ap, ALU.add, [xn_, B_emb], [hb_])

    def sA4(j):
        s = j % 2
        hf_, hb_ = hf[s], hb[s]
        DMA("sp", h_s[(j - 1) * 128:j * 128, :], hf_.ap, [hf_], [tk_h])
        CP("act", hb_.ap, hf_.ap, [hf_], [hb_])

    def sA5(j):
        hb_ = hb[j % 2]
        for c in range(8):
            TR(pbf(0)[:, c * 128:(c + 1) * 128], hb_[:, c * 128:(c + 1) * 128], identb, [hb_, cstb], [PB[0]])

    def sA6(j):
        CP(("act", "dve")[j % 2], hT[j % 2].ap, pbf(0).rearrange("p (c t) -> p c t", t=128), [PB[0]], [hT[j % 2]])

    def sA7(j):
        proj(hT[j % 2], 1, 288, 1536)

    def sA8(j):
        kf = kvf[j % 4]
        p1 = PB[1]
        CP("act", kf.ap, p1[:, 0:288], [p1], [kf])
        ACT(sq3[j % 2].ap, kf.ap, AF.Square, [kf], [sq3[j % 2]])

    def sA9(j):
        kf = kvf[j % 4]
        n = 32
        tA_, tB_ = rt3[j % 2]
        src = kf[:, 256:288].rearrange("p (h t f) -> p h t f", h=1, t=2)
        tA = tA_[:, 0:n].rearrange("p (h t f) -> p h t f", t=2, f=16)
        tB = tB_[:, 0:n].rearrange("p (h t f) -> p h t f", t=2, f=16)
        cos, sin = tabB[:, j, 1, :], tabB[:, j, 0, :]
        cb = cos.unsqueeze(1).unsqueeze(1).to_broadcast([128, 1, 2, 16])
        sb_ = sin.unsqueeze(1).to_broadcast([128, 1, 16])
        TTo("dve", tA, src, cb, ALU.mult, [kf, tabB], [tA_])
        TTo("dve", tB[:, :, 0, :], src[:, :, 1, :], sb_, ALU.mult, [kf, tabB], [tB_])
        TTo("dve", tB[:, :, 1, :], src[:, :, 0, :], sb_, ALU.mult, [kf, tabB], [tB_])
        RED(ssqkv[:, j:j + 1], sq3[j % 2][:, 0:256], ALU.add, [sq3[j % 2]], [ssqkv])
        RED(kr2all[:, j:j + 1], sq3[j % 2][:, 256:288], ALU.add, [sq3[j % 2]], [kr2all])
        TSo("dve", ssqkv[:, j:j + 1], ssqkv[:, j:j + 1], 256.0 * RMS_EPS, None, ALU.add, None, [ssqkv], [ssqkv])

    def sA10(j):
        kv_ = kvb[j % 2]
        tA_, tB_ = rt3[j % 2]
        tA = tA_[:, 0:32].rearrange("p (h t f) -> p h t f", t=2, f=16)
        tB = tB_[:, 0:32].rearrange("p (h t f) -> p h t f", t=2, f=16)
        dst = kv_[:, 256:288].rearrange("p (h t f) -> p h t f", h=1, t=2)
        ACT(ssqkv[:, j:j + 1], ssqkv[:, j:j + 1], AF.Ln, [ssqkv], [ssqkv])
        ACT(ssqkv[:, j:j + 1], ssqkv[:, j:j + 1], AF.Exp, [ssqkv], [ssqkv], scale=-0.5)
        TTo("pool", dst[:, :, 0, :], tA[:, :, 0, :], tB[:, :, 0, :], ALU.subtract, [tA_, tB_], [kv_])
        TTo("pool", dst[:, :, 1, :], tA[:, :, 1, :], tB[:, :, 1, :], ALU.add, [tA_, tB_], [kv_])

    def sA11(j):
        kf, kv_ = kvf[j % 4], kvb[j % 2]
        STT("dve", kv_[:, 0:256], kf[:, 0:256], ssqkv[:, j:j + 1], Gkv.ap, ALU.mult, ALU.mult, [kf, ssqkv, Gkv], [kv_])

    def sA12(j):
        kv_ = kvb[j % 2]
        for c in range(2):
            TR(pbf(2)[:, c * 128:(c + 1) * 128], kv_[:, c * 128:(c + 1) * 128], identb, [kv_, cstb], [PB[2]])
        TR(pbf(2)[0:32, 256:384], kv_[:, 256:288], identb, [kv_, cstb], [PB[2]])

    def sA13(j):
        kt = keyidx[j]
        g, q = (kt // 4) % 2, kt % 4
        lt_ = latT[j % 2]
        CP("act", lt_.ap, pbf(2)[:, 0:256].rearrange("p (c t) -> p c t", t=128), [PB[2]], [lt_])
        CP("act", KrTbuf[g][:, q * 128:(q + 1) * 128], pbf(2)[0:32, 256:384], [PB[2]], [KrTbuf[g]])

    def sA14(j):
        lt_ = latT[j % 2]
        for half in range(2):
            for c in range(2):
                MM(PB[3 + half].ap, lt_[:, c, :], wkvb[:, c, half * 512:(half + 1) * 512], c == 0, c == 1,
                   [lt_, wkvb], [PB[3 + half]])
        p5 = PB[5]
        for pair in range(4):
            for c in range(2):
                MM(p5[:, pair * 128:(pair + 1) * 128], wkb[:, c, pair * 128:(pair + 1) * 128], lt_[:, c, :],
                   c == 0, c == 1, [wkb, lt_], [p5])

    def sA15(j):
        kt = keyidx[j]
        g, q = (kt // 4) % 2, kt % 4
        for half in range(2):
            pv = PB[3 + half].ap.rearrange("p (h x) -> p h x", x=128)
            CP("dve", Vbuf[g][:, half * 4:(half + 1) * 4, q, 0:64], pv[:, :, 64:128], [PB[3 + half]], [Vbuf[g]])
            ACT(sq5.ap.rearrange("p (h d) -> p h d", d=64)[:, half * 4:(half + 1) * 4, :], pv[:, :, 0:64],
                AF.Square, [PB[3 + half]], [sq5])
        p5 = PB[5]
        CP(("dve", "act")[j % 2], KTbuf[g][:, :, q * 128:(q + 1) * 128], p5.ap.rearrange("p (a t) -> p a t", t=128),
           [p5], [KTbuf[g]])

    def sA16(j):
        kt = keyidx[j]
        g, q = (kt // 4) % 2, kt % 4
        RED(kn2.ap, sq5.ap.rearrange("p (h d) -> p h d", d=64), ALU.add, [sq5], [kn2])
        STT("dve", kmax2.ap, kn2.ap, kr2all[:, j:j + 1], kmax2.ap, ALU.add, ALU.max, [kn2, kr2all, kmax2], [kmax2])
        if q == 3 or kt == NK - 1:
            kt0 = kt - q
            nq = q + 1
            DMA("sp", KnT_s.rearrange("(a p) t -> p a t", p=128)[:, :, kt0 * 128:(kt0 + nq) * 128],
                KTbuf[g][:, :, 0:nq * 128], [KTbuf[g]], [tk_KnT])
            DMA("sp", KrT_s[:, kt0 * 128:(kt0 + nq) * 128], KrTbuf[g][:, 0:nq * 128], [KrTbuf[g]], [tk_KrT])
            DMA("sp", V_s.rearrange("h p f -> p h f")[:, :, kt0 * 72:(kt0 + nq) * 72],
                Vbuf[g][:, :, 0:nq, :].rearrange("p h q x -> p h (q x)"), [Vbuf[g]], [tk_V])

    def st7(j):
        s = j % 2
        w = j
        hT_ = hT[s]
        proj(hT_, 6, 256, 512)
        p6 = PB[6]
        rope(p6[:, 0:128].rearrange("p (h t f) -> p h t f", h=2, t=2), 2, 32, tabA[:, w, 1, :], tabA[:, w, 0, :],
             katok.ap.rearrange("p (h t f) -> p h t f", h=2, t=2), [p6, tabA], [katok], rt7)
        ACT(sq7.ap, p6[:, 0:128], AF.Square, [p6], [sq7])
        CP("act", Va[:, w % 8, :, 0:64], p6[:, 128:256].rearrange("p (h d) -> p h d", d=64), [p6], [Va])
        RED(wsm7[:, 56:58], sq7.ap.rearrange("p (h d) -> p h d", d=64), ALU.add, [sq7], [wsm7])
        pmax_bcast(wsm7[:, 56:58], wsm7.t, 2, kmaxa[:, w, :], kmaxa.t, 7, wsm7)
        for g_ in range(2):
            TR(pbf(2)[0:64, g_ * 128:(g_ + 1) * 128], katok[:, g_ * 64:(g_ + 1) * 64], identb, [katok, cstb], [PB[2]])
        evac(KaT[0:64, :, (w % 8) * 128:(w % 8 + 1) * 128], pbf(2)[0:64, 0:256].rearrange("p (g t) -> p g t", t=128),
             [PB[2]], [KaT])

    def st8a(j):
        s = j % 2
        n = j - 1
        sq_ = n % 6
        proj(hT[s], 3, 512, 0)
        p7 = PB[3]
        rope(p7.ap.rearrange("p (h t f) -> p h t f", h=8, t=2), 8, 32, tabA[:, j, 1, :], tabA[:, j, 0, :],
             qatok[sq_][:, :, 0:64].rearrange("p h (t f) -> p h t f", t=2), [p7, tabA], [qatok[sq_]], rt8a)
        ACT(sq8[:, 0:512], p7.ap, AF.Square, [p7], [sq8])
        RED(qn2a[sq_].ap, sq8[:, 0:512].rearrange("p (h d) -> p h d", d=64), ALU.add, [sq8], [qn2a[sq_]])

    def st8b(j):
        s = j % 2
        sm8 = sm8s[s]
        proj(hT[s], 6, 384, 768)
        proj(hT[s], 7, 384, 1152)
        ACT(sq8[:, 0:384], PB[6][:, 0:384], AF.Square, [PB[6]], [sq8])
        ACT(sq8[:, 384:768], PB[7][:, 0:384], AF.Square, [PB[7]], [sq8])
        STT("dve", cqn[:, 0:384], PB[6][:, 0:384], 1.0, Gq[:, 0:384], ALU.mult, ALU.mult, [PB[6], Gq], [cqn])
        STT("dve", cqn[:, 384:768], PB[7][:, 0:384], 1.0, Gq[:, 384:768], ALU.mult, ALU.mult, [PB[7], Gq], [cqn])
        RED(sm8[:, 4:5], sq8[:, 0:768], ALU.add, [sq8], [sm8])
        RSQ(sm8[:, 5:6], sm8[:, 4:5], 768.0 * RMS_EPS, [sm8], [sm8])

    def st8c(j):
        for c in range(6):
            TR(pbf(0)[:, c * 128:(c + 1) * 128], cqn[:, c * 128:(c + 1) * 128], identb, [cqn, cstb], [PB[0]])
        evac(cqT.ap, pbf(0)[:, 0:768].rearrange("p (c t) -> p c t", t=128), [PB[0]], [cqT])

    def st8d(j):
        n = j - 1
        sm8 = sm8s[j % 2]
        for half in range(2):
            for c in range(6):
                MM(PB[6 + half][:, 0:384], cqT[:, c, :], wqb[:, c, half * 384:(half + 1) * 384], c == 0, c == 5,
                   [cqT, wqb], [PB[6 + half]])
        for half in range(2):
            pq = PB[6 + half][:, 0:384].rearrange("p (h x) -> p h x", x=96)
            OP("act", "activation", [PB[6 + half], sm8], [qtok], qtok[:, half * 4:(half + 1) * 4, 0:64], pq[:, :, 0:64],
               AF.Identity, scale=sm8[:, 5:6])
            ACT(sq8[:, 0:768].rearrange("p (h x) -> p h x", x=96)[:, half * 4:(half + 1) * 4, :], pq, AF.Square,
                [PB[6 + half], sm8], [sq8], scale=sm8[:, 5:6])
            rope(pq[:, :, 64:96].rearrange("p h (t f) -> p h t f", t=2), 4, 16, tabB[:, j, 1, :], tabB[:, j, 0, :],
                 qtok[:, half * 4:(half + 1) * 4, 64:96].rearrange("p h (t f) -> p h t f", t=2),
                 [PB[6 + half], tabB, sm8], [qtok], rt8d, scl=sm8[:, 5:6])
        RED(qn2b[:, n, :], sq8[:, 0:768].rearrange("p (h x) -> p h x", x=96), ALU.add, [sq8], [qn2b])

    def st8e(j):
        n = j - 1
        for h in range(8):
            TR(pbf(0)[0:96, h * 128:(h + 1) * 128], qtok[:, h, :], identb, [qtok, cstb], [PB[0]])
        evac(qst[n % 2].ap, pbf(0)[0:96, :].rearrange("p (h t) -> p h t", t=128), [PB[0]], [qst[n % 2]])
        DMA("sp", QT_s[0:96, :, n * 128:(n + 1) * 128], qst[n % 2].ap, [qst[n % 2]], [tk_QT])

    has_wa = lambda j: 2 <= j <= NOWN + 1 and "wattn" not in SKIP
    def m2(f, g_):
        def h(j):
            f(j)
            g_(j)
        return h
    stages = [
        (0, sA0, lambda j: True),
        (1, sA1, lambda j: True),
        (2, sA2, lambda j: True),
        (3, sA3, lambda j: True),
        (4, sA4, is_own_),
        (5, m2(sA5, sA6), lambda j: True),
        (6, m2(sA7, sA8), is_key_),
        (6, lambda j: interleave2((lambda: st7(j)), (lambda: st8a(j)) if is_own_(j) else None), is_win_),
        (7, sA9, is_key_),
        (7, st8b, is_own_),
        (8, sA10, is_key_),
        (8, st8c, is_own_),
        (9, sA11, is_key_),
        (9, lambda j: interleave2((lambda: st8d(j)) if is_own_(j) else None,
                                  (lambda: wa_a(j - 2)) if has_wa(j) else None),
         lambda j: is_own_(j) or has_wa(j)),
        (10, m2(sA12, sA13), is_key_),
        (10, st8e, is_own_),
        (11, m2(sA14, sA15), is_key_),
        (11, wa_pair, lambda j: has_wa(j) or has_wa(j + 1)),
        (12, sA16, is_key_),
        (12, lambda j: wa_d(j - 2), has_wa),
    ]
    maxoff = max(o for o, _, _ in stages)
    for step in range(NJ + maxoff):
        for off, fn, pred in sorted(stages, key=lambda x: -x[0]):
            j = step - off
            if 0 <= j < NJ and pred(j):
                fn(j)
        if STOPAT == "S%d" % step:
            S.barrier()
            S.emit(nc, es, final=True)
            return nc


    if dbg:
        d_tabB = dout("d_tabB", [128, NJ * 32])
        DMA("sp", d_tabB, tabB.ap.rearrange("p n t f -> p (n t f)"), [tabB], [Tok()])
    S.barrier()
    S.emit(nc, es)
    esA.close()
    esX = ExitStack()
    nmT = sb("nmT", [8, NQ], BF16, esX)
    nm = sb("nm", [128, NOWN, 8], BF16, esX)
    wsm = sb("wsm2", [128, 64], F32, esX)
    sqs = sb("sqs2", [128, NOWN * 8], F32, esX)
    pmax_bcast(kmax2.ap, kmax2.t, 8, wsm[:, 56:64], wsm.t, 7, wsm)
    for n in range(NOWN):
        TTo("dve", sqs[:, n * 8:(n + 1) * 8], qn2b[:, n, :], wsm[:, 56:64], ALU.mult, [qn2b, wsm], [sqs])
    ACT(sqs[:, 0:NOWN * 8], sqs[:, 0:NOWN * 8], AF.Ln, [sqs], [sqs])
    ACT(sqs[:, 0:NOWN * 8], sqs[:, 0:NOWN * 8], AF.Exp, [sqs], [sqs], scale=0.5)
    TSo("dve", nm.ap.rearrange("p n h -> p (n h)"), sqs[:, 0:NOWN * 8], -1.0, None, ALU.mult, None, [sqs], [nm])
    for n0 in range(0, NOWN, 8):
        nn = min(8, NOWN - n0)
        for i in range(nn):
            TR(pbf(0)[0:8, i * 128:(i + 1) * 128], nm[:, n0 + i, :], identb, [nm, cstb], [PB[0]])
        evac(nmT[:, n0 * 128:(n0 + nn) * 128], pbf(0)[0:8, 0:nn * 128], [PB[0]], [nmT])
    DMA("sp", QT_s[96], nmT.ap, [nmT], [tk_QT])

    if dbg:
        d_QT = dout("d_QT", [97, 8 * NQ], BF16)
        DMA("sp", d_QT, QT_s.rearrange("p h n -> p (h n)"), [tk_QT], [Tok()])
        d_KnT = dout("d_KnT", [512, NK * 128], BF16)
        DMA("sp", d_KnT, KnT_s, [tk_KnT], [Tok()])
        d_KrT = dout("d_KrT", [32, NK * 128], BF16)
        DMA("sp", d_KrT, KrT_s, [tk_KrT], [Tok()])
        d_V = dout("d_V", [8 * 128, NK * 72], BF16)
        DMA("sp", d_V, V_s.rearrange("h p f -> (h p) f"), [tk_V], [Tok()])
        if "wattn" not in SKIP:
            d_mixa = dout("d_mixa", [NQ, 512], BF16)
            DMA("sp", d_mixa, mixa_s, [tk_mixa], [Tok()])
        d_h = dout("d_h", [NQ, 1024], F32)
        DMA("sp", d_h, h_s, [tk_h], [Tok()])

    S.barrier()
    S.emit(nc, es, final=(stage == "A"))
    esX.close()
    esT.close()
    print("phase A ops", len(S.ops), "standalone waits", S.nwait)

    if stage == "A":
        es.close()
        return nc

    esB = ExitStack()
    Bt = lambda name, shape, dt: sb(name, shape, dt, esB)
    KT = [Bt("KT%d" % i, [97, NK * 128], BF16) for i in range(2)]
    Vh = [Bt("Vh%d" % i, [128, NK, 72], BF16) for i in range(2)]
    QTh = [Bt("QTh%d" % i, [97, NQ], BF16) for i in range(2)]
    OsbB = [Bt("OsbB%d" % i, [65, 512], F32) for i in range(2)]
    obt = [Bt("obt%d" % i, [128, 4, 64], F32) for i in range(2)]
    wsB = [Bt("wsB%d" % i, [128, 8], F32) for i in range(2)]
    for kt_ in KT:
        OP("pool", "memset", [], [kt_], kt_[64:97, :], 1.0)
    NQB = NOWN // 4

    def load_head(h):
        hb_ = h % 2
        half = (NK * 128) // 2
        DMA("sp", KT[hb_][0:64, 0:half], KnT_s[h * 64:(h + 1) * 64, 0:half], [tk_KnT], [KT[hb_]])
        DMA("pool", KT[hb_][0:64, half:], KnT_s[h * 64:(h + 1) * 64, half:], [tk_KnT], [KT[hb_]])
        DMA("sp", KT[hb_][64:96, :], KrT_s, [tk_KrT], [KT[hb_]])
        DMA("pool", Vh[hb_].ap.rearrange("p c x -> p (c x)"), V_s[h], [tk_V], [Vh[hb_]])
        DMA("sp", QTh[hb_].ap, QT_s[:, h, :], [tk_QT], [QTh[hb_]])

    PT = [Bt("PT%d" % i, [128, 512], BF16) for i in range(4)]
    units = [(h, qb, c) for h in range(8) for qb in range(NQB) for c in range(NK)]
    NU = len(units)
    LA = 2
    pending = []

    def epilogue(h, qb, blk):
        ob_ = PB[4 + blk % 2]
        os_ = OsbB[blk % 2]
        bt = PB[6 + blk % 2]
        ot = obt[blk % 2]
        ws = wsB[blk % 2]
        CP("dve", os_.ap, ob_[0:65, :], [ob_], [os_])

        def part2():
            btv = bt[:, 0:260].rearrange("p (i x) -> p i x", x=65)
            for i in range(4):
                TR(bt[:, i * 65:(i + 1) * 65], os_[:, i * 128:(i + 1) * 128], identf[0:65, 0:65], [os_, identf], [bt])
            OP("dve", "reciprocal", [bt], [ws], ws[:, 0:4].unsqueeze(2), btv[:, :, 64:65])
            TTo("dve", ot.ap, btv[:, :, 0:64], ws[:, 0:4].unsqueeze(2).to_broadcast([128, 4, 64]), ALU.mult,
                [bt, ws], [ot])
            DMA("pool", outb_s[qb * 512:(qb + 1) * 512, h * 64:(h + 1) * 64].rearrange("(i p) d -> p i d", p=128),
                ot.ap, [ot], [tk_outb])
        return part2

    for i in range(NU + LA):
        while pending and pending[0][0] <= i:
            pending.pop(0)[1]()
        if i < NU:
            h, qb, c = units[i]
            if qb == 0 and c == 0 and h == 0:
                load_head(0)
            hb_ = h % 2
            bs = PB[i % 3]
            MM(bs.ap, KT[hb_][0:97, c * 128:(c + 1) * 128], QTh[hb_][0:97, qb * 512:(qb + 1) * 512], True, True,
               [KT[hb_], QTh[hb_]], [bs])
            ACT(PT[i % 4].ap, bs.ap, AF.Exp, [bs], [PT[i % 4]])
        ip = i - LA
        if ip >= 0:
            h, qb, c = units[ip]
            if qb == 0 and c == 0 and h + 1 < 8:
                load_head(h + 1)
            hb_ = h % 2
            blk = h * NQB + qb
            ob_ = PB[4 + blk % 2]
            MM(ob_[0:65, :], Vh[hb_][:, c, 0:65], PT[ip % 4].ap, c == 0, c == NK - 1, [Vh[hb_], PT[ip % 4]], [ob_])
            if c == NK - 1:
                pending.append((i + 6, epilogue(h, qb, blk)))
    for _, f in pending:
        f()

    if dbg:
        d_outb = dout("d_outb", [NQ, 512], F32)
        DMA("sp", d_outb, outb_s, [tk_outb], [Tok()])

    S.barrier()
    S.emit(nc, es, final=(stage == "AB"))
    esB.close()
    print("phase B ops", len(S.ops), "standalone waits", S.nwait)
    if stage == "AB":
        es.close()
        return nc

    NSB = 2 if NOWN >= 8 else 1
    SBT = NOWN // NSB
    NT = SBT * 128
    cmb_s = dscr("cmb_s", [NSB, 32, NT], F32)
    tk_cmb = Tok("cmb_s")
    tk_y = Tok("y")
    esC = ExitStack()
    C = lambda name, shape, dt: sb(name, shape, dt, esC)
    Gob = C("Gob", [128, 512], F32)
    G_at = C("G_at", [128, 1024], F32)
    B_at = C("B_at", [128, 1024], F32)
    G_ff = C("G_ff", [128, 1024], F32)
    B_ff = C("B_ff", [128, 1024], F32)
    brB = C("brB", [128, 36], F32)
    wr_f = C("wr_f", [128, 8, 36], F32)
    yacc = C("yacc", [128, SBT, 1024], F32)
    h2T = C("h2T", [128, 8, NT], BF16)
    cmbT = C("cmbT", [32, NT], F32)
    vload(Gob, "ob_g")
    vload(G_at, "ln_attn_g")
    vload(B_at, "ln_attn_b")
    vload(G_ff, "ln_ffn_g")
    vload(B_ff, "ln_ffn_b")
    vload(brB, "b_router")
    TSo("dve", Gob.ap, Gob.ap, math.sqrt(512.0), None, ALU.mult, None, [Gob], [Gob])
    DMA("sp", wr_f.ap, w_router.rearrange("(c p) n -> p c n", p=128), [], [wr_f])

    def layer_norm_tile(src, st_, Gt, Bt_, dst):
        OP("dve", "bn_stats", [src], [st_], st_[:, 0:6], src[:, 0:512])
        OP("dve", "bn_stats", [src], [st_], st_[:, 6:12], src[:, 512:1024])
        OP("dve", "bn_aggr", [st_], [st_], st_[:, 12:14], st_[:, 0:12])
        RSQ(st_[:, 14:15], st_[:, 13:14], LN_EPS, [st_], [st_])
        STT("dve", st_[:, 15:16], st_[:, 12:13], -1.0, st_[:, 14:15], ALU.mult, ALU.mult, [st_], [st_])
        ACT(src.ap, src.ap, AF.Identity, [src, st_], [src], bias=st_[:, 15:16], scale=st_[:, 14:15])
        TTo("pool", src.ap, src.ap, Gt.ap, ALU.mult, [src, Gt], [src])
        TTo("dve", dst.ap, src.ap, Bt_.ap, ALU.add, [src, Bt_], [dst])

    for sbi in range(NSB):
        es1 = ExitStack()
        mk1 = lambda name, shape, dt: sb("%s_s%d" % (name, sbi), shape, dt, es1)
        woutb = mk1("woutb", [128, 8, 1024], BF16)
        mix = [mk1("mix%d" % i, [128, 1024], BF16) for i in range(2)]
        obf = [mk1("obf%d" % i, [128, 512], F32) for i in range(6)]
        hin = [mk1("hin%d" % i, [128, 1024], F32) for i in range(2)]
        rr = [mk1("rr%d" % i, [128, 1024], F32) for i in range(2)]
        rn = [mk1("rn%d" % i, [128, 1024], F32) for i in range(2)]
        h2 = [mk1("h2_%d" % i, [128, 1024], F32) for i in range(2)]
        h2Tf = mk1("h2Tf", [128, 8, 128], F32)
        mixT = mk1("mixT", [128, 8, 128], BF16)
        sq1 = [mk1("sq1_%d" % i, [128, 512], F32) for i in range(2)]
        sta = [mk1("sta_%d" % i, [128, 4], F32) for i in range(4)]
        stb = [mk1("stb_%d" % i, [128, 16], F32) for i in range(2)]
        rt = [mk1("rt%d" % i, [128, 256], F32) for i in range(4)]
        wst1 = rr
        for c in range(8):
            DMA("sp", wst1[c % 2].ap, w_out[c * 128:(c + 1) * 128, :], [], [wst1[c % 2]])
            CP(("dve", "pool")[c % 2], woutb[:, c, :], wst1[c % 2].ap, [wst1[c % 2]], [woutb])

        def rows_(l):
            n = sbi * SBT + l
            return slice(n * 128, (n + 1) * 128)

        def c0(l):
            DMA("sp", obf[l % 6].ap, outb_s[rows_(l), :], [tk_outb], [obf[l % 6]])

        def c1(l):
            ACT(sq1[l % 2].ap, obf[l % 6].ap, AF.Square, [obf[l % 6]], [sq1[l % 2]])

        def c2(l):
            st_ = sta[l % 4]
            RED(st_[:, 0:1], sq1[l % 2].ap, ALU.add, [sq1[l % 2]], [st_])
            TSo("dve", st_[:, 1:2], st_[:, 0:1], 512.0 * RMS_EPS, None, ALU.add, None, [st_], [st_])

        def c3(l):
            st_ = sta[l % 4]
            ACT(st_[:, 1:2], st_[:, 1:2], AF.Ln, [st_], [st_])
            ACT(st_[:, 1:2], st_[:, 1:2], AF.Exp, [st_], [st_], scale=-0.5)
            DMA("sp", mix[l % 2][:, 0:512], mixa_s[rows_(l), :], [tk_mixa], [mix[l % 2]])

        def c4(l):
            mx = mix[l % 2]
            STT("dve", mx[:, 512:1024], obf[l % 6].ap, sta[l % 4][:, 1:2], Gob.ap, ALU.mult, ALU.mult,
                [obf[l % 6], sta[l % 4], Gob], [mx])
            DMA("pool", hin[l % 2].ap, h_s[rows_(l), :], [tk_h], [hin[l % 2]])

        def c5(l):
            mx = mix[l % 2]
            for c in range(8):
                TR(pbf(0)[:, c * 128:(c + 1) * 128], mx[:, c * 128:(c + 1) * 128], identb, [mx, cstb], [PB[0]])
            CP("act", mixT.ap, pbf(0).rearrange("p (c t) -> p c t", t=128), [PB[0]], [mixT])

        def c6(l):
            hi_, r_ = hin[l % 2], rr[l % 2]
            for half in range(2):
                for c in range(8):
                    MM(PB[1 + half].ap, mixT[:, c, :], woutb[:, c, half * 512:(half + 1) * 512], c == 0, c == 7,
                       [mixT, woutb], [PB[1 + half]])
            for half in range(2):
                cs = slice(half * 512, (half + 1) * 512)
                STT("dve", r_[:, cs], hi_[:, cs], ALPHA, PB[1 + half].ap, ALU.mult, ALU.add, [hi_, PB[1 + half]], [r_])

        def c7(l):
            r_, st_ = rr[l % 2], stb[l % 2]
            OP("dve", "bn_stats", [r_], [st_], st_[:, 0:6], r_[:, 0:512])
            OP("dve", "bn_stats", [r_], [st_], st_[:, 6:12], r_[:, 512:1024])
            OP("dve", "bn_aggr", [st_], [st_], st_[:, 12:14], st_[:, 0:12])
            TSo("dve", st_[:, 14:15], st_[:, 13:14], LN_EPS, None, ALU.add, None, [st_], [st_])

        def c8(l):
            r_, st_, rn_ = rr[l % 2], stb[l % 2], rn[l % 2]
            ACT(st_[:, 14:15], st_[:, 14:15], AF.Ln, [st_], [st_])
            ACT(st_[:, 14:15], st_[:, 14:15], AF.Exp, [st_], [st_], scale=-0.5)
            ACT(st_[:, 15:16], st_[:, 12:13], AF.Identity, [st_], [st_], scale=st_[:, 14:15])
            OP("act", "mul", [st_], [st_], st_[:, 15:16], st_[:, 15:16], -1.0)
            ACT(rn_.ap, r_.ap, AF.Identity, [r_, st_], [rn_], bias=st_[:, 15:16], scale=st_[:, 14:15])

        def c9(l):
            rn_ = rn[l % 2]
            TTo("pool", rn_.ap, rn_.ap, G_at.ap, ALU.mult, [rn_, G_at], [rn_])

        def c10(l):
            TTo("dve", h2[l % 2].ap, rn[l % 2].ap, B_at.ap, ALU.add, [rn[l % 2], B_at], [h2[l % 2]])

        def c11(l):
            h2_ = h2[l % 2]
            OP("act", "mul", [h2_], [yacc], yacc[:, l, :], h2_.ap, ALPHA)
            for c in range(8):
                TR(PB[3 + c // 4][:, (c % 4) * 128:(c % 4 + 1) * 128], h2_[:, c * 128:(c + 1) * 128], identf.ap,
                   [h2_, identf], [PB[3 + c // 4]])
            for hh in range(2):
                pv = PB[3 + hh].ap.rearrange("p (c t) -> p c t", t=128)
                CP("act", h2T[:, hh * 4:(hh + 1) * 4, l * 128:(l + 1) * 128], pv, [PB[3 + hh]], [h2T])
                CP("dve", h2Tf[:, hh * 4:(hh + 1) * 4, :], pv, [PB[3 + hh]], [h2Tf])

        def c12(l):
            rt_ = rt[l % 4]
            for c in range(8):
                MM(PB[5][:, 0:36], h2Tf[:, c, :], wr_f[:, c, :], c == 0, c == 7, [h2Tf, wr_f], [PB[5]])
            TTo("dve", rt_[:, 0:36], PB[5][:, 0:36], brB.ap, ALU.add, [PB[5], brB], [rt_])

        def c13(l):
            rt_ = rt[l % 4]
            RED(rt_[:, 40:41], rt_[:, 0:4], ALU.max, [rt_], [rt_])
            TSo("dve", rt_[:, 44:48], rt_[:, 0:4], rt_[:, 40:41], None, ALU.is_equal, None, [rt_], [rt_])
            TSo("dve", rt_[:, 48:52], rt_[:, 44:48], 1.0, 1e30, ALU.subtract, ALU.mult, [rt_], [rt_])
            elm = rt_[:, 64:96]
            TTo("dve", elm.rearrange("p (g j) -> p g j", j=8), rt_[:, 4:36].rearrange("p (g j) -> p g j", j=8),
                rt_[:, 48:52].unsqueeze(2).to_broadcast([128, 4, 8]), ALU.add, [rt_], [rt_])
            RED(rt_[:, 41:42], elm, ALU.max, [rt_], [rt_])
            TSo("dve", rt_[:, 96:128], elm, rt_[:, 41:42], None, ALU.is_equal, None, [rt_], [rt_])
            STT("dve", rt_[:, 128:160], rt_[:, 96:128], -1e30, elm, ALU.mult, ALU.add, [rt_], [rt_])
            RED(rt_[:, 42:43], rt_[:, 128:160], ALU.max, [rt_], [rt_])
            TSo("dve", rt_[:, 160:192], rt_[:, 128:160], rt_[:, 42:43], None, ALU.is_equal, None, [rt_], [rt_])
            TSo("dve", rt_[:, 43:44], rt_[:, 40:41], -1.0, None, ALU.mult, None, [rt_], [rt_])
            TTo("dve", rt_[:, 57:58], rt_[:, 42:43], rt_[:, 41:42], ALU.subtract, [rt_], [rt_])

        def c14(l):
            rt_ = rt[l % 4]
            ACT(rt_[:, 52:56], rt_[:, 0:4], AF.Exp, [rt_], [rt_], bias=rt_[:, 43:44], scale=1.0)
            ACT(rt_[:, 58:59], rt_[:, 57:58], AF.Exp, [rt_], [rt_])

        def c15(l):
            rt_ = rt[l % 4]
            RED(rt_[:, 56:57], rt_[:, 52:56], ALU.add, [rt_], [rt_])
            STT("dve", rt_[:, 59:60], rt_[:, 58:59], 1.0, rt_[:, 56:57], ALU.add, ALU.mult, [rt_], [rt_])
            OP("dve", "reciprocal", [rt_], [rt_], rt_[:, 60:61], rt_[:, 59:60])
            TTo("dve", rt_[:, 61:62], rt_[:, 60:61], rt_[:, 58:59], ALU.mult, [rt_], [rt_])
            TSo("dve", rt_[:, 192:224], rt_[:, 96:128], rt_[:, 60:61], None, ALU.mult, None, [rt_], [rt_])
            STT("dve", rt_[:, 192:224], rt_[:, 160:192], rt_[:, 61:62], rt_[:, 192:224], ALU.mult, ALU.add,
                [rt_], [rt_])
            TR(PB[6][0:32, 0:128], rt_[:, 192:224], identf.ap, [rt_, identf], [PB[6]])
            CP("dve", cmbT[:, l * 128:(l + 1) * 128], PB[6][0:32, 0:128], [PB[6]], [cmbT])

        cstages = [c0, c1, c2, c3, c4, c5, c6, c7, c8, c9, c10, c11, c12, c13, c14, c15]
        for step in range(SBT + len(cstages) - 1):
            for off in range(len(cstages) - 1, -1, -1):
                l = step - off
                if 0 <= l < SBT:
                    cstages[off](l)

        DMA("sp", cmb_s[sbi], cmbT.ap, [cmbT], [tk_cmb])
        if dbg and sbi == 0:
            d_h2T = dout("d_h2T", [128, 8 * NT], BF16)
            DMA("sp", d_h2T, h2T.ap.rearrange("p c n -> p (c n)"), [h2T], [Tok()])
            d_cmbT = dout("d_cmbT", [32, NT], F32)
            DMA("sp", d_cmbT, cmbT.ap, [cmbT], [Tok()])
        S.barrier()
        S.emit(nc, es)
        es1.close()
        es2 = ExitStack()
        mk2 = lambda name, shape, dt: sb("%s_s%d" % (name, sbi), shape, dt, es2)
        wstg = [mk2("wstg%d" % i, [128, 2048], F32) for i in range(3)]
        wg = [mk2("wg%d" % i, [128, 8, 256], BF16) for i in range(2)]
        wu = [mk2("wu%d" % i, [128, 8, 256], BF16) for i in range(2)]
        wd = [mk2("wd%d" % i, [128, 2, 1024], BF16) for i in range(2)]
        cB = [mk2("cB%d" % i, [128, NT], F32) for i in range(2)]
        sg = mk2("sg", [128, 1024], F32)
        t1 = mk2("t1", [128, 1024], F32)
        hid = [mk2("hid%d" % i, [128, 2, 512], BF16) for i in range(2)]
        NBLK = SBT // 4

        def load_expert(e):
            sl = e % 2
            DMA("sp", wstg[0].ap.rearrange("p (c f) -> p c f", f=256), w_gate[e].rearrange("(c p) f -> p c f", p=128),
                [], [wstg[0]])
            CP("act", wg[sl].ap.rearrange("p c f -> p (c f)"), wstg[0].ap, [wstg[0]], [wg[sl]])
            DMA("sp", wstg[1].ap.rearrange("p (c f) -> p c f", f=256), w_up[e].rearrange("(c p) f -> p c f", p=128),
                [], [wstg[1]])
            CP("act", wu[sl].ap.rearrange("p c f -> p (c f)"), wstg[1].ap, [wstg[1]], [wu[sl]])
            DMA("sp", wstg[2].ap.rearrange("p (c f) -> p c f", f=1024), w_down[e].rearrange("(c p) f -> p c f", p=128),
                [], [wstg[2]])
            CP("act", wd[sl].ap.rearrange("p c f -> p (c f)"), wstg[2].ap, [wstg[2]], [wd[sl]])
            DMA("sp", cB[sl].ap, cmb_s[sbi, e:e + 1, :].to_broadcast([128, NT]), [tk_cmb], [cB[sl]])

        eunits = [(e, b) for e in range(32) for b in range(NBLK)]
        dcount = [0]

        def down_pairs(e, b, ui):
            sl = e % 2
            hd = hid[ui % 2]
            outl = []
            for i in range(4):
                for half in range(2):
                    def f(i=i, half=half):
                        bank = PB[4 + dcount[0] % 4]
                        dcount[0] += 1
                        for ff in range(2):
                            MM(bank.ap, hd[:, ff, i * 128:(i + 1) * 128], wd[sl][:, ff, half * 512:(half + 1) * 512],
                               ff == 0, ff == 1, [hd, wd[sl]], [bank])
                        ya = yacc[:, b * 4 + i, half * 512:(half + 1) * 512]
                        TTo("dve", ya, ya, bank.ap, ALU.add, [yacc, bank], [yacc])
                    outl.append(f)
            return outl

        def gu(e, b, ui, inter):
            sl = e % 2
            T0 = b * 512
            nmm = 0
            for which, wt in ((0, wg), (1, wu)):
                for f in range(2):
                    bank = PB[which * 2 + f]
                    for c in range(8):
                        MM(bank.ap, wt[sl][:, c, f * 128:(f + 1) * 128], h2T[:, c, T0:T0 + 512], c == 0, c == 7,
                           [wt[sl], h2T], [bank])
                        nmm += 1
                        if nmm > 16 and nmm % 2 == 0 and inter:
                            inter.pop(0)()
            hd = hid[ui % 2]
            for f in range(2):
                fs = slice(f * 512, (f + 1) * 512)
                ACT(sg[:, fs], PB[f].ap, AF.Silu, [PB[f]], [sg])
                TTo("dve", t1[:, fs], sg[:, fs], PB[2 + f].ap, ALU.mult, [sg, PB[2 + f]], [t1])
                TTo("pool", hd[:, f, :], t1[:, fs], cB[sl][:, T0:T0 + 512], ALU.mult, [t1, cB[sl]], [hd])

        for ui in range(len(eunits) + 1):
            inter = []
            if ui >= 1:
                e, b = eunits[ui - 1]
                inter = down_pairs(e, b, ui - 1)
            if ui < len(eunits):
                e, b = eunits[ui]
                if b == 0 and e == 0:
                    load_expert(0)
                gu(e, b, ui, inter)
            for f in inter:
                f()
            if ui < len(eunits):
                e, b = eunits[ui]
                if b == 0 and e + 1 < 32:
                    load_expert(e + 1)
        S.barrier()
        S.emit(nc, es)
        es2.close()
        es3 = ExitStack()
        yo = [sb("yo%d_s%d" % (i, sbi), [128, 1024], F32, es3) for i in range(2)]
        yi = [sb("yi%d_s%d" % (i, sbi), [128, 1024], F32, es3) for i in range(2)]
        st3 = [sb("st3_%d_s%d" % (i, sbi), [128, 16], F32, es3) for i in range(2)]
        for l in range(SBT):
            n = sbi * SBT + l
            s = l % 2
            CP("act", yi[s].ap, yacc[:, l, :], [yacc], [yi[s]])
            layer_norm_tile(yi[s], st3[s], G_ff, B_ff, yo[s])
            DMA("sp", y_out[n * 128:(n + 1) * 128, :], yo[s].ap, [yo[s]], [tk_y])
        S.barrier()
        S.emit(nc, es, final=(sbi == NSB - 1))
        es3.close()
    esC.close()
    es.close()
    print("total ops", len(S.ops), "standalone waits", S.nwait)
    return nc

COMPUTE = ("pe", "act", "dve", "pool")
KD = 8
EPOCH = 12000


class Tok:
    __slots__ = ("n", "lw", "rd", "rdma", "x")

    def __init__(s, n="", x=False):
        s.n = n
        s.x = x
        s.lw = -1
        s.rd = {}
        s.rdma = []


class Sched:
    def __init__(s):
        s.ops = []
        s.lastc = {}
        s.bar = None
        s.bar_idx = 0
        s.bar_done = set()
        s.dmas_since_bar = []
        s.emitted = 0
        s.dq = {}
        s.semof = {}
        s.pos = {}
        s.cpos = {e: 0 for e in COMPUTE}
        s.cnt = {e: 0 for e in COMPUTE}
        s.seen = {}
        s.sems = {}
        s.final = {}
        s.nwait = 0

    def add(s, eng, fn, reads=(), writes=(), dma=False):
        i = len(s.ops)
        deps = set()
        xr = [t for t in reads if t.x]
        if xr:
            reads = [t for t in reads if not t.x]
            writes = list(writes) + [t for t in xr if t not in writes]
        for t in reads:
            if t.lw >= 0:
                deps.add(t.lw)
        for t in writes:
            if t.lw >= 0:
                deps.add(t.lw)
            deps.update(t.rd.values())
            deps.update(t.rdma)
        bi = s.bar_idx
        deps = set(d for d in deps if d >= bi)
        if s.bar is not None and eng not in s.bar_done:
            deps.update(s.bar)
            s.bar_done.add(eng)
        s.ops.append([eng, fn, deps, dma])
        for t in reads:
            if dma:
                t.rdma.append(i)
            else:
                t.rd[eng] = i
        for t in writes:
            t.lw = i
            t.rd = {}
            t.rdma = []
        if dma:
            s.dmas_since_bar.append(i)
        else:
            s.lastc[eng] = i
        return i

    def barrier(s):
        b = set(s.dmas_since_bar)
        b.update(s.lastc.values())
        if s.bar is not None and len(s.bar_done) < 5:
            b.update(s.bar)
        s.bar = b
        s.bar_idx = len(s.ops)
        s.bar_done = set()
        s.dmas_since_bar = []

    def emit(s, nc, stack, final=False):
        ops = s.ops
        lo, n = s.emitted, len(ops)
        semof, pos = s.semof, s.pos
        for i in range(lo, n):
            eng, fn, deps, dma = ops[i]
            if dma:
                lst = s.dq.setdefault(eng, [])
                k = len(lst)
                if k >= KD:
                    deps.add(lst[k - KD])
                lst.append(i)
                semof[i] = (("d", eng, k % KD), 16 * (k // KD + 1))
            else:
                pos[i] = s.cpos[eng]
                s.cpos[eng] += 1
        hasdep = set()
        for i in range(lo, n):
            hasdep.update(ops[i][2])
        hasdep.update(s.lastc.values())
        for i in range(lo, n):
            eng, fn, deps, dma = ops[i]
            if not dma and i in hasdep:
                c = s.cnt[eng]
                s.cnt[eng] = c + 1
                semof[i] = (("c", eng, c // EPOCH), c % EPOCH + 1)
        waits = {}
        for i in range(lo, n):
            eng, fn, deps, dma = ops[i]
            need = {}
            for d in deps:
                de, _, _, ddma = ops[d]
                if not ddma and de == eng and not dma:
                    if eng == "pe":
                        continue
                    if pos[i] - pos[d] > 3:
                        continue
                key, val = semof[d]
                if need.get(key, 0) < val:
                    need[key] = val
            sn = s.seen.setdefault(eng, {})
            w = []
            for key, val in need.items():
                if sn.get(key, 0) >= val:
                    continue
                sn[key] = val
                w.append((key, val))
            waits[i] = w
        sems = s.sems
        for i in range(lo, n):
            x = semof.get(i)
            if x is None:
                continue
            if x[0] not in sems:
                sems[x[0]] = stack.enter_context(nc.semaphore("s_%s_%s_%d" % x[0]))
            if x[0][0] == "d" and s.final.get(x[0], 0) < x[1]:
                s.final[x[0]] = x[1]
        with nc.Block() as block:
            def run(engname, eobj, tail=False):
                for i in range(lo, n):
                    o = ops[i]
                    if o[0] != engname:
                        continue
                    w = waits[i]
                    for key, val in w[:-1]:
                        eobj.wait_ge(sems[key], val)
                        s.nwait += 1
                    ins = o[1](eobj)
                    if w:
                        ins._wait_ge(sems[w[-1][0]], w[-1][1])
                    x = semof.get(i)
                    if x is not None:
                        ins.then_inc(sems[x[0]], 16 if x[0][0] == "d" else 1)
                    o[1] = None
                if tail:
                    for key, val in s.final.items():
                        eobj.wait_ge(sems[key], val)

            @block.sync
            def _(e):
                run("sp", e, tail=final)

            @block.scalar
            def _(e):
                run("act", e)

            @block.vector
            def _(e):
                run("dve", e)

            @block.gpsimd
            def _(e):
                run("pool", e)

            @block.tensor
            def _(e):
                run("pe", e)
        s.emitted = n


N_CORES = 8
NOWN_FULL = 32
NOTH_FULL = 96
_NC_CACHE = {}


def _host_prep(inputs, c):
    bf = ml_dtypes.bfloat16
    NOWN, NOTH = NOWN_FULL, NOTH_FULL
    seq_tiles = NOWN + NOTH
    b = c // 4
    qd = c % 4
    first = qd == 0
    last = qd == 3
    own0 = qd * NOWN
    x = inputs["x"][b]
    pos = inputs["positions"][b]
    own = list(range(own0, own0 + NOWN))
    others = [t for t in range(seq_tiles) if t not in own]
    P = own0 - 1 if not first else own0
    N = own0 + NOWN if not last else own0 + NOWN - 1
    order = [P] + own + [N] + others
    xr = np.concatenate([x[t * 128:(t + 1) * 128] for t in order], 0)
    posr = np.ascontiguousarray(np.stack([pos[t * 128:(t + 1) * 128] for t in order], 1).astype(np.int32))
    jj = np.arange(128)[:, None]
    ii = np.arange(128)[None, :]
    mP = np.where(jj >= ii, 0.0, NEGB).astype(np.float32)
    mN = np.where(jj <= ii, 0.0, NEGB).astype(np.float32)
    full = np.full((128, 128), NEGB, np.float32)
    ms = [mP, mN, full if first else mP, full if last else mN]
    cst_b = np.concatenate([np.eye(128, dtype=np.float32)] + [np.tile(m, (1, 4)) for m in ms], 1).astype(bf)
    return dict(xr=xr, posr=posr, cst_b=cst_b)


def kernel(**inputs):
    inputs = {k: np.asarray(v) for k, v in inputs.items()}
    g = lambda k: np.ascontiguousarray(inputs[k], dtype=np.float32)
    invA = (1.0 / (np.float32(10000.0) ** (np.arange(0, 64, 2, dtype=np.float32) / np.float32(64)))).astype(np.float32)
    invB = (1.0 / (np.float32(10000.0) ** (np.arange(0, 32, 2, dtype=np.float32) / np.float32(32)))).astype(np.float32)
    parts = dict(ln_emb_g=g("ln_emb_g"), ln_emb_b=g("ln_emb_b"), q_g=g("q_a_norm_g")[0], kv_g=g("kv_a_norm_g")[0],
                 oa_g=g("out_norm_a_g")[0], ob_g=g("out_norm_b_g")[0], ln_attn_g=g("ln_attn_g")[0],
                 ln_attn_b=g("ln_attn_b")[0], ln_ffn_g=g("ln_ffn_g")[0], ln_ffn_b=g("ln_ffn_b")[0], sink=g("a_sink")[0],
                 b_router=np.concatenate([g("b_group")[0], g("b_expert")[0]]), invA=invA, invB=invB)
    vecs = np.zeros((1, NVEC), np.float32)
    for k, (o, l) in VOFF.items():
        vecs[0, o:o + l] = parts[k]
    shared = dict(w_in=g("w_in")[0], w_q_b=g("w_q_b")[0], w_kv_b=g("w_kv_b")[0], w_out=g("w_out")[0],
                  w_router=np.ascontiguousarray(np.concatenate([g("w_group")[0], g("w_expert")[0]], 1)),
                  w_gate=g("w_gate")[0], w_up=g("w_up")[0], w_down=g("w_down")[0], vecs=vecs,
                  cst_f=np.eye(128, dtype=np.float32))
    in_maps = []
    for c in range(N_CORES):
        m = dict(shared)
        m.update(_host_prep(inputs, c))
        in_maps.append(m)
    if "nc" not in _NC_CACHE:
        _NC_CACHE["nc"] = build(NOWN_FULL, NOTH_FULL, stage="ABC", dbg=False)
    nc = _NC_CACHE["nc"]
    res = run_bass_kernel_spmd(nc, in_maps, core_ids=list(range(N_CORES)))
    B, Sq, D = inputs["x"].shape
    out = np.zeros((B, Sq, D), np.float32)
    for c in range(N_CORES):
        b = c // 4
        r0 = (c % 4) * NOWN_FULL * 128
        out[b, r0:r0 + NOWN_FULL * 128] = np.asarray(res.results[c]["y"], dtype=np.float32)
    return out
```

```python
import math
import numpy as np
import ml_dtypes
from contextlib import ExitStack
import concourse.bass as bass
import concourse.mybir as mybir
from concourse.bass_utils import run_bass_kernel_spmd

F32 = mybir.dt.float32
BF16 = mybir.dt.bfloat16
I32 = mybir.dt.int32
AF = mybir.ActivationFunctionType
ALU = mybir.AluOpType
AX = mybir.AxisListType

LN_EPS = 1e-5
RMS_EPS = 1e-6
ALPHA = 2.0 ** 0.25
NEGB = -30000.0
TWO_PI = 2.0 * math.pi
C1 = 6.28125
C2 = float(np.float32(TWO_PI - C1))
C3 = float(TWO_PI - C1 - float(np.float32(TWO_PI - C1)))
MAGIC = 12582912.0
PI_LIM = 3.1415925

VOFF = {}
_o = 0
for _n, _l in [("ln_emb_g", 1024), ("ln_emb_b", 1024), ("q_g", 768), ("kv_g", 256), ("oa_g", 512),
               ("ob_g", 512), ("ln_attn_g", 1024), ("ln_attn_b", 1024), ("ln_ffn_g", 1024),
               ("ln_ffn_b", 1024), ("sink", 8), ("b_router", 36), ("invA", 32), ("invB", 16)]:
    VOFF[_n] = (_o, _l)
    _o += _l
NVEC = _o


class TT:
    def __init__(s, h, name):
        s.h = h
        s.t = Tok(name)

    def __getitem__(s, k):
        return s.h[k]

    @property
    def ap(s):
        return s.h[:]


def build(NOWN, NOTH, stage="ABC", dbg=False):
    NJ = NOWN + NOTH + 2
    NK = NOWN + NOTH
    NW = NOWN + 2
    NQ = NOWN * 128
    nc = bass.Bass("TRN2", target_bir_lowering=False)
    S = Sched()
    es = ExitStack()

    def din(name, shape, dt=F32):
        return nc.dram_tensor(name, list(shape), dt, kind="ExternalInput").ap()

    def dscr(name, shape, dt):
        return nc.dram_tensor(name, list(shape), dt, kind="Internal").ap()

    def dout(name, shape, dt=F32):
        return nc.dram_tensor(name, list(shape), dt, kind="ExternalOutput").ap()

    xr = din("xr", [NJ * 128, 1024])
    posr = din("posr", [128, NJ], I32)
    w_in = din("w_in", [1024, 1824])
    w_q_b = din("w_q_b", [768, 768])
    w_kv_b = din("w_kv_b", [256, 1024])
    w_out = din("w_out", [1024, 1024])
    w_router = din("w_router", [1024, 36])
    w_gate = din("w_gate", [32, 1024, 256])
    w_up = din("w_up", [32, 1024, 256])
    w_down = din("w_down", [32, 256, 1024])
    vecs = din("vecs", [1, NVEC])
    cst_f = din("cst_f", [128, 128])
    cst_b = din("cst_b", [128, 128 + 4 * 512], BF16)
    y_out = dout("y", [NQ, 1024])

    KnT_s = dscr("KnT_s", [512, NK * 128], BF16)
    KrT_s = dscr("KrT_s", [32, NK * 128], BF16)
    V_s = dscr("V_s", [8, 128, NK * 72], BF16)
    h_s = dscr("h_s", [NQ, 1024], F32)
    mixa_s = dscr("mixa_s", [NQ, 512], BF16)
    outb_s = dscr("outb_s", [NQ, 512], F32)
    QT_s = dscr("QT_s", [97, 8, NQ], BF16)
    tk_QT = Tok("QT_s")
    tk_h = Tok("h_s")
    tk_mixa = Tok("mixa_s")
    tk_outb = Tok("outb_s")
    tk_KnT = Tok("KnT_s")
    tk_KrT = Tok("KrT_s")
    tk_V = Tok("V_s")

    dbg_outs = {}

    def sb(name, shape, dt, stack=None):
        h = (stack or es).enter_context(nc.sbuf_tensor(name, list(shape), dt))
        return TT(h, name)

    def OP(eng, meth, R, W, *a, **kw):
        return S.add(eng, lambda e: getattr(e, meth)(*a, **kw), [x.t if isinstance(x, TT) else x for x in R],
                     [x.t if isinstance(x, TT) else x for x in W])

    def DMA(q, out, in_, R, W):
        return S.add(q, lambda e: e.dma_start(out=out, in_=in_), [x.t if isinstance(x, TT) else x for x in R],
                     [x.t if isinstance(x, TT) else x for x in W], dma=True)

    def MM(out, lhsT, rhs, start, stop, R, W):
        return OP("pe", "matmul", R, W, out, lhsT, rhs, start=start, stop=stop)

    def TR(out, in_, ident, R, W):
        return OP("pe", "transpose", R, W, out, in_, ident)

    def ACT(out, in_, func, R, W, **kw):
        return OP("act", "activation", R, W, out, in_, func, **kw)

    def TTo(eng, out, in0, in1, op, R, W):
        return OP(eng, "tensor_tensor", R, W, out, in0, in1, op)

    def TSo(eng, out, in0, s1, s2, op0, op1, R, W):
        if op1 is None:
            return OP(eng, "tensor_scalar", R, W, out, in0, s1, None, op0)
        return OP(eng, "tensor_scalar", R, W, out, in0, s1, s2, op0, op1)

    def STT(eng, out, in0, sc, in1, op0, op1, R, W):
        return OP(eng, "scalar_tensor_tensor", R, W, out, in0, sc, in1, op0, op1)

    def CP(eng, out, in_, R, W):
        if eng == "act":
            return OP("act", "copy", R, W, out, in_)
        return OP(eng, "tensor_copy", R, W, out, in_)

    EPSC = {}

    def RSQ(dst, src, addc, R, W):
        col = EPSC[addc]
        ACT(dst, src, AF.Ln, list(R) + [epst], W, bias=epst[:, col:col + 1])
        ACT(dst, dst, AF.Exp, W, W, scale=-0.5)

    def RED(out, in_, op, R, W, axis=AX.X):
        return OP("dve", "tensor_reduce", R, W, out, in_, axis, op)

    PB = []
    PP = []
    for b in range(4):
        h = es.enter_context(nc.psum_tensor("pp%d" % b, [128, 1024], F32))
        PP.append(h)
        for half in range(2):
            PB.append(TT(h[:, half * 512:(half + 1) * 512], "pb%d" % (2 * b + half)))
            PB[-1].t.x = True

    def pbf(b):
        return PB[b].ap.bitcast(BF16)

    identf = sb("identf", [128, 128], F32)
    cstb = sb("cstb", [128, 128 + 2048], BF16)
    onesf = sb("onesf", [128, 128], F32)
    DMA("sp", identf.ap, cst_f, [], [identf])
    DMA("sp", cstb.ap, cst_b, [], [cstb])
    OP("pool", "memset", [], [onesf], onesf.ap, 1.0)
    identb = cstb[:, 0:128]
    epst = sb("epst", [128, 4], F32)
    for ci, ev in enumerate([LN_EPS, 256.0 * RMS_EPS, 768.0 * RMS_EPS, 512.0 * RMS_EPS]):
        EPSC[ev] = ci
        OP("pool", "memset", [], [epst], epst[:, ci:ci + 1], ev)

    def maskap(i):
        return cstb[:, 128 + i * 512:128 + (i + 1) * 512]

    def vload(dst, name, eng="sp"):
        o, l = VOFF[name]
        DMA(eng, dst.ap, vecs[0:1, o:o + l].to_broadcast([128, l]), [], [dst])

    esT = ExitStack()
    qn2b = sb("qn2b", [128, NOWN, 8], F32, esT)
    kmax2 = sb("kmax2", [128, 8], F32, esT)

    esA = ExitStack()
    A = lambda name, shape, dt: sb(name, shape, dt, esA)
    winb = A("winb", [128, 8, 1824], BF16)
    wqb = A("wqb", [128, 6, 768], BF16)
    wkvb = A("wkvb", [128, 2, 1024], BF16)
    wkb = A("wkb", [128, 2, 512], BF16)
    G_emb = A("G_emb", [128, 1024], F32)
    B_emb = A("B_emb", [128, 1024], F32)
    Gq = A("Gq", [128, 768], F32)
    Gkv = A("Gkv", [128, 256], F32)
    Ga = A("Ga", [128, 512], F32)
    sinkB = A("sinkB", [128, 8], F32)
    invA = A("invA", [128, 32], F32)
    invB = A("invB", [128, 16], F32)
    posi = A("posi", [128, NJ], I32)
    posf = A("posf", [128, NJ], F32)
    tabB = A("tabB", [128, NJ, 2, 16], F32)
    tabA = A("tabA", [128, NW, 2, 32], F32)
    esP = ExitStack()
    tabk = sb("tabk", [128, max(NJ * 32, NW * 64)], F32, esP)
    stg = [sb("stg%d" % i, [128, 1824], F32, esP) for i in range(2)]

    vload(G_emb, "ln_emb_g")
    vload(B_emb, "ln_emb_b")
    vload(Gq, "q_g")
    vload(Gkv, "kv_g")
    vload(Ga, "oa_g")
    vload(sinkB, "sink")
    vload(invA, "invA")
    vload(invB, "invB")
    DMA("sp", posi.ap, posr, [], [posi])
    TSo("dve", Gq.ap, Gq.ap, math.sqrt(768.0), None, ALU.mult, None, [Gq], [Gq])
    TSo("dve", Gkv.ap, Gkv.ap, 16.0, None, ALU.mult, None, [Gkv], [Gkv])
    TSo("dve", Ga.ap, Ga.ap, math.sqrt(512.0), None, ALU.mult, None, [Ga], [Ga])
    CP("dve", posf.ap, posi.ap, [posi], [posf])

    def make_table(tab, n, half, inv):
        tv = tab.ap
        kv = tabk[:, 0:n * 2 * half].rearrange("p (n t f) -> p n t f", t=2, f=half)
        pb_ = posf[:, 0:n].unsqueeze(2).to_broadcast([128, n, half])
        ib_ = inv.ap.unsqueeze(1).to_broadcast([128, n, half])
        TTo("dve", tv[:, :, 0, :], pb_, ib_, ALU.mult, [posf, inv], [tab])
        TSo("dve", tv[:, :, 1, :], tv[:, :, 0, :], math.pi / 2, None, ALU.add, None, [tab], [tab])
        TSo("dve", kv, tv, 1.0 / TWO_PI, MAGIC, ALU.mult, ALU.add, [tab], [tabk])
        TSo("dve", kv, kv, -MAGIC, None, ALU.add, None, [tabk], [tabk])
        STT("dve", tv, kv, -C1, tv, ALU.mult, ALU.add, [tabk, tab], [tab])
        STT("dve", tv, kv, -C2, tv, ALU.mult, ALU.add, [tabk, tab], [tab])
        STT("dve", tv, kv, -C3, tv, ALU.mult, ALU.add, [tabk, tab], [tab])
        TSo("dve", tv, tv, PI_LIM, -PI_LIM, ALU.min, ALU.max, [tab], [tab])
        ACT(tv, tv, AF.Sin, [tab], [tab])

    import os
    SKIP = os.environ.get("SKIP", "").split(",")
    if "tables" not in SKIP:
        make_table(tabB, NJ, 16, invB)
        make_table(tabA, NW, 32, invA)

    ncast = [0]

    def cast(out, in_, R, W, scale=None):
        eng = ("dve", "pool")[ncast[0] % 2]
        ncast[0] += 1
        if scale is None:
            CP(eng, out, in_, R, W)
        else:
            TSo(eng, out, in_, scale, None, ALU.mult, None, R, W)

    si = 0
    for c in range(8 if "weights" not in SKIP else 0):
        st_ = stg[si % 2]
        si += 1
        DMA("sp", st_[:, 0:1824], w_in[c * 128:(c + 1) * 128, :], [], [st_])
        cast(winb[:, c, 0:512], st_[:, 0:512], [st_], [winb], scale=0.125)
        cast(winb[:, c, 512:1824], st_[:, 512:1824], [st_], [winb])
    for c in range(6 if "weights" not in SKIP else 0):
        st_ = stg[si % 2]
        si += 1
        DMA("sp", st_[:, 0:768], w_q_b[c * 128:(c + 1) * 128, :], [], [st_])
        cast(wqb[:, c, :], st_[:, 0:768], [st_], [wqb], scale=96.0 ** -0.5)
    for c in range(2 if "weights" not in SKIP else 0):
        st_ = stg[si % 2]
        si += 1
        DMA("sp", st_[:, 0:1024], w_kv_b[c * 128:(c + 1) * 128, :], [], [st_])
        cast(wkvb[:, c, :], st_[:, 0:1024], [st_], [wkvb])
        cast(wkb[:, c, :].rearrange("p (h d) -> p h d", d=64),
             st_[:, 0:1024].rearrange("p (h x) -> p h x", x=128)[:, :, 0:64], [st_], [wkb])

    STOPAT = os.environ.get("STOPAT", "")
    if STOPAT == "P":
        d_tabB = dout("d_tabB", [128, NJ * 32])
        DMA("sp", d_tabB, tabB.ap.rearrange("p n t f -> p (n t f)"), [tabB], [Tok()])
        S.barrier()
        S.emit(nc, es, final=True)
        return nc
    S.barrier()
    S.emit(nc, es)
    esP.close()
    xt = [A("xt%d" % i, [128, 1024], F32) for i in range(2)]
    hf = [A("hf%d" % i, [128, 1024], F32) for i in range(2)]
    hb = [A("hb%d" % i, [128, 1024], BF16) for i in range(2)]
    hT = [A("hT%d" % i, [128, 8, 128], BF16) for i in range(2)]
    stt = [A("stt%d" % i, [128, 16], F32) for i in range(2)]
    kvb = [A("kvb%d" % i, [128, 288], BF16) for i in range(2)]
    sml = [A("sml%d" % i, [128, 32], F32) for i in range(2)]
    latT = [A("latT%d" % i, [128, 2, 128], BF16) for i in range(2)]
    KTbuf = [A("KTbuf%d" % i, [128, 4, 512], BF16) for i in range(2)]
    KrTbuf = [A("KrTbuf%d" % i, [32, 512], BF16) for i in range(2)]
    Vbuf = [A("Vbuf%d" % i, [128, 8, 4, 72], BF16) for i in range(2)]
    kn2 = A("kn2", [128, 8], F32)
    cqn = A("cqn", [128, 768], BF16)
    cqT = A("cqT", [128, 6, 128], BF16)
    qtok = A("qtok", [128, 8, 96], BF16)
    qst = [A("qst%d" % i, [96, 8, 128], BF16) for i in range(2)]
    KaT = A("KaT", [65, 2, 8 * 128], BF16)
    Va = A("Va", [128, 8, 2, 72], BF16)
    kmaxa = A("kmaxa", [128, NW, 2], F32)
    katok = A("katok", [128, 128], BF16)
    qatok = [A("qatok%d" % i, [128, 8, 65], BF16) for i in range(6)]
    qn2a = [A("qn2a%d" % i, [128, 8], F32) for i in range(6)]
    PTa = [A("PTa%d" % i, [128, 512], BF16) for i in range(2)]
    Osb = A("Osb", [65, 512], F32)
    Osb_default = Osb
    mixa = [A("mixa%d" % i, [128, 512], BF16) for i in range(2)]
    wsm = A("wsm", [128, 64], F32)
    wsm_default = wsm

    for v in Vbuf:
        OP("pool", "memset", [], [v], v.ap, 0.0)
        OP("pool", "memset", [], [v], v[:, :, :, 64:65], 1.0)
    OP("pool", "memset", [], [Va], Va.ap, 0.0)
    OP("pool", "memset", [], [Va], Va[:, :, :, 64:65], 1.0)
    OP("pool", "memset", [], [KaT], KaT[64:65, :, :], 1.0)
    OP("pool", "memset", [], [kmax2], kmax2.ap, 0.0)

    def rope(src, H, half, cos, sin, dst, R, W, tmps=None, scl=None):
        n = H * 2 * half
        tA_, tB_ = tmps
        tA = tA_[:, 0:n].rearrange("p (h t f) -> p h t f", t=2, f=half)
        tB = tB_[:, 0:n].rearrange("p (h t f) -> p h t f", t=2, f=half)
        cb = cos.unsqueeze(1).unsqueeze(1).to_broadcast([128, H, 2, half])
        sb_ = sin.unsqueeze(1).to_broadcast([128, H, half])
        if scl is None:
            TTo("dve", tA, src, cb, ALU.mult, R, [tA_])
            TTo("dve", tB[:, :, 0, :], src[:, :, 1, :], sb_, ALU.mult, R, [tB_])
            TTo("dve", tB[:, :, 1, :], src[:, :, 0, :], sb_, ALU.mult, R, [tB_])
        else:
            cb3 = cos.unsqueeze(1).to_broadcast([128, H, half])
            STT("dve", tA[:, :, 0, :], src[:, :, 0, :], scl, cb3, ALU.mult, ALU.mult, R, [tA_])
            STT("dve", tA[:, :, 1, :], src[:, :, 1, :], scl, cb3, ALU.mult, ALU.mult, R, [tA_])
            STT("dve", tB[:, :, 0, :], src[:, :, 1, :], scl, sb_, ALU.mult, ALU.mult, R, [tB_])
            STT("dve", tB[:, :, 1, :], src[:, :, 0, :], scl, sb_, ALU.mult, ALU.mult, R, [tB_])
        TTo("dve", dst[:, :, 0, :], tA[:, :, 0, :], tB[:, :, 0, :], ALU.subtract, [tA_, tB_], W)
        TTo("dve", dst[:, :, 1, :], tA[:, :, 1, :], tB[:, :, 1, :], ALU.add, [tA_, tB_], W)

    def pmax_bcast(src_ap, src_tok, ncol, dst_ap, dst_tok, bank, wsm=None):
        if wsm is None:
            wsm = wsm_default
        pb_ = PB[bank]
        TR(pb_[0:ncol, 0:128], src_ap, identf.ap, [src_tok, identf], [pb_])
        RED(wsm[0:ncol, 0:1], pb_[0:ncol, 0:128], ALU.max, [pb_], [wsm])
        TSo("dve", wsm[0:ncol, 8:8 + ncol], identf[0:ncol, 0:ncol], wsm[0:ncol, 0:1], None, ALU.mult, None,
            [wsm, identf], [wsm])
        MM(pb_[:, 256:256 + ncol], onesf[0:ncol, :], wsm[0:ncol, 8:8 + ncol], True, True, [onesf, wsm], [pb_])
        CP("dve", dst_ap, pb_[:, 256:256 + ncol], [pb_], [dst_tok])

    evi = [0]

    def evac(out, in_, R, W):
        eng = ("act", "dve")[evi[0] % 2]
        evi[0] += 1
        CP(eng, out, in_, R, W)

    wsm9 = [A("wsm9_%d" % i, [128, 64], F32) for i in range(2)]
    QaT2 = [A("QaT2_%d" % i, [65, 1024], BF16) for i in range(2)]
    oa2 = [A("oa2_%d" % i, [128, 8, 64], F32) for i in range(2)]
    sq9 = A("sq9", [128, 512], F32)
    for w9 in wsm9:
        OP("pool", "memset", [], [w9], w9.ap, 0.0)

    def wa_a(n):
        w = n + 1
        sq_ = n % 6
        qa_ = qatok[sq_]
        wsm = wsm9[n % 2]
        TTo("dve", wsm[:, 16:18], kmaxa[:, w - 1, :], kmaxa[:, w, :], ALU.max, [kmaxa], [wsm])
        TTo("dve", wsm[:, 16:18], wsm[:, 16:18], kmaxa[:, w + 1, :], ALU.max, [kmaxa, wsm], [wsm])
        TTo("dve", wsm[:, 20:28].rearrange("p (g f) -> p g f", f=4),
            qn2a[sq_].ap.rearrange("p (g f) -> p g f", f=4),
            wsm[:, 16:18].unsqueeze(2).to_broadcast([128, 2, 4]), ALU.mult, [qn2a[sq_], wsm], [wsm])
        ACT(wsm[:, 20:28], wsm[:, 20:28], AF.Ln, [wsm], [wsm])
        ACT(wsm[:, 20:28], wsm[:, 20:28], AF.Exp, [wsm], [wsm], scale=0.5)
        TSo("dve", qa_[:, :, 64:65], wsm[:, 20:28].unsqueeze(2), -1.0, None, ALU.mult, None, [wsm], [qa_])
        TTo("dve", wsm[:, 28:36].unsqueeze(2), qa_[:, :, 64:65], sinkB.ap.unsqueeze(2), ALU.add, [qa_, sinkB], [wsm])
        ACT(wsm[:, 36:44], wsm[:, 28:36], AF.Exp, [wsm], [wsm])
        for h in range(8):
            TR(pbf(0)[0:65, h * 128:(h + 1) * 128], qa_[:, h, 0:65], identb, [qa_, cstb], [PB[0]])
        evac(QaT2[n % 2][:, :], pbf(0)[0:65, :], [PB[0]], [QaT2[n % 2]])

    PTa2 = [A("PTa2_%d" % i, [128, 512], BF16) for i in range(2)]
    Osb2 = A("Osb2", [65, 512], F32)

    def capture(fn):
        rec = []
        orig = S.add
        S.add = lambda *a, **kw: rec.append((a, kw))
        try:
            fn()
        finally:
            S.add = orig
        return rec

    def interleave2(fa, fb):
        ra = capture(fa) if fa is not None else []
        rb = capture(fb) if fb is not None else []
        i = 0
        while i < len(ra) or i < len(rb):
            if i < len(ra):
                S.add(*ra[i][0], **ra[i][1])
            if i < len(rb):
                S.add(*rb[i][0], **rb[i][1])
            i += 1

    def wa_g(n, g, bset=0):
        w = n + 1
        wsm = wsm9[n % 2]
        QaT = QaT2[n % 2]
        oa = oa2[n % 2]
        first_tile = (n == 0)
        last_tile = (n == NOWN - 1)
        if bset == 0:
            bo, bsl, bt, PTx, Osb = PB[5], (PB[6], PB[7]), PB[4], PTa, Osb_default
        else:
            bo, bsl, bt, PTx, Osb = PB[1], (PB[2], PB[3]), PB[0], PTa2, Osb2
        kts = [w - 1, w, w + 1]
        for ki, kt in enumerate(kts):
            bs = bsl[ki % 2]
            pt = PTx[ki % 2]
            MM(bs.ap, KaT[0:65, g, (kt % 8) * 128:(kt % 8 + 1) * 128], QaT[:, g * 512:(g + 1) * 512], True, kt == w,
               [KaT, QaT], [bs])
            if kt != w:
                if kt < w:
                    mi = 2 if first_tile else 0
                else:
                    mi = 3 if last_tile else 1
                MM(bs.ap, identb, maskap(mi), False, True, [cstb], [bs])
            ACT(pt.ap, bs.ap, AF.Exp, [bs], [pt])
            MM(bo[0:65, :], Va[:, kt % 8, g, 0:65], pt.ap, ki == 0, ki == 2, [Va, pt], [bo])
        CP("dve", Osb.ap, bo[0:65, :], [bo], [Osb])
        btv = bt[:, 0:260].rearrange("p (i x) -> p i x", x=65)
        for i in range(4):
            TR(bt[:, i * 65:(i + 1) * 65], Osb[:, i * 128:(i + 1) * 128], identf[0:65, 0:65], [Osb, identf], [bt])
        TTo("dve", wsm[:, 44:48].unsqueeze(2), btv[:, :, 64:65], wsm[:, 36 + g * 4:40 + g * 4].unsqueeze(2),
            ALU.add, [bt, wsm], [wsm])
        OP("dve", "reciprocal", [wsm], [wsm], wsm[:, 48:52], wsm[:, 44:48])
        TTo("dve", oa[:, g * 4:(g + 1) * 4, :], btv[:, :, 0:64],
            wsm[:, 48:52].unsqueeze(2).to_broadcast([128, 4, 64]), ALU.mult, [bt, wsm], [oa])

    def wa_pair(j):
        fa = (lambda: wa_g(j - 2, 1, 0)) if has_wa(j) else None
        fb = (lambda: wa_g(j - 1, 0, 1)) if has_wa(j + 1) else None
        interleave2(fa, fb)

    def wa_d(n):
        wsm = wsm9[n % 2]
        oa = oa2[n % 2]
        oaf = oa.ap.rearrange("p h d -> p (h d)")
        ACT(sq9.ap, oaf, AF.Square, [oa], [sq9])
        RED(wsm[:, 52:53], sq9.ap, ALU.add, [sq9], [wsm])
        RSQ(wsm[:, 53:54], wsm[:, 52:53], 512.0 * RMS_EPS, [wsm], [wsm])
        mx = mixa[n % 2]
        STT("dve", mx.ap, oaf, wsm[:, 53:54], Ga.ap, ALU.mult, ALU.mult, [oa, wsm, Ga], [mx])
        DMA("sp", mixa_s[n * 128:(n + 1) * 128, :], mx.ap, [mx], [tk_mixa])

    sq3 = [A("sq3_%d" % i, [128, 288], F32) for i in range(2)]
    sq5 = A("sq5", [128, 512], F32)
    sq7 = A("sq7", [128, 128], F32)
    sq8 = A("sq8", [128, 768], F32)
    rt3 = [(A("rt3a%d" % i, [128, 32], F32), A("rt3b%d" % i, [128, 32], F32)) for i in range(2)]
    rt7 = (A("rt7a", [128, 128], F32), A("rt7b", [128, 128], F32))
    rt8a = (A("rt8aa", [128, 512], F32), A("rt8ab", [128, 512], F32))
    rt8d = (A("rt8da", [128, 128], F32), A("rt8db", [128, 128], F32))
    sm8s = [A("sm8_%d" % i, [128, 8], F32) for i in range(2)]
    kvf = [A("kvf%d" % i, [128, 288], F32) for i in range(4)]
    xn = [A("xn%d" % i, [128, 1024], F32) for i in range(2)]
    kr2all = A("kr2all", [128, NJ], F32)
    ssqkv = A("ssqkv", [128, NJ], F32)
    wsm7 = A("wsm7", [128, 64], F32)
    keyidx = {}
    _kt = 0
    for j in range(NJ):
        if j not in (0, NOWN + 1):
            keyidx[j] = _kt
            _kt += 1

    def proj(hT_, bank, ncols, col0):
        for c in range(8):
            MM(PB[bank][:, 0:ncols], hT_[:, c, :], winb[:, c, col0:col0 + ncols], c == 0, c == 7,
               [hT_, winb], [PB[bank]])

    is_key_ = lambda j: j not in (0, NOWN + 1)
    is_win_ = lambda j: j <= NOWN + 1
    is_own_ = lambda j: 1 <= j <= NOWN

    def sA0(j):
        s = j % 2
        x_, st_ = xt[s], stt[s]
        if j == 0:
            DMA("pool", x_.ap, xr[0:128, :], [], [x_])
        if j + 1 < NJ:
            DMA("pool", xt[(j + 1) % 2].ap, xr[(j + 1) * 128:(j + 2) * 128, :], [], [xt[(j + 1) % 2]])
        OP("dve", "bn_stats", [x_], [st_], st_[:, 0:6], x_[:, 0:512])
        OP("dve", "bn_stats", [x_], [st_], st_[:, 6:12], x_[:, 512:1024])
        OP("dve", "bn_aggr", [st_], [st_], st_[:, 12:14], st_[:, 0:12])
        TSo("dve", st_[:, 14:15], st_[:, 13:14], LN_EPS, None, ALU.add, None, [st_], [st_])

    def sA1(j):
        s = j % 2
        x_, st_, xn_ = xt[s], stt[s], xn[s]
        ACT(st_[:, 14:15], st_[:, 14:15], AF.Ln, [st_], [st_])
        ACT(st_[:, 14:15], st_[:, 14:15], AF.Exp, [st_], [st_], scale=-0.5)
        ACT(st_[:, 15:16], st_[:, 12:13], AF.Identity, [st_], [st_], scale=st_[:, 14:15])
        OP("act", "mul", [st_], [st_], st_[:, 15:16], st_[:, 15:16], -1.0)
        ACT(xn_.ap, x_.ap, AF.Identity, [x_, st_], [xn_], bias=st_[:, 15:16], scale=st_[:, 14:15])

    def sA2(j):
        xn_ = xn[j % 2]
        TTo("pool", xn_.ap, xn_.ap, G_emb.ap, ALU.mult, [xn_, G_emb], [xn_])

    def sA3(j):
        s = j % 2
        xn_, hf_, hb_ = xn[s], hf[s], hb[s]
        if is_own_(j):
            TTo("dve", hf_.ap, xn_.ap, B_emb.ap, ALU.add, [xn_, B_emb], [hf_])
        else:
            TTo("pool", hb_.ap, xn_.ap, B_emb.ap, ALU.add, [xn_, B_emb], [hb_])

    def sA4(j):
        s = j % 2
        hf_, hb_ = hf[s], hb[s]
        DMA("sp", h_s[(j - 1) * 128:j * 128, :], hf_.ap, [hf_], [tk_h])
        CP("act", hb_.ap, hf_.ap, [hf_], [hb_])

    def sA5(j):
        hb_ = hb[j % 2]
        for c in range(8):
            TR(pbf(0)[:, c * 128:(c + 1) * 128], hb_[:, c * 128:(c + 1) * 128], identb, [hb_, cstb], [PB[0]])

    def sA6(j):
        CP(("act", "dve")[j % 2], hT[j % 2].ap, pbf(0).rearrange("p (c t) -> p c t", t=128), [PB[0]], [hT[j % 2]])

    def sA7(j):
        proj(hT[j % 2], 1, 288, 1536)

    def sA8(j):
        kf = kvf[j % 4]
        p1 = PB[1]
        CP("act", kf.ap, p1[:, 0:288], [p1], [kf])
        ACT(sq3[j % 2].ap, kf.ap, AF.Square, [kf], [sq3[j % 2]])

    def sA9(j):
        kf = kvf[j % 4]
        n = 32
        tA_, tB_ = rt3[j % 2]
        src = kf[:, 256:288].rearrange("p (h t f) -> p h t f", h=1, t=2)
        tA = tA_[:, 0:n].rearrange("p (h t f) -> p h t f", t=2, f=16)
        tB = tB_[:, 0:n].rearrange("p (h t f) -> p h t f", t=2, f=16)
        cos, sin = tabB[:, j, 1, :], tabB[:, j, 0, :]
        cb = cos.unsqueeze(1).unsqueeze(1).to_broadcast([128, 1, 2, 16])
        sb_ = sin.unsqueeze(1).to_broadcast([128, 1, 16])
        TTo("dve", tA, src, cb, ALU.mult, [kf, tabB], [tA_])
        TTo("dve", tB[:, :, 0, :], src[:, :, 1, :], sb_, ALU.mult, [kf, tabB], [tB_])
        TTo("dve", tB[:, :, 1, :], src[:, :, 0, :], sb_, ALU.mult, [kf, tabB], [tB_])
        RED(ssqkv[:, j:j + 1], sq3[j % 2][:, 0:256], ALU.add, [sq3[j % 2]], [ssqkv])
        RED(kr2all[:, j:j + 1], sq3[j % 2][:, 256:288], ALU.add, [sq3[j % 2]], [kr2all])
        TSo("dve", ssqkv[:, j:j + 1], ssqkv[:, j:j + 1], 256.0 * RMS_EPS, None, ALU.add, None, [ssqkv], [ssqkv])

    def sA10(j):
        kv_ = kvb[j % 2]
        tA_, tB_ = rt3[j % 2]
        tA = tA_[:, 0:32].rearrange("p (h t f) -> p h t f", t=2, f=16)
        tB = tB_[:, 0:32].rearrange("p (h t f) -> p h t f", t=2, f=16)
        dst = kv_[:, 256:288].rearrange("p (h t f) -> p h t f", h=1, t=2)
        ACT(ssqkv[:, j:j + 1], ssqkv[:, j:j + 1], AF.Ln, [ssqkv], [ssqkv])
        ACT(ssqkv[:, j:j + 1], ssqkv[:, j:j + 1], AF.Exp, [ssqkv], [ssqkv], scale=-0.5)
        TTo("pool", dst[:, :, 0, :], tA[:, :, 0, :], tB[:, :, 0, :], ALU.subtract, [tA_, tB_], [kv_])
        TTo("pool", dst[:, :, 1, :], tA[:, :, 1, :], tB[:, :, 1, :], ALU.add, [tA_, tB_], [kv_])

    def sA11(j):
        kf, kv_ = kvf[j % 4], kvb[j % 2]
        STT("dve", kv_[:, 0:256], kf[:, 0:256], ssqkv[:, j:j + 1], Gkv.ap, ALU.mult, ALU.mult, [kf, ssqkv, Gkv], [kv_])

    def sA12(j):
        kv_ = kvb[j % 2]
        for c in range(2):
            TR(pbf(2)[:, c * 128:(c + 1) * 128], kv_[:, c * 128:(c + 1) * 128], identb, [kv_, cstb], [PB[2]])
        TR(pbf(2)[0:32, 256:384], kv_[:, 256:288], identb, [kv_, cstb], [PB[2]])

    def sA13(j):
        kt = keyidx[j]
        g, q = (kt // 4) % 2, kt % 4
        lt_ = latT[j % 2]
        CP("act", lt_.ap, pbf(2)[:, 0:256].rearrange("p (c t) -> p c t", t=128), [PB[2]], [lt_])
        CP("act", KrTbuf[g][:, q * 128:(q + 1) * 128], pbf(2)[0:32, 256:384], [PB[2]], [KrTbuf[g]])

    def sA14(j):
        lt_ = latT[j % 2]
        for half in range(2):
            for c in range(2):
                MM(PB[3 + half].ap, lt_[:, c, :], wkvb[:, c, half * 512:(half + 1) * 512], c == 0, c == 1,
                   [lt_, wkvb], [PB[3 + half]])
        p5 = PB[5]
        for pair in range(4):
            for c in range(2):
                MM(p5[:, pair * 128:(pair + 1) * 128], wkb[:, c, pair * 128:(pair + 1) * 128], lt_[:, c, :],
                   c == 0, c == 1, [wkb, lt_], [p5])

    def sA15(j):
        kt = keyidx[j]
        g, q = (kt // 4) % 2, kt % 4
        for half in range(2):
            pv = PB[3 + half].ap.rearrange("p (h x) -> p h x", x=128)
            CP("act", Vbuf[g][:, half * 4:(half + 1) * 4, q, 0:64], pv[:, :, 64:128], [PB[3 + half]], [Vbuf[g]])
            ACT(sq5.ap.rearrange("p (h d) -> p h d", d=64)[:, half * 4:(half + 1) * 4, :], pv[:, :, 0:64],
                AF.Square, [PB[3 + half]], [sq5])
        p5 = PB[5]
        CP(("dve", "act")[j % 2], KTbuf[g][:, :, q * 128:(q + 1) * 128], p5.ap.rearrange("p (a t) -> p a t", t=128),
           [p5], [KTbuf[g]])

    def sA16(j):
        kt = keyidx[j]
        g, q = (kt // 4) % 2, kt % 4
        RED(kn2.ap, sq5.ap.rearrange("p (h d) -> p h d", d=64), ALU.add, [sq5], [kn2])
        STT("dve", kmax2.ap, kn2.ap, kr2all[:, j:j + 1], kmax2.ap, ALU.add, ALU.max, [kn2, kr2all, kmax2], [kmax2])
        if q == 3 or kt == NK - 1:
            kt0 = kt - q
            nq = q + 1
            DMA("sp", KnT_s.rearrange("(a p) t -> p a t", p=128)[:, :, kt0 * 128:(kt0 + nq) * 128],
                KTbuf[g][:, :, 0:nq * 128], [KTbuf[g]], [tk_KnT])
            DMA("sp", KrT_s[:, kt0 * 128:(kt0 + nq) * 128], KrTbuf[g][:, 0:nq * 128], [KrTbuf[g]], [tk_KrT])
            DMA("sp", V_s.rearrange("h p f -> p h f")[:, :, kt0 * 72:(kt0 + nq) * 72],
                Vbuf[g][:, :, 0:nq, :].rearrange("p h q x -> p h (q x)"), [Vbuf[g]], [tk_V])

    def st7(j):
        s = j % 2
        w = j
        hT_ = hT[s]
        proj(hT_, 6, 256, 512)
        p6 = PB[6]
        rope(p6[:, 0:128].rearrange("p (h t f) -> p h t f", h=2, t=2), 2, 32, tabA[:, w, 1, :], tabA[:, w, 0, :],
             katok.ap.rearrange("p (h t f) -> p h t f", h=2, t=2), [p6, tabA], [katok], rt7)
        ACT(sq7.ap, p6[:, 0:128], AF.Square, [p6], [sq7])
        CP("act", Va[:, w % 8, :, 0:64], p6[:, 128:256].rearrange("p (h d) -> p h d", d=64), [p6], [Va])
        RED(wsm7[:, 56:58], sq7.ap.rearrange("p (h d) -> p h d", d=64), ALU.add, [sq7], [wsm7])
        pmax_bcast(wsm7[:, 56:58], wsm7.t, 2, kmaxa[:, w, :], kmaxa.t, 7, wsm7)
        for g_ in range(2):
            TR(pbf(2)[0:64, g_ * 128:(g_ + 1) * 128], katok[:, g_ * 64:(g_ + 1) * 64], identb, [katok, cstb], [PB[2]])
        evac(KaT[0:64, :, (w % 8) * 128:(w % 8 + 1) * 128], pbf(2)[0:64, 0:256].rearrange("p (g t) -> p g t", t=128),
             [PB[2]], [KaT])

    def st8a(j):
        s = j % 2
        n = j - 1
        sq_ = n % 6
        proj(hT[s], 3, 512, 0)
        p7 = PB[3]
        rope(p7.ap.rearrange("p (h t f) -> p h t f", h=8, t=2), 8, 32, tabA[:, j, 1, :], tabA[:, j, 0, :],
             qatok[sq_][:, :, 0:64].rearrange("p h (t f) -> p h t f", t=2), [p7, tabA], [qatok[sq_]], rt8a)
        ACT(sq8[:, 0:512], p7.ap, AF.Square, [p7], [sq8])
        RED(qn2a[sq_].ap, sq8[:, 0:512].rearrange("p (h d) -> p h d", d=64), ALU.add, [sq8], [qn2a[sq_]])

    def st8b(j):
        s = j % 2
        sm8 = sm8s[s]
        proj(hT[s], 6, 384, 768)
        proj(hT[s], 7, 384, 1152)
        ACT(sq8[:, 0:384], PB[6][:, 0:384], AF.Square, [PB[6]], [sq8])
        ACT(sq8[:, 384:768], PB[7][:, 0:384], AF.Square, [PB[7]], [sq8])
        STT("dve", cqn[:, 0:384], PB[6][:, 0:384], 1.0, Gq[:, 0:384], ALU.mult, ALU.mult, [PB[6], Gq], [cqn])
        STT("dve", cqn[:, 384:768], PB[7][:, 0:384], 1.0, Gq[:, 384:768], ALU.mult, ALU.mult, [PB[7], Gq], [cqn])
        RED(sm8[:, 4:5], sq8[:, 0:768], ALU.add, [sq8], [sm8])
        RSQ(sm8[:, 5:6], sm8[:, 4:5], 768.0 * RMS_EPS, [sm8], [sm8])

    def st8c(j):
        for c in range(6):
            TR(pbf(0)[:, c * 128:(c + 1) * 128], cqn[:, c * 128:(c + 1) * 128], identb, [cqn, cstb], [PB[0]])
        evac(cqT.ap, pbf(0)[:, 0:768].rearrange("p (c t) -> p c t", t=128), [PB[0]], [cqT])

    def st8d(j):
        n = j - 1
        sm8 = sm8s[j % 2]
        for half in range(2):
            for c in range(6):
                MM(PB[6 + half][:, 0:384], cqT[:, c, :], wqb[:, c, half * 384:(half + 1) * 384], c == 0, c == 5,
                   [cqT, wqb], [PB[6 + half]])
        for half in range(2):
            pq = PB[6 + half][:, 0:384].rearrange("p (h x) -> p h x", x=96)
            OP("act", "activation", [PB[6 + half], sm8], [qtok], qtok[:, half * 4:(half + 1) * 4, 0:64], pq[:, :, 0:64],
               AF.Identity, scale=sm8[:, 5:6])
            ACT(sq8[:, 0:768].rearrange("p (h x) -> p h x", x=96)[:, half * 4:(half + 1) * 4, :], pq, AF.Square,
                [PB[6 + half], sm8], [sq8], scale=sm8[:, 5:6])
            rope(pq[:, :, 64:96].rearrange("p h (t f) -> p h t f", t=2), 4, 16, tabB[:, j, 1, :], tabB[:, j, 0, :],
                 qtok[:, half * 4:(half + 1) * 4, 64:96].rearrange("p h (t f) -> p h t f", t=2),
                 [PB[6 + half], tabB, sm8], [qtok], rt8d, scl=sm8[:, 5:6])
        RED(qn2b[:, n, :], sq8[:, 0:768].rearrange("p (h x) -> p h x", x=96), ALU.add, [sq8], [qn2b])

    def st8e(j):
        n = j - 1
        for h in range(8):
            TR(pbf(0)[0:96, h * 128:(h + 1) * 128], qtok[:, h, :], identb, [qtok, cstb], [PB[0]])
        evac(qst[n % 2].ap, pbf(0)[0:96, :].rearrange("p (h t) -> p h t", t=128), [PB[0]], [qst[n % 2]])
        DMA("sp", QT_s[0:96, :, n * 128:(n + 1) * 128], qst[n % 2].ap, [qst[n % 2]], [tk_QT])

    has_wa = lambda j: 2 <= j <= NOWN + 1 and "wattn" not in SKIP
    def m2(f, g_):
        def h(j):
            f(j)
            g_(j)
        return h
    stages = [
        (0, sA0, lambda j: True),
        (1, sA1, lambda j: True),
        (2, sA2, lambda j: True),
        (3, sA3, lambda j: True),
        (4, sA4, is_own_),
        (5, m2(sA5, sA6), lambda j: True),
        (6, m2(sA7, sA8), is_key_),
        (6, lambda j: interleave2((lambda: st7(j)), (lambda: st8a(j)) if is_own_(j) else None), is_win_),
        (7, sA9, is_key_),
        (7, st8b, is_own_),
        (8, sA10, is_key_),
        (8, st8c, is_own_),
        (9, sA11, is_key_),
        (9, lambda j: interleave2((lambda: st8d(j)) if is_own_(j) else None,
                                  (lambda: wa_a(j - 2)) if has_wa(j) else None),
         lambda j: is_own_(j) or has_wa(j)),
        (10, m2(sA12, sA13), is_key_),
        (10, st8e, is_own_),
        (11, m2(sA14, sA15), is_key_),
        (11, wa_pair, lambda j: has_wa(j) or has_wa(j + 1)),
        (12, sA16, is_key_),
        (12, lambda j: wa_d(j - 2), has_wa),
    ]
    maxoff = max(o for o, _, _ in stages)
    for step in range(NJ + maxoff):
        for off, fn, pred in sorted(stages, key=lambda x: -x[0]):
            j = step - off
            if 0 <= j < NJ and pred(j):
                fn(j)
        if STOPAT == "S%d" % step:
            S.barrier()
            S.emit(nc, es, final=True)
            return nc


    if dbg:
        d_tabB = dout("d_tabB", [128, NJ * 32])
        DMA("sp", d_tabB, tabB.ap.rearrange("p n t f -> p (n t f)"), [tabB], [Tok()])
    S.barrier()
    S.emit(nc, es)
    esA.close()
    esX = ExitStack()
    nmT = sb("nmT", [8, NQ], BF16, esX)
    nm = sb("nm", [128, NOWN, 8], BF16, esX)
    wsm = sb("wsm2", [128, 64], F32, esX)
    sqs = sb("sqs2", [128, NOWN * 8], F32, esX)
    pmax_bcast(kmax2.ap, kmax2.t, 8, wsm[:, 56:64], wsm.t, 7, wsm)
    for n in range(NOWN):
        TTo("dve", sqs[:, n * 8:(n + 1) * 8], qn2b[:, n, :], wsm[:, 56:64], ALU.mult, [qn2b, wsm], [sqs])
    ACT(sqs[:, 0:NOWN * 8], sqs[:, 0:NOWN * 8], AF.Ln, [sqs], [sqs])
    ACT(sqs[:, 0:NOWN * 8], sqs[:, 0:NOWN * 8], AF.Exp, [sqs], [sqs], scale=0.5)
    TSo("dve", nm.ap.rearrange("p n h -> p (n h)"), sqs[:, 0:NOWN * 8], -1.0, None, ALU.mult, None, [sqs], [nm])
    for n0 in range(0, NOWN, 8):
        nn = min(8, NOWN - n0)
        for i in range(nn):
            TR(pbf(0)[0:8, i * 128:(i + 1) * 128], nm[:, n0 + i, :], identb, [nm, cstb], [PB[0]])
        evac(nmT[:, n0 * 128:(n0 + nn) * 128], pbf(0)[0:8, 0:nn * 128], [PB[0]], [nmT])
    DMA("sp", QT_s[96], nmT.ap, [nmT], [tk_QT])

    if dbg:
        d_QT = dout("d_QT", [97, 8 * NQ], BF16)
        DMA("sp", d_QT, QT_s.rearrange("p h n -> p (h n)"), [tk_QT], [Tok()])
        d_KnT = dout("d_KnT", [512, NK * 128], BF16)
        DMA("sp", d_KnT, KnT_s, [tk_KnT], [Tok()])
        d_KrT = dout("d_KrT", [32, NK * 128], BF16)
        DMA("sp", d_KrT, KrT_s, [tk_KrT], [Tok()])
        d_V = dout("d_V", [8 * 128, NK * 72], BF16)
        DMA("sp", d_V, V_s.rearrange("h p f -> (h p) f"), [tk_V], [Tok()])
        if "wattn" not in SKIP:
            d_mixa = dout("d_mixa", [NQ, 512], BF16)
            DMA("sp", d_mixa, mixa_s, [tk_mixa], [Tok()])
        d_h = dout("d_h", [NQ, 1024], F32)
        DMA("sp", d_h, h_s, [tk_h], [Tok()])

    S.barrier()
    S.emit(nc, es, final=(stage == "A"))
    esX.close()
    esT.close()
    print("phase A ops", len(S.ops), "standalone waits", S.nwait)

    if stage == "A":
        es.close()
        return nc

    esB = ExitStack()
    Bt = lambda name, shape, dt: sb(name, shape, dt, esB)
    KT = [Bt("KT%d" % i, [97, NK * 128], BF16) for i in range(2)]
    Vh = [Bt("Vh%d" % i, [128, NK, 72], BF16) for i in range(2)]
    QTh = [Bt("QTh%d" % i, [97, NQ], BF16) for i in range(2)]
    OsbB = [Bt("OsbB%d" % i, [65, 512], F32) for i in range(2)]
    obt = [Bt("obt%d" % i, [128, 4, 64], F32) for i in range(2)]
    wsB = [Bt("wsB%d" % i, [128, 8], F32) for i in range(2)]
    for kt_ in KT:
        OP("pool", "memset", [], [kt_], kt_[64:97, :], 1.0)
    NQB = NOWN // 4

    def load_head(h):
        hb_ = h % 2
        half = (NK * 128) // 2
        DMA("sp", KT[hb_][0:64, 0:half], KnT_s[h * 64:(h + 1) * 64, 0:half], [tk_KnT], [KT[hb_]])
        DMA("pool", KT[hb_][0:64, half:], KnT_s[h * 64:(h + 1) * 64, half:], [tk_KnT], [KT[hb_]])
        DMA("sp", KT[hb_][64:96, :], KrT_s, [tk_KrT], [KT[hb_]])
        DMA("pool", Vh[hb_].ap.rearrange("p c x -> p (c x)"), V_s[h], [tk_V], [Vh[hb_]])
        DMA("sp", QTh[hb_].ap, QT_s[:, h, :], [tk_QT], [QTh[hb_]])

    PT = [Bt("PT%d" % i, [128, 512], BF16) for i in range(4)]
    units = [(h, qb, c) for h in range(8) for qb in range(NQB) for c in range(NK)]
    NU = len(units)
    LA = 2
    pending = []

    def epilogue(h, qb, blk):
        ob_ = PB[4 + blk % 2]
        os_ = OsbB[blk % 2]
        bt = PB[6 + blk % 2]
        ot = obt[blk % 2]
        ws = wsB[blk % 2]
        CP("dve", os_.ap, ob_[0:65, :], [ob_], [os_])

        def part2():
            btv = bt[:, 0:260].rearrange("p (i x) -> p i x", x=65)
            for i in range(4):
                TR(bt[:, i * 65:(i + 1) * 65], os_[:, i * 128:(i + 1) * 128], identf[0:65, 0:65], [os_, identf], [bt])
            OP("dve", "reciprocal", [bt], [ws], ws[:, 0:4].unsqueeze(2), btv[:, :, 64:65])
            TTo("dve", ot.ap, btv[:, :, 0:64], ws[:, 0:4].unsqueeze(2).to_broadcast([128, 4, 64]), ALU.mult,
                [bt, ws], [ot])
            DMA("pool", outb_s[qb * 512:(qb + 1) * 512, h * 64:(h + 1) * 64].rearrange("(i p) d -> p i d", p=128),
                ot.ap, [ot], [tk_outb])
        return part2

    for i in range(NU + LA):
        while pending and pending[0][0] <= i:
            pending.pop(0)[1]()
        if i < NU:
            h, qb, c = units[i]
            if qb == 0 and c == 0 and h == 0:
                load_head(0)
            hb_ = h % 2
            bs = PB[i % 3]
            MM(bs.ap, KT[hb_][0:97, c * 128:(c + 1) * 128], QTh[hb_][0:97, qb * 512:(qb + 1) * 512], True, True,
               [KT[hb_], QTh[hb_]], [bs])
            ACT(PT[i % 4].ap, bs.ap, AF.Exp, [bs], [PT[i % 4]])
        ip = i - LA
        if ip >= 0:
            h, qb, c = units[ip]
            if qb == 0 and c == 0 and h + 1 < 8:
                load_head(h + 1)
            hb_ = h % 2
            blk = h * NQB + qb
            ob_ = PB[4 + blk % 2]
            MM(ob_[0:65, :], Vh[hb_][:, c, 0:65], PT[ip % 4].ap, c == 0, c == NK - 1, [Vh[hb_], PT[ip % 4]], [ob_])
            if c == NK - 1:
                pending.append((i + 6, epilogue(h, qb, blk)))
    for _, f in pending:
        f()

    if dbg:
        d_outb = dout("d_outb", [NQ, 512], F32)
        DMA("sp", d_outb, outb_s, [tk_outb], [Tok()])

    S.barrier()
    S.emit(nc, es, final=(stage == "AB"))
    esB.close()
    print("phase B ops", len(S.ops), "standalone waits", S.nwait)
    if stage == "AB":
        es.close()
        return nc

    NSB = 2 if NOWN >= 8 else 1
    SBT = NOWN // NSB
    NT = SBT * 128
    cmb_s = dscr("cmb_s", [NSB, 32, NT], F32)
    tk_cmb = Tok("cmb_s")
    tk_y = Tok("y")
    esC = ExitStack()
    C = lambda name, shape, dt: sb(name, shape, dt, esC)
    Gob = C("Gob", [128, 512], F32)
    G_at = C("G_at", [128, 1024], F32)
    B_at = C("B_at", [128, 1024], F32)
    G_ff = C("G_ff", [128, 1024], F32)
    B_ff = C("B_ff", [128, 1024], F32)
    brB = C("brB", [128, 36], F32)
    wr_f = C("wr_f", [128, 8, 36], F32)
    yacc = C("yacc", [128, SBT, 1024], F32)
    h2T = C("h2T", [128, 8, NT], BF16)
    cmbT = C("cmbT", [32, NT], F32)
    vload(Gob, "ob_g")
    vload(G_at, "ln_attn_g")
    vload(B_at, "ln_attn_b")
    vload(G_ff, "ln_ffn_g")
    vload(B_ff, "ln_ffn_b")
    vload(brB, "b_router")
    TSo("dve", Gob.ap, Gob.ap, math.sqrt(512.0), None, ALU.mult, None, [Gob], [Gob])
    DMA("sp", wr_f.ap, w_router.rearrange("(c p) n -> p c n", p=128), [], [wr_f])

    def layer_norm_tile(src, st_, Gt, Bt_, dst):
        OP("dve", "bn_stats", [src], [st_], st_[:, 0:6], src[:, 0:512])
        OP("dve", "bn_stats", [src], [st_], st_[:, 6:12], src[:, 512:1024])
        OP("dve", "bn_aggr", [st_], [st_], st_[:, 12:14], st_[:, 0:12])
        RSQ(st_[:, 14:15], st_[:, 13:14], LN_EPS, [st_], [st_])
        STT("dve", st_[:, 15:16], st_[:, 12:13], -1.0, st_[:, 14:15], ALU.mult, ALU.mult, [st_], [st_])
        ACT(src.ap, src.ap, AF.Identity, [src, st_], [src], bias=st_[:, 15:16], scale=st_[:, 14:15])
        TTo("pool", src.ap, src.ap, Gt.ap, ALU.mult, [src, Gt], [src])
        TTo("dve", dst.ap, src.ap, Bt_.ap, ALU.add, [src, Bt_], [dst])

    for sbi in range(NSB):
        es1 = ExitStack()
        mk1 = lambda name, shape, dt: sb("%s_s%d" % (name, sbi), shape, dt, es1)
        woutb = mk1("woutb", [128, 8, 1024], BF16)
        mix = [mk1("mix%d" % i, [128, 1024], BF16) for i in range(2)]
        obf = [mk1("obf%d" % i, [128, 512], F32) for i in range(6)]
        hin = [mk1("hin%d" % i, [128, 1024], F32) for i in range(2)]
        rr = [mk1("rr%d" % i, [128, 1024], F32) for i in range(2)]
        rn = [mk1("rn%d" % i, [128, 1024], F32) for i in range(2)]
        h2 = [mk1("h2_%d" % i, [128, 1024], F32) for i in range(2)]
        h2Tf = mk1("h2Tf", [128, 8, 128], F32)
        mixT = mk1("mixT", [128, 8, 128], BF16)
        sq1 = [mk1("sq1_%d" % i, [128, 512], F32) for i in range(2)]
        sta = [mk1("sta_%d" % i, [128, 4], F32) for i in range(4)]
        stb = [mk1("stb_%d" % i, [128, 16], F32) for i in range(2)]
        rt = [mk1("rt%d" % i, [128, 256], F32) for i in range(4)]
        wst1 = rr
        for c in range(8):
            DMA("sp", wst1[c % 2].ap, w_out[c * 128:(c + 1) * 128, :], [], [wst1[c % 2]])
            CP(("dve", "pool")[c % 2], woutb[:, c, :], wst1[c % 2].ap, [wst1[c % 2]], [woutb])

        def rows_(l):
            n = sbi * SBT + l
            return slice(n * 128, (n + 1) * 128)

        def c0(l):
            DMA("sp", obf[l % 6].ap, outb_s[rows_(l), :], [tk_outb], [obf[l % 6]])

        def c1(l):
            ACT(sq1[l % 2].ap, obf[l % 6].ap, AF.Square, [obf[l % 6]], [sq1[l % 2]])

        def c2(l):
            st_ = sta[l % 4]
            RED(st_[:, 0:1], sq1[l % 2].ap, ALU.add, [sq1[l % 2]], [st_])
            TSo("dve", st_[:, 1:2], st_[:, 0:1], 512.0 * RMS_EPS, None, ALU.add, None, [st_], [st_])

        def c3(l):
            st_ = sta[l % 4]
            ACT(st_[:, 1:2], st_[:, 1:2], AF.Ln, [st_], [st_])
            ACT(st_[:, 1:2], st_[:, 1:2], AF.Exp, [st_], [st_], scale=-0.5)
            DMA("sp", mix[l % 2][:, 0:512], mixa_s[rows_(l), :], [tk_mixa], [mix[l % 2]])

        def c4(l):
            mx = mix[l % 2]
            STT("dve", mx[:, 512:1024], obf[l % 6].ap, sta[l % 4][:, 1:2], Gob.ap, ALU.mult, ALU.mult,
                [obf[l % 6], sta[l % 4], Gob], [mx])
            DMA("pool", hin[l % 2].ap, h_s[rows_(l), :], [tk_h], [hin[l % 2]])

        def c5(l):
            mx = mix[l % 2]
            for c in range(8):
                TR(pbf(0)[:, c * 128:(c + 1) * 128], mx[:, c * 128:(c + 1) * 128], identb, [mx, cstb], [PB[0]])
            CP("act", mixT.ap, pbf(0).rearrange("p (c t) -> p c t", t=128), [PB[0]], [mixT])

        def c6(l):
            hi_, r_ = hin[l % 2], rr[l % 2]
            for half in range(2):
                for c in range(8):
                    MM(PB[1 + half].ap, mixT[:, c, :], woutb[:, c, half * 512:(half + 1) * 512], c == 0, c == 7,
                       [mixT, woutb], [PB[1 + half]])
            for half in range(2):
                cs = slice(half * 512, (half + 1) * 512)
                STT("dve", r_[:, cs], hi_[:, cs], ALPHA, PB[1 + half].ap, ALU.mult, ALU.add, [hi_, PB[1 + half]], [r_])

        def c7(l):
            r_, st_ = rr[l % 2], stb[l % 2]
            OP("dve", "bn_stats", [r_], [st_], st_[:, 0:6], r_[:, 0:512])
            OP("dve", "bn_stats", [r_], [st_], st_[:, 6:12], r_[:, 512:1024])
            OP("dve", "bn_aggr", [st_], [st_], st_[:, 12:14], st_[:, 0:12])
            TSo("dve", st_[:, 14:15], st_[:, 13:14], LN_EPS, None, ALU.add, None, [st_], [st_])

        def c8(l):
            r_, st_, rn_ = rr[l % 2], stb[l % 2], rn[l % 2]
            ACT(st_[:, 14:15], st_[:, 14:15], AF.Ln, [st_], [st_])
            ACT(st_[:, 14:15], st_[:, 14:15], AF.Exp, [st_], [st_], scale=-0.5)
            ACT(st_[:, 15:16], st_[:, 12:13], AF.Identity, [st_], [st_], scale=st_[:, 14:15])
            OP("act", "mul", [st_], [st_], st_[:, 15:16], st_[:, 15:16], -1.0)
            ACT(rn_.ap, r_.ap, AF.Identity, [r_, st_], [rn_], bias=st_[:, 15:16], scale=st_[:, 14:15])

        def c9(l):
            rn_ = rn[l % 2]
            TTo("pool", rn_.ap, rn_.ap, G_at.ap, ALU.mult, [rn_, G_at], [rn_])

        def c10(l):
            TTo("dve", h2[l % 2].ap, rn[l % 2].ap, B_at.ap, ALU.add, [rn[l % 2], B_at], [h2[l % 2]])

        def c11(l):
            h2_ = h2[l % 2]
            OP("act", "mul", [h2_], [yacc], yacc[:, l, :], h2_.ap, ALPHA)
            for c in range(8):
                TR(PB[3 + c // 4][:, (c % 4) * 128:(c % 4 + 1) * 128], h2_[:, c * 128:(c + 1) * 128], identf.ap,
                   [h2_, identf], [PB[3 + c // 4]])
            for hh in range(2):
                pv = PB[3 + hh].ap.rearrange("p (c t) -> p c t", t=128)
                CP("act", h2T[:, hh * 4:(hh + 1) * 4, l * 128:(l + 1) * 128], pv, [PB[3 + hh]], [h2T])
                CP("dve", h2Tf[:, hh * 4:(hh + 1) * 4, :], pv, [PB[3 + hh]], [h2Tf])

        def c12(l):
            rt_ = rt[l % 4]
            for c in range(8):
                MM(PB[5][:, 0:36], h2Tf[:, c, :], wr_f[:, c, :], c == 0, c == 7, [h2Tf, wr_f], [PB[5]])
            TTo("dve", rt_[:, 0:36], PB[5][:, 0:36], brB.ap, ALU.add, [PB[5], brB], [rt_])

        def c13(l):
            rt_ = rt[l % 4]
            RED(rt_[:, 40:41], rt_[:, 0:4], ALU.max, [rt_], [rt_])
            TSo("dve", rt_[:, 44:48], rt_[:, 0:4], rt_[:, 40:41], None, ALU.is_equal, None, [rt_], [rt_])
            TSo("dve", rt_[:, 48:52], rt_[:, 44:48], 1.0, 1e30, ALU.subtract, ALU.mult, [rt_], [rt_])
            elm = rt_[:, 64:96]
            TTo("dve", elm.rearrange("p (g j) -> p g j", j=8), rt_[:, 4:36].rearrange("p (g j) -> p g j", j=8),
                rt_[:, 48:52].unsqueeze(2).to_broadcast([128, 4, 8]), ALU.add, [rt_], [rt_])
            RED(rt_[:, 41:42], elm, ALU.max, [rt_], [rt_])
            TSo("dve", rt_[:, 96:128], elm, rt_[:, 41:42], None, ALU.is_equal, None, [rt_], [rt_])
            STT("dve", rt_[:, 128:160], rt_[:, 96:128], -1e30, elm, ALU.mult, ALU.add, [rt_], [rt_])
            RED(rt_[:, 42:43], rt_[:, 128:160], ALU.max, [rt_], [rt_])
            TSo("dve", rt_[:, 160:192], rt_[:, 128:160], rt_[:, 42:43], None, ALU.is_equal, None, [rt_], [rt_])
            TSo("dve", rt_[:, 43:44], rt_[:, 40:41], -1.0, None, ALU.mult, None, [rt_], [rt_])
            TTo("dve", rt_[:, 57:58], rt_[:, 42:43], rt_[:, 41:42], ALU.subtract, [rt_], [rt_])

        def c14(l):
            rt_ = rt[l % 4]
            ACT(rt_[:, 52:56], rt_[:, 0:4], AF.Exp, [rt_], [rt_], bias=rt_[:, 43:44], scale=1.0)
            ACT(rt_[:, 58:59], rt_[:, 57:58], AF.Exp, [rt_], [rt_])

        def c15(l):
            rt_ = rt[l % 4]
            RED(rt_[:, 56:57], rt_[:, 52:56], ALU.add, [rt_], [rt_])
            STT("dve", rt_[:, 59:60], rt_[:, 58:59], 1.0, rt_[:, 56:57], ALU.add, ALU.mult, [rt_], [rt_])
            OP("dve", "reciprocal", [rt_], [rt_], rt_[:, 60:61], rt_[:, 59:60])
            TTo("dve", rt_[:, 61:62], rt_[:, 60:61], rt_[:, 58:59], ALU.mult, [rt_], [rt_])
            TSo("dve", rt_[:, 192:224], rt_[:, 96:128], rt_[:, 60:61], None, ALU.mult, None, [rt_], [rt_])
            STT("dve", rt_[:, 192:224], rt_[:, 160:192], rt_[:, 61:62], rt_[:, 192:224], ALU.mult, ALU.add,
                [rt_], [rt_])
            TR(PB[6][0:32, 0:128], rt_[:, 192:224], identf.ap, [rt_, identf], [PB[6]])
            CP("dve", cmbT[:, l * 128:(l + 1) * 128], PB[6][0:32, 0:128], [PB[6]], [cmbT])

        cstages = [c0, c1, c2, c3, c4, c5, c6, c7, c8, c9, c10, c11, c12, c13, c14, c15]
        for step in range(SBT + len(cstages) - 1):
            for off in range(len(cstages) - 1, -1, -1):
                l = step - off
                if 0 <= l < SBT:
                    cstages[off](l)

        DMA("sp", cmb_s[sbi], cmbT.ap, [cmbT], [tk_cmb])
        if dbg and sbi == 0:
            d_h2T = dout("d_h2T", [128, 8 * NT], BF16)
            DMA("sp", d_h2T, h2T.ap.rearrange("p c n -> p (c n)"), [h2T], [Tok()])
            d_cmbT = dout("d_cmbT", [32, NT], F32)
            DMA("sp", d_cmbT, cmbT.ap, [cmbT], [Tok()])
        S.barrier()
        S.emit(nc, es)
        es1.close()
        es2 = ExitStack()
        mk2 = lambda name, shape, dt: sb("%s_s%d" % (name, sbi), shape, dt, es2)
        wstg = [mk2("wstg%d" % i, [128, 2048], F32) for i in range(3)]
        wg = [mk2("wg%d" % i, [128, 8, 256], BF16) for i in range(2)]
        wu = [mk2("wu%d" % i, [128, 8, 256], BF16) for i in range(2)]
        wd = [mk2("wd%d" % i, [128, 2, 1024], BF16) for i in range(2)]
        cB = [mk2("cB%d" % i, [128, NT], F32) for i in range(2)]
        sg = mk2("sg", [128, 1024], F32)
        t1 = mk2("t1", [128, 1024], F32)
        hid = [mk2("hid%d" % i, [128, 2, 512], BF16) for i in range(2)]
        NBLK = SBT // 4

        def load_expert(e):
            sl = e % 2
            DMA("sp", wstg[0].ap.rearrange("p (c f) -> p c f", f=256), w_gate[e].rearrange("(c p) f -> p c f", p=128),
                [], [wstg[0]])
            CP("act", wg[sl].ap.rearrange("p c f -> p (c f)"), wstg[0].ap, [wstg[0]], [wg[sl]])
            DMA("sp", wstg[1].ap.rearrange("p (c f) -> p c f", f=256), w_up[e].rearrange("(c p) f -> p c f", p=128),
                [], [wstg[1]])
            CP("act", wu[sl].ap.rearrange("p c f -> p (c f)"), wstg[1].ap, [wstg[1]], [wu[sl]])
            DMA("sp", wstg[2].ap.rearrange("p (c f) -> p c f", f=1024), w_down[e].rearrange("(c p) f -> p c f", p=128),
                [], [wstg[2]])
            CP("act", wd[sl].ap.rearrange("p c f -> p (c f)"), wstg[2].ap, [wstg[2]], [wd[sl]])
            DMA("sp", cB[sl].ap, cmb_s[sbi, e:e + 1, :].to_broadcast([128, NT]), [tk_cmb], [cB[sl]])

        eunits = [(e, b) for e in range(32) for b in range(NBLK)]
        dcount = [0]

        def down_pairs(e, b, ui):
            sl = e % 2
            hd = hid[ui % 2]
            outl = []
            for i in range(4):
                for half in range(2):
                    def f(i=i, half=half):
                        bank = PB[4 + dcount[0] % 4]
                        dcount[0] += 1
                        for ff in range(2):
                            MM(bank.ap, hd[:, ff, i * 128:(i + 1) * 128], wd[sl][:, ff, half * 512:(half + 1) * 512],
                               ff == 0, ff == 1, [hd, wd[sl]], [bank])
                        ya = yacc[:, b * 4 + i, half * 512:(half + 1) * 512]
                        TTo("dve", ya, ya, bank.ap, ALU.add, [yacc, bank], [yacc])
                    outl.append(f)
            return outl

        def gu(e, b, ui, inter):
            sl = e % 2
            T0 = b * 512
            nmm = 0
            for which, wt in ((0, wg), (1, wu)):
                for f in range(2):
                    bank = PB[which * 2 + f]
                    for c in range(8):
                        MM(bank.ap, wt[sl][:, c, f * 128:(f + 1) * 128], h2T[:, c, T0:T0 + 512], c == 0, c == 7,
                           [wt[sl], h2T], [bank])
                        nmm += 1
                        if nmm > 16 and nmm % 2 == 0 and inter:
                            inter.pop(0)()
            hd = hid[ui % 2]
            for f in range(2):
                fs = slice(f * 512, (f + 1) * 512)
                ACT(sg[:, fs], PB[f].ap, AF.Silu, [PB[f]], [sg])
                TTo("dve", t1[:, fs], sg[:, fs], PB[2 + f].ap, ALU.mult, [sg, PB[2 + f]], [t1])
                TTo("pool", hd[:, f, :], t1[:, fs], cB[sl][:, T0:T0 + 512], ALU.mult, [t1, cB[sl]], [hd])

        for ui in range(len(eunits) + 1):
            inter = []
            if ui >= 1:
                e, b = eunits[ui - 1]
                inter = down_pairs(e, b, ui - 1)
            if ui < len(eunits):
                e, b = eunits[ui]
                if b == 0 and e == 0:
                    load_expert(0)
                gu(e, b, ui, inter)
            for f in inter:
                f()
            if ui < len(eunits):
                e, b = eunits[ui]
                if b == 0 and e + 1 < 32:
                    load_expert(e + 1)
        S.barrier()
        S.emit(nc, es)
        es2.close()
        es3 = ExitStack()
        yo = [sb("yo%d_s%d" % (i, sbi), [128, 1024], F32, es3) for i in range(2)]
        yi = [sb("yi%d_s%d" % (i, sbi), [128, 1024], F32, es3) for i in range(2)]
        st3 = [sb("st3_%d_s%d" % (i, sbi), [128, 16], F32, es3) for i in range(2)]
        for l in range(SBT):
            n = sbi * SBT + l
            s = l % 2
            CP("act", yi[s].ap, yacc[:, l, :], [yacc], [yi[s]])
            layer_norm_tile(yi[s], st3[s], G_ff, B_ff, yo[s])
            DMA("sp", y_out[n * 128:(n + 1) * 128, :], yo[s].ap, [yo[s]], [tk_y])
        S.barrier()
        S.emit(nc, es, final=(sbi == NSB - 1))
        es3.close()
    esC.close()
    es.close()
    print("total ops", len(S.ops), "standalone waits", S.nwait)
    return nc

COMPUTE = ("pe", "act", "dve", "pool")
KD = 8
EPOCH = 12000


class Tok:
    __slots__ = ("n", "lw", "rd", "rdma", "x")

    def __init__(s, n="", x=False):
        s.n = n
        s.x = x
        s.lw = -1
        s.rd = {}
        s.rdma = []


class Sched:
    def __init__(s):
        s.ops = []
        s.lastc = {}
        s.bar = None
        s.bar_idx = 0
        s.bar_done = set()
        s.dmas_since_bar = []
        s.emitted = 0
        s.dq = {}
        s.semof = {}
        s.pos = {}
        s.cpos = {e: 0 for e in COMPUTE}
        s.cnt = {e: 0 for e in COMPUTE}
        s.seen = {}
        s.sems = {}
        s.final = {}
        s.nwait = 0

    def add(s, eng, fn, reads=(), writes=(), dma=False):
        i = len(s.ops)
        deps = set()
        xr = [t for t in reads if t.x]
        if xr:
            reads = [t for t in reads if not t.x]
            writes = list(writes) + [t for t in xr if t not in writes]
        for t in reads:
            if t.lw >= 0:
                deps.add(t.lw)
        for t in writes:
            if t.lw >= 0:
                deps.add(t.lw)
            deps.update(t.rd.values())
            deps.update(t.rdma)
        bi = s.bar_idx
        deps = set(d for d in deps if d >= bi)
        if s.bar is not None and eng not in s.bar_done:
            deps.update(s.bar)
            s.bar_done.add(eng)
        s.ops.append([eng, fn, deps, dma])
        for t in reads:
            if dma:
                t.rdma.append(i)
            else:
                t.rd[eng] = i
        for t in writes:
            t.lw = i
            t.rd = {}
            t.rdma = []
        if dma:
            s.dmas_since_bar.append(i)
        else:
            s.lastc[eng] = i
        return i

    def barrier(s):
        b = set(s.dmas_since_bar)
        b.update(s.lastc.values())
        if s.bar is not None and len(s.bar_done) < 5:
            b.update(s.bar)
        s.bar = b
        s.bar_idx = len(s.ops)
        s.bar_done = set()
        s.dmas_since_bar = []

    def emit(s, nc, stack, final=False):
        ops = s.ops
        lo, n = s.emitted, len(ops)
        semof, pos = s.semof, s.pos
        for i in range(lo, n):
            eng, fn, deps, dma = ops[i]
            if dma:
                lst = s.dq.setdefault(eng, [])
                k = len(lst)
                if k >= KD:
                    deps.add(lst[k - KD])
                lst.append(i)
                semof[i] = (("d", eng, k % KD), 16 * (k // KD + 1))
            else:
                pos[i] = s.cpos[eng]
                s.cpos[eng] += 1
        hasdep = set()
        for i in range(lo, n):
            hasdep.update(ops[i][2])
        hasdep.update(s.lastc.values())
        for i in range(lo, n):
            eng, fn, deps, dma = ops[i]
            if not dma and i in hasdep:
                c = s.cnt[eng]
                s.cnt[eng] = c + 1
                semof[i] = (("c", eng, c // EPOCH), c % EPOCH + 1)
        waits = {}
        for i in range(lo, n):
            eng, fn, deps, dma = ops[i]
            need = {}
            for d in deps:
                de, _, _, ddma = ops[d]
                if not ddma and de == eng and not dma:
                    if eng == "pe":
                        continue
                    if pos[i] - pos[d] > 3:
                        continue
                key, val = semof[d]
                if need.get(key, 0) < val:
                    need[key] = val
            sn = s.seen.setdefault(eng, {})
            w = []
            for key, val in need.items():
                if sn.get(key, 0) >= val:
                    continue
                sn[key] = val
                w.append((key, val))
            waits[i] = w
        sems = s.sems
        for i in range(lo, n):
            x = semof.get(i)
            if x is None:
                continue
            if x[0] not in sems:
                sems[x[0]] = stack.enter_context(nc.semaphore("s_%s_%s_%d" % x[0]))
            if x[0][0] == "d" and s.final.get(x[0], 0) < x[1]:
                s.final[x[0]] = x[1]
        with nc.Block() as block:
            def run(engname, eobj, tail=False):
                for i in range(lo, n):
                    o = ops[i]
                    if o[0] != engname:
                        continue
                    w = waits[i]
                    for key, val in w[:-1]:
                        eobj.wait_ge(sems[key], val)
                        s.nwait += 1
                    ins = o[1](eobj)
                    if w:
                        ins._wait_ge(sems[w[-1][0]], w[-1][1])
                    x = semof.get(i)
                    if x is not None:
                        ins.then_inc(sems[x[0]], 16 if x[0][0] == "d" else 1)
                    o[1] = None
                if tail:
                    for key, val in s.final.items():
                        eobj.wait_ge(sems[key], val)

            @block.sync
            def _(e):
                run("sp", e, tail=final)

            @block.scalar
            def _(e):
                run("act", e)

            @block.vector
            def _(e):
                run("dve", e)

            @block.gpsimd
            def _(e):
                run("pool", e)

            @block.tensor
            def _(e):
                run("pe", e)
        s.emitted = n


N_CORES = 8
NOWN_FULL = 32
NOTH_FULL = 96
_NC_CACHE = {}


def _host_prep(inputs, c):
    bf = ml_dtypes.bfloat16
    NOWN, NOTH = NOWN_FULL, NOTH_FULL
    seq_tiles = NOWN + NOTH
    b = c // 4
    qd = c % 4
    first = qd == 0
    last = qd == 3
    own0 = qd * NOWN
    x = inputs["x"][b]
    pos = inputs["positions"][b]
    own = list(range(own0, own0 + NOWN))
    others = [t for t in range(seq_tiles) if t not in own]
    P = own0 - 1 if not first else own0
    N = own0 + NOWN if not last else own0 + NOWN - 1
    order = [P] + own + [N] + others
    xr = np.concatenate([x[t * 128:(t + 1) * 128] for t in order], 0)
    posr = np.ascontiguousarray(np.stack([pos[t * 128:(t + 1) * 128] for t in order], 1).astype(np.int32))
    jj = np.arange(128)[:, None]
    ii = np.arange(128)[None, :]
    mP = np.where(jj >= ii, 0.0, NEGB).astype(np.float32)
    mN = np.where(jj <= ii, 0.0, NEGB).astype(np.float32)
    full = np.full((128, 128), NEGB, np.float32)
    ms = [mP, mN, full if first else mP, full if last else mN]
    cst_b = np.concatenate([np.eye(128, dtype=np.float32)] + [np.tile(m, (1, 4)) for m in ms], 1).astype(bf)
    return dict(xr=xr, posr=posr, cst_b=cst_b)


def kernel(**inputs):
    inputs = {k: np.asarray(v) for k, v in inputs.items()}
    g = lambda k: np.ascontiguousarray(inputs[k], dtype=np.float32)
    invA = (1.0 / (np.float32(10000.0) ** (np.arange(0, 64, 2, dtype=np.float32) / np.float32(64)))).astype(np.float32)
    invB = (1.0 / (np.float32(10000.0) ** (np.arange(0, 32, 2, dtype=np.float32) / np.float32(32)))).astype(np.float32)
    parts = dict(ln_emb_g=g("ln_emb_g"), ln_emb_b=g("ln_emb_b"), q_g=g("q_a_norm_g")[0], kv_g=g("kv_a_norm_g")[0],
                 oa_g=g("out_norm_a_g")[0], ob_g=g("out_norm_b_g")[0], ln_attn_g=g("ln_attn_g")[0],
                 ln_attn_b=g("ln_attn_b")[0], ln_ffn_g=g("ln_ffn_g")[0], ln_ffn_b=g("ln_ffn_b")[0], sink=g("a_sink")[0],
                 b_router=np.concatenate([g("b_group")[0], g("b_expert")[0]]), invA=invA, invB=invB)
    vecs = np.zeros((1, NVEC), np.float32)
    for k, (o, l) in VOFF.items():
        vecs[0, o:o + l] = parts[k]
    shared = dict(w_in=g("w_in")[0], w_q_b=g("w_q_b")[0], w_kv_b=g("w_kv_b")[0], w_out=g("w_out")[0],
                  w_router=np.ascontiguousarray(np.concatenate([g("w_group")[0], g("w_expert")[0]], 1)),
                  w_gate=g("w_gate")[0], w_up=g("w_up")[0], w_down=g("w_down")[0], vecs=vecs,
                  cst_f=np.eye(128, dtype=np.float32))
    in_maps = []
    for c in range(N_CORES):
        m = dict(shared)
        m.update(_host_prep(inputs, c))
        in_maps.append(m)
    if "nc" not in _NC_CACHE:
        _NC_CACHE["nc"] = build(NOWN_FULL, NOTH_FULL, stage="ABC", dbg=False)
    nc = _NC_CACHE["nc"]
    res = run_bass_kernel_spmd(nc, in_maps, core_ids=list(range(N_CORES)))
    B, Sq, D = inputs["x"].shape
    out = np.zeros((B, Sq, D), np.float32)
    for c in range(N_CORES):
        b = c // 4
        r0 = (c % 4) * NOWN_FULL * 128
        out[b, r0:r0 + NOWN_FULL * 128] = np.asarray(res.results[c]["y"], dtype=np.float32)
    return out
```

```python
import math
import numpy as np
import ml_dtypes
from contextlib import ExitStack
import concourse.bass as bass
import concourse.mybir as mybir
from concourse.bass_utils import run_bass_kernel_spmd

F32 = mybir.dt.float32
BF16 = mybir.dt.bfloat16
I32 = mybir.dt.int32
AF = mybir.ActivationFunctionType
ALU = mybir.AluOpType
AX = mybir.AxisListType

LN_EPS = 1e-5
RMS_EPS = 1e-6
ALPHA = 2.0 ** 0.25
NEGB = -30000.0
TWO_PI = 2.0 * math.pi
C1 = 6.28125
C2 = float(np.float32(TWO_PI - C1))
C3 = float(TWO_PI - C1 - float(np.float32(TWO_PI - C1)))
MAGIC = 12582912.0
PI_LIM = 3.1415925

VOFF = {}
_o = 0
for _n, _l in [("ln_emb_g", 1024), ("ln_emb_b", 1024), ("q_g", 768), ("kv_g", 256), ("oa_g", 512),
               ("ob_g", 512), ("ln_attn_g", 1024), ("ln_attn_b", 1024), ("ln_ffn_g", 1024),
               ("ln_ffn_b", 1024), ("sink", 8), ("b_router", 36), ("invA", 32), ("invB", 16)]:
    VOFF[_n] = (_o, _l)
    _o += _l
NVEC = _o


class TT:
    def __init__(s, h, name):
        s.h = h
        s.t = Tok(name)

    def __getitem__(s, k):
        return s.h[k]

    @property
    def ap(s):
        return s.h[:]


def build(NOWN, NOTH, stage="ABC", dbg=False):
    NJ = NOWN + NOTH + 2
    NK = NOWN + NOTH
    NW = NOWN + 2
    NQ = NOWN * 128
    nc = bass.Bass("TRN2", target_bir_lowering=False)
    S = Sched()
    es = ExitStack()

    def din(name, shape, dt=F32):
        return nc.dram_tensor(name, list(shape), dt, kind="ExternalInput").ap()

    def dscr(name, shape, dt):
        return nc.dram_tensor(name, list(shape), dt, kind="Internal").ap()

    def dout(name, shape, dt=F32):
        return nc.dram_tensor(name, list(shape), dt, kind="ExternalOutput").ap()

    xr = din("xr", [NJ * 128, 1024])
    posr = din("posr", [128, NJ], I32)
    w_in = din("w_in", [1024, 1824])
    w_q_b = din("w_q_b", [768, 768])
    w_kv_b = din("w_kv_b", [256, 1024])
    w_out = din("w_out", [1024, 1024])
    w_router = din("w_router", [1024, 36])
    w_gate = din("w_gate", [32, 1024, 256])
    w_up = din("w_up", [32, 1024, 256])
    w_down = din("w_down", [32, 256, 1024])
    vecs = din("vecs", [1, NVEC])
    cst_f = din("cst_f", [128, 128])
    cst_b = din("cst_b", [128, 128 + 4 * 512], BF16)
    y_out = dout("y", [NQ, 1024])

    KnT_s = dscr("KnT_s", [512, NK * 128], BF16)
    KrT_s = dscr("KrT_s", [32, NK * 128], BF16)
    V_s = dscr("V_s", [8, 128, NK * 72], BF16)
    h_s = dscr("h_s", [NQ, 1024], F32)
    mixa_s = dscr("mixa_s", [NQ, 512], BF16)
    outb_s = dscr("outb_s", [NQ, 512], F32)
    QT_s = dscr("QT_s", [97, 8, NQ], BF16)
    tk_QT = Tok("QT_s")
    tk_h = Tok("h_s")
    tk_mixa = Tok("mixa_s")
    tk_outb = Tok("outb_s")
    tk_KnT = Tok("KnT_s")
    tk_KrT = Tok("KrT_s")
    tk_V = Tok("V_s")

    dbg_outs = {}

    def sb(name, shape, dt, stack=None):
        h = (stack or es).enter_context(nc.sbuf_tensor(name, list(shape), dt))
        return TT(h, name)

    def OP(eng, meth, R, W, *a, **kw):
        return S.add(eng, lambda e: getattr(e, meth)(*a, **kw), [x.t if isinstance(x, TT) else x for x in R],
                     [x.t if isinstance(x, TT) else x for x in W])

    def DMA(q, out, in_, R, W):
        return S.add(q, lambda e: e.dma_start(out=out, in_=in_), [x.t if isinstance(x, TT) else x for x in R],
                     [x.t if isinstance(x, TT) else x for x in W], dma=True)

    def MM(out, lhsT, rhs, start, stop, R, W):
        return OP("pe", "matmul", R, W, out, lhsT, rhs, start=start, stop=stop)

    def TR(out, in_, ident, R, W):
        return OP("pe", "transpose", R, W, out, in_, ident)

    def ACT(out, in_, func, R, W, **kw):
        return OP("act", "activation", R, W, out, in_, func, **kw)

    def TTo(eng, out, in0, in1, op, R, W):
        return OP(eng, "tensor_tensor", R, W, out, in0, in1, op)

    def TSo(eng, out, in0, s1, s2, op0, op1, R, W):
        if op1 is None:
            return OP(eng, "tensor_scalar", R, W, out, in0, s1, None, op0)
        return OP(eng, "tensor_scalar", R, W, out, in0, s1, s2, op0, op1)

    def STT(eng, out, in0, sc, in1, op0, op1, R, W):
        return OP(eng, "scalar_tensor_tensor", R, W, out, in0, sc, in1, op0, op1)

    def CP(eng, out, in_, R, W):
        if eng == "act":
            return OP("act", "copy", R, W, out, in_)
        return OP(eng, "tensor_copy", R, W, out, in_)

    EPSC = {}

    def RSQ(dst, src, addc, R, W):
        col = EPSC[addc]
        ACT(dst, src, AF.Ln, list(R) + [epst], W, bias=epst[:, col:col + 1])
        ACT(dst, dst, AF.Exp, W, W, scale=-0.5)

    def RED(out, in_, op, R, W, axis=AX.X):
        return OP("dve", "tensor_reduce", R, W, out, in_, axis, op)

    PB = []
    PP = []
    for b in range(4):
        h = es.enter_context(nc.psum_tensor("pp%d" % b, [128, 1024], F32))
        PP.append(h)
        for half in range(2):
            PB.append(TT(h[:, half * 512:(half + 1) * 512], "pb%d" % (2 * b + half)))
            PB[-1].t.x = True

    def pbf(b):
        return PB[b].ap.bitcast(BF16)

    identf = sb("identf", [128, 128], F32)
    cstb = sb("cstb", [128, 128 + 2048], BF16)
    onesf = sb("onesf", [128, 128], F32)
    DMA("sp", identf.ap, cst_f, [], [identf])
    DMA("sp", cstb.ap, cst_b, [], [cstb])
    OP("pool", "memset", [], [onesf], onesf.ap, 1.0)
    identb = cstb[:, 0:128]
    epst = sb("epst", [128, 4], F32)
    for ci, ev in enumerate([LN_EPS, 256.0 * RMS_EPS, 768.0 * RMS_EPS, 512.0 * RMS_EPS]):
        EPSC[ev] = ci
        OP("pool", "memset", [], [epst], epst[:, ci:ci + 1], ev)

    def maskap(i):
        return cstb[:, 128 + i * 512:128 + (i + 1) * 512]

    def vload(dst, name, eng="sp"):
        o, l = VOFF[name]
        DMA(eng, dst.ap, vecs[0:1, o:o + l].to_broadcast([128, l]), [], [dst])

    esT = ExitStack()
    qn2b = sb("qn2b", [128, NOWN, 8], F32, esT)
    kmax2 = sb("kmax2", [128, 8], F32, esT)

    esA = ExitStack()
    A = lambda name, shape, dt: sb(name, shape, dt, esA)
    winb = A("winb", [128, 8, 1824], BF16)
    wqb = A("wqb", [128, 6, 768], BF16)
    wkvb = A("wkvb", [128, 2, 1024], BF16)
    wkb = A("wkb", [128, 2, 512], BF16)
    G_emb = A("G_emb", [128, 1024], F32)
    B_emb = A("B_emb", [128, 1024], F32)
    Gq = A("Gq", [128, 768], F32)
    Gkv = A("Gkv", [128, 256], F32)
    Ga = A("Ga", [128, 512], F32)
    sinkB = A("sinkB", [128, 8], F32)
    invA = A("invA", [128, 32], F32)
    invB = A("invB", [128, 16], F32)
    posi = A("posi", [128, NJ], I32)
    posf = A("posf", [128, NJ], F32)
    tabB = A("tabB", [128, NJ, 2, 16], F32)
    tabA = A("tabA", [128, NW, 2, 32], F32)
    esP = ExitStack()
    tabk = sb("tabk", [128, max(NJ * 32, NW * 64)], F32, esP)
    stg = [sb("stg%d" % i, [128, 1824], F32, esP) for i in range(2)]

    vload(G_emb, "ln_emb_g")
    vload(B_emb, "ln_emb_b")
    vload(Gq, "q_g")
    vload(Gkv, "kv_g")
    vload(Ga, "oa_g")
    vload(sinkB, "sink")
    vload(invA, "invA")
    vload(invB, "invB")
    DMA("sp", posi.ap, posr, [], [posi])
    TSo("dve", Gq.ap, Gq.ap, math.sqrt(768.0), None, ALU.mult, None, [Gq], [Gq])
    TSo("dve", Gkv.ap, Gkv.ap, 16.0, None, ALU.mult, None, [Gkv], [Gkv])
    TSo("dve", Ga.ap, Ga.ap, math.sqrt(512.0), None, ALU.mult, None, [Ga], [Ga])
    CP("dve", posf.ap, posi.ap, [posi], [posf])

    def make_table(tab, n, half, inv):
        tv = tab.ap
        kv = tabk[:, 0:n * 2 * half].rearrange("p (n t f) -> p n t f", t=2, f=half)
        pb_ = posf[:, 0:n].unsqueeze(2).to_broadcast([128, n, half])
        ib_ = inv.ap.unsqueeze(1).to_broadcast([128, n, half])
        TTo("dve", tv[:, :, 0, :], pb_, ib_, ALU.mult, [posf, inv], [tab])
        TSo("dve", tv[:, :, 1, :], tv[:, :, 0, :], math.pi / 2, None, ALU.add, None, [tab], [tab])
        TSo("dve", kv, tv, 1.0 / TWO_PI, MAGIC, ALU.mult, ALU.add, [tab], [tabk])
        TSo("dve", kv, kv, -MAGIC, None, ALU.add, None, [tabk], [tabk])
        STT("dve", tv, kv, -C1, tv, ALU.mult, ALU.add, [tabk, tab], [tab])
        STT("dve", tv, kv, -C2, tv, ALU.mult, ALU.add, [tabk, tab], [tab])
        STT("dve", tv, kv, -C3, tv, ALU.mult, ALU.add, [tabk, tab], [tab])
        TSo("dve", tv, tv, PI_LIM, -PI_LIM, ALU.min, ALU.max, [tab], [tab])
        ACT(tv, tv, AF.Sin, [tab], [tab])

    import os
    SKIP = os.environ.get("SKIP", "").split(",")
    if "tables" not in SKIP:
        make_table(tabB, NJ, 16, invB)
        make_table(tabA, NW, 32, invA)

    ncast = [0]

    def cast(out, in_, R, W, scale=None):
        eng = ("dve", "pool")[ncast[0] % 2]
        ncast[0] += 1
        if scale is None:
            CP(eng, out, in_, R, W)
        else:
            TSo(eng, out, in_, scale, None, ALU.mult, None, R, W)

    si = 0
    for c in range(8 if "weights" not in SKIP else 0):
        st_ = stg[si % 2]
        si += 1
        DMA("sp", st_[:, 0:1824], w_in[c * 128:(c + 1) * 128, :], [], [st_])
        cast(winb[:, c, 0:512], st_[:, 0:512], [st_], [winb], scale=0.125)
        cast(winb[:, c, 512:1824], st_[:, 512:1824], [st_], [winb])
    for c in range(6 if "weights" not in SKIP else 0):
        st_ = stg[si % 2]
        si += 1
        DMA("sp", st_[:, 0:768], w_q_b[c * 128:(c + 1) * 128, :], [], [st_])
        cast(wqb[:, c, :], st_[:, 0:768], [st_], [wqb], scale=96.0 ** -0.5)
    for c in range(2 if "weights" not in SKIP else 0):
        st_ = stg[si % 2]
        si += 1
        DMA("sp", st_[:, 0:1024], w_kv_b[c * 128:(c + 1) * 128, :], [], [st_])
        cast(wkvb[:, c, :], st_[:, 0:1024], [st_], [wkvb])
        cast(wkb[:, c, :].rearrange("p (h d) -> p h d", d=64),
             st_[:, 0:1024].rearrange("p (h x) -> p h x", x=128)[:, :, 0:64], [st_], [wkb])

    STOPAT = os.environ.get("STOPAT", "")
    if STOPAT == "P":
        d_tabB = dout("d_tabB", [128, NJ * 32])
        DMA("sp", d_tabB, tabB.ap.rearrange("p n t f -> p (n t f)"), [tabB], [Tok()])
        S.barrier()
        S.emit(nc, es, final=True)
        return nc
    S.barrier()
    S.emit(nc, es)
    esP.close()
    xt = [A("xt%d" % i, [128, 1024], F32) for i in range(2)]
    hf = [A("hf%d" % i, [128, 1024], F32) for i in range(2)]
    hb = [A("hb%d" % i, [128, 1024], BF16) for i in range(2)]
    hT = [A("hT%d" % i, [128, 8, 128], BF16) for i in range(2)]
    stt = [A("stt%d" % i, [128, 16], F32) for i in range(2)]
    kvb = [A("kvb%d" % i, [128, 288], BF16) for i in range(2)]
    sml = [A("sml%d" % i, [128, 32], F32) for i in range(2)]
    latT = [A("latT%d" % i, [128, 2, 128], BF16) for i in range(2)]
    KTbuf = [A("KTbuf%d" % i, [128, 4, 512], BF16) for i in range(2)]
    KrTbuf = [A("KrTbuf%d" % i, [32, 512], BF16) for i in range(2)]
    Vbuf = [A("Vbuf%d" % i, [128, 8, 4, 72], BF16) for i in range(2)]
    kn2 = A("kn2", [128, 8], F32)
    cqn = A("cqn", [128, 768], BF16)
    cqT = A("cqT", [128, 6, 128], BF16)
    qtok = A("qtok", [128, 8, 96], BF16)
    qst = [A("qst%d" % i, [96, 8, 128], BF16) for i in range(2)]
    KaT = A("KaT", [65, 2, 8 * 128], BF16)
    Va = A("Va", [128, 8, 2, 72], BF16)
    kmaxa = A("kmaxa", [128, NW, 2], F32)
    katok = A("katok", [128, 128], BF16)
    qatok = [A("qatok%d" % i, [128, 8, 65], BF16) for i in range(6)]
    qn2a = [A("qn2a%d" % i, [128, 8], F32) for i in range(6)]
    PTa = [A("PTa%d" % i, [128, 512], BF16) for i in range(2)]
    Osb = A("Osb", [65, 512], F32)
    Osb_default = Osb
    mixa = [A("mixa%d" % i, [128, 512], BF16) for i in range(2)]
    wsm = A("wsm", [128, 64], F32)
    wsm_default = wsm

    for v in Vbuf:
        OP("pool", "memset", [], [v], v.ap, 0.0)
        OP("pool", "memset", [], [v], v[:, :, :, 64:65], 1.0)
    OP("pool", "memset", [], [Va], Va.ap, 0.0)
    OP("pool", "memset", [], [Va], Va[:, :, :, 64:65], 1.0)
    OP("pool", "memset", [], [KaT], KaT[64:65, :, :], 1.0)
    OP("pool", "memset", [], [kmax2], kmax2.ap, 0.0)

    def rope(src, H, half, cos, sin, dst, R, W, tmps=None, scl=None):
        n = H * 2 * half
        tA_, tB_ = tmps
        tA = tA_[:, 0:n].rearrange("p (h t f) -> p h t f", t=2, f=half)
        tB = tB_[:, 0:n].rearrange("p (h t f) -> p h t f", t=2, f=half)
        cb = cos.unsqueeze(1).unsqueeze(1).to_broadcast([128, H, 2, half])
        sb_ = sin.unsqueeze(1).to_broadcast([128, H, half])
        if scl is None:
            TTo("dve", tA, src, cb, ALU.mult, R, [tA_])
            TTo("dve", tB[:, :, 0, :], src[:, :, 1, :], sb_, ALU.mult, R, [tB_])
            TTo("dve", tB[:, :, 1, :], src[:, :, 0, :], sb_, ALU.mult, R, [tB_])
        else:
            cb3 = cos.unsqueeze(1).to_broadcast([128, H, half])
            STT("dve", tA[:, :, 0, :], src[:, :, 0, :], scl, cb3, ALU.mult, ALU.mult, R, [tA_])
            STT("dve", tA[:, :, 1, :], src[:, :, 1, :], scl, cb3, ALU.mult, ALU.mult, R, [tA_])
            STT("dve", tB[:, :, 0, :], src[:, :, 1, :], scl, sb_, ALU.mult, ALU.mult, R, [tB_])
            STT("dve", tB[:, :, 1, :], src[:, :, 0, :], scl, sb_, ALU.mult, ALU.mult, R, [tB_])
        TTo("dve", dst[:, :, 0, :], tA[:, :, 0, :], tB[:, :, 0, :], ALU.subtract, [tA_, tB_], W)
        TTo("dve", dst[:, :, 1, :], tA[:, :, 1, :], tB[:, :, 1, :], ALU.add, [tA_, tB_], W)

    def pmax_bcast(src_ap, src_tok, ncol, dst_ap, dst_tok, bank, wsm=None):
        if wsm is None:
            wsm = wsm_default
        pb_ = PB[bank]
        TR(pb_[0:ncol, 0:128], src_ap, identf.ap, [src_tok, identf], [pb_])
        RED(wsm[0:ncol, 0:1], pb_[0:ncol, 0:128], ALU.max, [pb_], [wsm])
        TSo("dve", wsm[0:ncol, 8:8 + ncol], identf[0:ncol, 0:ncol], wsm[0:ncol, 0:1], None, ALU.mult, None,
            [wsm, identf], [wsm])
        MM(pb_[:, 256:256 + ncol], onesf[0:ncol, :], wsm[0:ncol, 8:8 + ncol], True, True, [onesf, wsm], [pb_])
        CP("dve", dst_ap, pb_[:, 256:256 + ncol], [pb_], [dst_tok])

    evi = [0]

    def evac(out, in_, R, W):
        eng = ("act", "dve")[evi[0] % 2]
        evi[0] += 1
        CP(eng, out, in_, R, W)

    wsm9 = [A("wsm9_%d" % i, [128, 64], F32) for i in range(2)]
    QaT2 = [A("QaT2_%d" % i, [65, 1024], BF16) for i in range(2)]
    oa2 = [A("oa2_%d" % i, [128, 8, 64], F32) for i in range(2)]
    sq9 = A("sq9", [128, 512], F32)
    for w9 in wsm9:
        OP("pool", "memset", [], [w9], w9.ap, 0.0)

    def wa_a(n):
        w = n + 1
        sq_ = n % 6
        qa_ = qatok[sq_]
        wsm = wsm9[n % 2]
        TTo("dve", wsm[:, 16:18], kmaxa[:, w - 1, :], kmaxa[:, w, :], ALU.max, [kmaxa], [wsm])
        TTo("dve", wsm[:, 16:18], wsm[:, 16:18], kmaxa[:, w + 1, :], ALU.max, [kmaxa, wsm], [wsm])
        TTo("dve", wsm[:, 20:28].rearrange("p (g f) -> p g f", f=4),
            qn2a[sq_].ap.rearrange("p (g f) -> p g f", f=4),
            wsm[:, 16:18].unsqueeze(2).to_broadcast([128, 2, 4]), ALU.mult, [qn2a[sq_], wsm], [wsm])
        ACT(wsm[:, 20:28], wsm[:, 20:28], AF.Ln, [wsm], [wsm])
        ACT(wsm[:, 20:28], wsm[:, 20:28], AF.Exp, [wsm], [wsm], scale=0.5)
        TSo("dve", qa_[:, :, 64:65], wsm[:, 20:28].unsqueeze(2), -1.0, None, ALU.mult, None, [wsm], [qa_])
        TTo("dve", wsm[:, 28:36].unsqueeze(2), qa_[:, :, 64:65], sinkB.ap.unsqueeze(2), ALU.add, [qa_, sinkB], [wsm])
        ACT(wsm[:, 36:44], wsm[:, 28:36], AF.Exp, [wsm], [wsm])
        for h in range(8):
            TR(pbf(0)[0:65, h * 128:(h + 1) * 128], qa_[:, h, 0:65], identb, [qa_, cstb], [PB[0]])
        evac(QaT2[n % 2][:, :], pbf(0)[0:65, :], [PB[0]], [QaT2[n % 2]])

    PTa2 = [A("PTa2_%d" % i, [128, 512], BF16) for i in range(2)]
    Osb2 = A("Osb2", [65, 512], F32)

    def capture(fn):
        rec = []
        orig = S.add
        S.add = lambda *a, **kw: rec.append((a, kw))
        try:
            fn()
        finally:
            S.add = orig
        return rec

    def interleave2(fa, fb):
        ra = capture(fa) if fa is not None else []
        rb = capture(fb) if fb is not None else []
        i = 0
        while i < len(ra) or i < len(rb):
            if i < len(ra):
                S.add(*ra[i][0], **ra[i][1])
            if i < len(rb):
                S.add(*rb[i][0], **rb[i][1])
            i += 1

    def wa_g(n, g, bset=0):
        w = n + 1
        wsm = wsm9[n % 2]
        QaT = QaT2[n % 2]
        oa = oa2[n % 2]
        first_tile = (n == 0)
        last_tile = (n == NOWN - 1)
        if bset == 0:
            bo, bsl, bt, PTx, Osb = PB[5], (PB[6], PB[7]), PB[4], PTa, Osb_default
        else:
            bo, bsl, bt, PTx, Osb = PB[1], (PB[2], PB[3]), PB[0], PTa2, Osb2
        kts = [w - 1, w, w + 1]
        for ki, kt in enumerate(kts):
            bs = bsl[ki % 2]
            pt = PTx[ki % 2]
            MM(bs.ap, KaT[0:65, g, (kt % 8) * 128:(kt % 8 + 1) * 128], QaT[:, g * 512:(g + 1) * 512], True, kt == w,
               [KaT, QaT], [bs])
            if kt != w:
                if kt < w:
                    mi = 2 if first_tile else 0
                else:
                    mi = 3 if last_tile else 1
                MM(bs.ap, identb, maskap(mi), False, True, [cstb], [bs])
            ACT(pt.ap, bs.ap, AF.Exp, [bs], [pt])
            MM(bo[0:65, :], Va[:, kt % 8, g, 0:65], pt.ap, ki == 0, ki == 2, [Va, pt], [bo])
        CP("dve", Osb.ap, bo[0:65, :], [bo], [Osb])
        btv = bt[:, 0:260].rearrange("p (i x) -> p i x", x=65)
        for i in range(4):
            TR(bt[:, i * 65:(i + 1) * 65], Osb[:, i * 128:(i + 1) * 128], identf[0:65, 0:65], [Osb, identf], [bt])
        TTo("dve", wsm[:, 44:48].unsqueeze(2), btv[:, :, 64:65], wsm[:, 36 + g * 4:40 + g * 4].unsqueeze(2),
            ALU.add, [bt, wsm], [wsm])
        OP("dve", "reciprocal", [wsm], [wsm], wsm[:, 48:52], wsm[:, 44:48])
        TTo("dve", oa[:, g * 4:(g + 1) * 4, :], btv[:, :, 0:64],
            wsm[:, 48:52].unsqueeze(2).to_broadcast([128, 4, 64]), ALU.mult, [bt, wsm], [oa])

    def wa_pair(j):
        fa = (lambda: wa_g(j - 2, 1, 0)) if has_wa(j) else None
        fb = (lambda: wa_g(j - 1, 0, 1)) if has_wa(j + 1) else None
        interleave2(fa, fb)

    def wa_d(n):
        wsm = wsm9[n % 2]
        oa = oa2[n % 2]
        oaf = oa.ap.rearrange("p h d -> p (h d)")
        ACT(sq9.ap, oaf, AF.Square, [oa], [sq9])
        RED(wsm[:, 52:53], sq9.ap, ALU.add, [sq9], [wsm])
        RSQ(wsm[:, 53:54], wsm[:, 52:53], 512.0 * RMS_EPS, [wsm], [wsm])
        mx = mixa[n % 2]
        STT("dve", mx.ap, oaf, wsm[:, 53:54], Ga.ap, ALU.mult, ALU.mult, [oa, wsm, Ga], [mx])
        DMA("sp", mixa_s[n * 128:(n + 1) * 128, :], mx.ap, [mx], [tk_mixa])

    sq3 = [A("sq3_%d" % i, [128, 288], F32) for i in range(2)]
    sq5 = A("sq5", [128, 512], F32)
    sq7 = A("sq7", [128, 128], F32)
    sq8 = A("sq8", [128, 768], F32)
    rt3 = [(A("rt3a%d" % i, [128, 32], F32), A("rt3b%d" % i, [128, 32], F32)) for i in range(2)]
    rt7 = (A("rt7a", [128, 128], F32), A("rt7b", [128, 128], F32))
    rt8a = (A("rt8aa", [128, 512], F32), A("rt8ab", [128, 512], F32))
    rt8d = (A("rt8da", [128, 128], F32), A("rt8db", [128, 128], F32))
    sm8s = [A("sm8_%d" % i, [128, 8], F32) for i in range(2)]
    kvf = [A("kvf%d" % i, [128, 288], F32) for i in range(4)]
    xn = [A("xn%d" % i, [128, 1024], F32) for i in range(2)]
    kr2all = A("kr2all", [128, NJ], F32)
    ssqkv = A("ssqkv", [128, NJ], F32)
    wsm7 = A("wsm7", [128, 64], F32)
    keyidx = {}
    _kt = 0
    for j in range(NJ):
        if j not in (0, NOWN + 1):
            keyidx[j] = _kt
            _kt += 1

    def proj(hT_, bank, ncols, col0):
        for c in range(8):
            MM(PB[bank][:, 0:ncols], hT_[:, c, :], winb[:, c, col0:col0 + ncols], c == 0, c == 7,
               [hT_, winb], [PB[bank]])

    is_key_ = lambda j: j not in (0, NOWN + 1)
    is_win_ = lambda j: j <= NOWN + 1
    is_own_ = lambda j: 1 <= j <= NOWN

    def sA0(j):
        s = j % 2
        x_, st_ = xt[s], stt[s]
        if j == 0:
            DMA("pool", x_.ap, xr[0:128, :], [], [x_])
        if j + 1 < NJ:
            DMA("pool", xt[(j + 1) % 2].ap, xr[(j + 1) * 128:(j + 2) * 128, :], [], [xt[(j + 1) % 2]])
        OP("dve", "bn_stats", [x_], [st_], st_[:, 0:6], x_[:, 0:512])
        OP("dve", "bn_stats", [x_], [st_], st_[:, 6:12], x_[:, 512:1024])
        OP("dve", "bn_aggr", [st_], [st_], st_[:, 12:14], st_[:, 0:12])
        TSo("dve", st_[:, 14:15], st_[:, 13:14], LN_EPS, None, ALU.add, None, [st_], [st_])

    def sA1(j):
        s = j % 2
        x_, st_, xn_ = xt[s], stt[s], xn[s]
        ACT(st_[:, 14:15], st_[:, 14:15], AF.Ln, [st_], [st_])
        ACT(st_[:, 14:15], st_[:, 14:15], AF.Exp, [st_], [st_], scale=-0.5)
        ACT(st_[:, 15:16], st_[:, 12:13], AF.Identity, [st_], [st_], scale=st_[:, 14:15])
        OP("act", "mul", [st_], [st_], st_[:, 15:16], st_[:, 15:16], -1.0)
        ACT(xn_.ap, x_.ap, AF.Identity, [x_, st_], [xn_], bias=st_[:, 15:16], scale=st_[:, 14:15])

    def sA2(j):
        xn_ = xn[j % 2]
        TTo("pool", xn_.ap, xn_.ap, G_emb.ap, ALU.mult, [xn_, G_emb], [xn_])

    def sA3(j):
        s = j % 2
        xn_, hf_, hb_ = xn[s], hf[s], hb[s]
        if is_own_(j):
            TTo("dve", hf_.ap, xn_.ap, B_emb.ap, ALU.add, [xn_, B_emb], [hf_])
        else:
            TTo("pool", hb_.ap, xn_.ap, B_emb.ap, ALU.add, [xn_, B_emb], [hb_])

    def sA4(j):
        s = j % 2
        hf_, hb_ = hf[s], hb[s]
        DMA("sp", h_s[(j - 1) * 128:j * 128, :], hf_.ap, [hf_], [tk_h])
        CP("act", hb_.ap, hf_.ap, [hf_], [hb_])

    def sA5(j):
        hb_ = hb[j % 2]
        for c in range(8):
            TR(pbf(0)[:, c * 128:(c + 1) * 128], hb_[:, c * 128:(c + 1) * 128], identb, [hb_, cstb], [PB[0]])

    def sA6(j):
        CP(("act", "dve")[j % 2], hT[j % 2].ap, pbf(0).rearrange("p (c t) -> p c t", t=128), [PB[0]], [hT[j % 2]])

    def sA7(j):
        proj(hT[j % 2], 1, 288, 1536)

    def sA8(j):
        kf = kvf[j % 4]
        p1 = PB[1]
        CP("act", kf.ap, p1[:, 0:288], [p1], [kf])
        ACT(sq3[j % 2].ap, kf.ap, AF.Square, [kf], [sq3[j % 2]])

    def sA9(j):
        kf = kvf[j % 4]
        n = 32
        tA_, tB_ = rt3[j % 2]
        src = kf[:, 256:288].rearrange("p (h t f) -> p h t f", h=1, t=2)
        tA = tA_[:, 0:n].rearrange("p (h t f) -> p h t f", t=2, f=16)
        tB = tB_[:, 0:n].rearrange("p (h t f) -> p h t f", t=2, f=16)
        cos, sin = tabB[:, j, 1, :], tabB[:, j, 0, :]
        cb = cos.unsqueeze(1).unsqueeze(1).to_broadcast([128, 1, 2, 16])
        sb_ = sin.unsqueeze(1).to_broadcast([128, 1, 16])
        TTo("dve", tA, src, cb, ALU.mult, [kf, tabB], [tA_])
        TTo("dve", tB[:, :, 0, :], src[:, :, 1, :], sb_, ALU.mult, [kf, tabB], [tB_])
        TTo("dve", tB[:, :, 1, :], src[:, :, 0, :], sb_, ALU.mult, [kf, tabB], [tB_])
        RED(ssqkv[:, j:j + 1], sq3[j % 2][:, 0:256], ALU.add, [sq3[j % 2]], [ssqkv])
        RED(kr2all[:, j:j + 1], sq3[j % 2][:, 256:288], ALU.add, [sq3[j % 2]], [kr2all])
        TSo("dve", ssqkv[:, j:j + 1], ssqkv[:, j:j + 1], 256.0 * RMS_EPS, None, ALU.add, None, [ssqkv], [ssqkv])

    def sA10(j):
        kv_ = kvb[j % 2]
        tA_, tB_ = rt3[j % 2]
        tA = tA_[:, 0:32].rearrange("p (h t f) -> p h t f", t=2, f=16)
        tB = tB_[:, 0:32].rearrange("p (h t f) -> p h t f", t=2, f=16)
        dst = kv_[:, 256:288].rearrange("p (h t f) -> p h t f", h=1, t=2)
        ACT(ssqkv[:, j:j + 1], ssqkv[:, j:j + 1], AF.Ln, [ssqkv], [ssqkv])
        ACT(ssqkv[:, j:j + 1], ssqkv[:, j:j + 1], AF.Exp, [ssqkv], [ssqkv], scale=-0.5)
        TTo("pool", dst[:, :, 0, :], tA[:, :, 0, :], tB[:, :, 0, :], ALU.subtract, [tA_, tB_], [kv_])
        TTo("pool", dst[:, :, 1, :], tA[:, :, 1, :], tB[:, :, 1, :], ALU.add, [tA_, tB_], [kv_])

    def sA11(j):
        kf, kv_ = kvf[j % 4], kvb[j % 2]
        STT("dve", kv_[:, 0:256], kf[:, 0:256], ssqkv[:, j:j + 1], Gkv.ap, ALU.mult, ALU.mult, [kf, ssqkv, Gkv], [kv_])

    def sA12(j):
        kv_ = kvb[j % 2]
        for c in range(2):
            TR(pbf(2)[:, c * 128:(c + 1) * 128], kv_[:, c * 128:(c + 1) * 128], identb, [kv_, cstb], [PB[2]])
        TR(pbf(2)[0:32, 256:384], kv_[:, 256:288], identb, [kv_, cstb], [PB[2]])

    def sA13(j):
        kt = keyidx[j]
        g, q = (kt // 4) % 2, kt % 4
        lt_ = latT[j % 2]
        CP("act", lt_.ap, pbf(2)[:, 0:256].rearrange("p (c t) -> p c t", t=128), [PB[2]], [lt_])
        CP("act", KrTbuf[g][:, q * 128:(q + 1) * 128], pbf(2)[0:32, 256:384], [PB[2]], [KrTbuf[g]])

    def sA14(j):
        lt_ = latT[j % 2]
        for half in range(2):
            for c in range(2):
                MM(PB[3 + half].ap, lt_[:, c, :], wkvb[:, c, half * 512:(half + 1) * 512], c == 0, c == 1,
                   [lt_, wkvb], [PB[3 + half]])
        p5 = PB[5]
        for pair in range(4):
            for c in range(2):
                MM(p5[:, pair * 128:(pair + 1) * 128], wkb[:, c, pair * 128:(pair + 1) * 128], lt_[:, c, :],
                   c == 0, c == 1, [wkb, lt_], [p5])

    def sA15(j):
        kt = keyidx[j]
        g, q = (kt // 4) % 2, kt % 4
        for half in range(2):
            pv = PB[3 + half].ap.rearrange("p (h x) -> p h x", x=128)
            CP("act", Vbuf[g][:, half * 4:(half + 1) * 4, q, 0:64], pv[:, :, 64:128], [PB[3 + half]], [Vbuf[g]])
            ACT(sq5.ap.rearrange("p (h d) -> p h d", d=64)[:, half * 4:(half + 1) * 4, :], pv[:, :, 0:64],
                AF.Square, [PB[3 + half]], [sq5])
        p5 = PB[5]
        CP(("dve", "act")[j % 2], KTbuf[g][:, :, q * 128:(q + 1) * 128], p5.ap.rearrange("p (a t) -> p a t", t=128),
           [p5], [KTbuf[g]])

    def sA16(j):
        kt = keyidx[j]
        g, q = (kt // 4) % 2, kt % 4
        RED(kn2.ap, sq5.ap.rearrange("p (h d) -> p h d", d=64), ALU.add, [sq5], [kn2])
        STT("dve", kmax2.ap, kn2.ap, kr2all[:, j:j + 1], kmax2.ap, ALU.add, ALU.max, [kn2, kr2all, kmax2], [kmax2])
        if q == 3 or kt == NK - 1:
            kt0 = kt - q
            nq = q + 1
            DMA("sp", KnT_s.rearrange("(a p) t -> p a t", p=128)[:, :, kt0 * 128:(kt0 + nq) * 128],
                KTbuf[g][:, :, 0:nq * 128], [KTbuf[g]], [tk_KnT])
            DMA("sp", KrT_s[:, kt0 * 128:(kt0 + nq) * 128], KrTbuf[g][:, 0:nq * 128], [KrTbuf[g]], [tk_KrT])
            DMA("sp", V_s.rearrange("h p f -> p h f")[:, :, kt0 * 72:(kt0 + nq) * 72],
                Vbuf[g][:, :, 0:nq, :].rearrange("p h q x -> p h (q x)"), [Vbuf[g]], [tk_V])

    def st7(j):
        s = j % 2
        w = j
        hT_ = hT[s]
        proj(hT_, 6, 256, 512)
        p6 = PB[6]
        rope(p6[:, 0:128].rearrange("p (h t f) -> p h t f", h=2, t=2), 2, 32, tabA[:, w, 1, :], tabA[:, w, 0, :],
             katok.ap.rearrange("p (h t f) -> p h t f", h=2, t=2), [p6, tabA], [katok], rt7)
        ACT(sq7.ap, p6[:, 0:128], AF.Square, [p6], [sq7])
        CP("act", Va[:, w % 8, :, 0:64], p6[:, 128:256].rearrange("p (h d) -> p h d", d=64), [p6], [Va])
        RED(wsm7[:, 56:58], sq7.ap.rearrange("p (h d) -> p h d", d=64), ALU.add, [sq7], [wsm7])
        pmax_bcast(wsm7[:, 56:58], wsm7.t, 2, kmaxa[:, w, :], kmaxa.t, 7, wsm7)
        for g_ in range(2):
            TR(pbf(2)[0:64, g_ * 128:(g_ + 1) * 128], katok[:, g_ * 64:(g_ + 1) * 64], identb, [katok, cstb], [PB[2]])
        evac(KaT[0:64, :, (w % 8) * 128:(w % 8 + 1) * 128], pbf(2)[0:64, 0:256].rearrange("p (g t) -> p g t", t=128),
             [PB[2]], [KaT])

    def st8a(j):
        s = j % 2
        n = j - 1
        sq_ = n % 6
        proj(hT[s], 3, 512, 0)
        p7 = PB[3]
        rope(p7.ap.rearrange("p (h t f) -> p h t f", h=8, t=2), 8, 32, tabA[:, j, 1, :], tabA[:, j, 0, :],
             qatok[sq_][:, :, 0:64].rearrange("p h (t f) -> p h t f", t=2), [p7, tabA], [qatok[sq_]], rt8a)
        ACT(sq8[:, 0:512], p7.ap, AF.Square, [p7], [sq8])
        RED(qn2a[sq_].ap, sq8[:, 0:512].rearrange("p (h d) -> p h d", d=64), ALU.add, [sq8], [qn2a[sq_]])

    def st8b(j):
        s = j % 2
        sm8 = sm8s[s]
        proj(hT[s], 6, 384, 768)
        proj(hT[s], 7, 384, 1152)
        ACT(sq8[:, 0:384], PB[6][:, 0:384], AF.Square, [PB[6]], [sq8])
        ACT(sq8[:, 384:768], PB[7][:, 0:384], AF.Square, [PB[7]], [sq8])
        STT("dve", cqn[:, 0:384], PB[6][:, 0:384], 1.0, Gq[:, 0:384], ALU.mult, ALU.mult, [PB[6], Gq], [cqn])
        STT("dve", cqn[:, 384:768], PB[7][:, 0:384], 1.0, Gq[:, 384:768], ALU.mult, ALU.mult, [PB[7], Gq], [cqn])
        RED(sm8[:, 4:5], sq8[:, 0:768], ALU.add, [sq8], [sm8])
        RSQ(sm8[:, 5:6], sm8[:, 4:5], 768.0 * RMS_EPS, [sm8], [sm8])

    def st8c(j):
        for c in range(6):
            TR(pbf(0)[:, c * 128:(c + 1) * 128], cqn[:, c * 128:(c + 1) * 128], identb, [cqn, cstb], [PB[0]])
        evac(cqT.ap, pbf(0)[:, 0:768].rearrange("p (c t) -> p c t", t=128), [PB[0]], [cqT])

    def st8d(j):
        n = j - 1
        sm8 = sm8s[j % 2]
        for half in range(2):
            for c in range(6):
                MM(PB[6 + half][:, 0:384], cqT[:, c, :], wqb[:, c, half * 384:(half + 1) * 384], c == 0, c == 5,
                   [cqT, wqb], [PB[6 + half]])
        for half in range(2):
            pq = PB[6 + half][:, 0:384].rearrange("p (h x) -> p h x", x=96)
            OP("act", "activation", [PB[6 + half], sm8], [qtok], qtok[:, half * 4:(half + 1) * 4, 0:64], pq[:, :, 0:64],
               AF.Identity, scale=sm8[:, 5:6])
            ACT(sq8[:, 0:768].rearrange("p (h x) -> p h x", x=96)[:, half * 4:(half + 1) * 4, :], pq, AF.Square,
                [PB[6 + half], sm8], [sq8], scale=sm8[:, 5:6])
            rope(pq[:, :, 64:96].rearrange("p h (t f) -> p h t f", t=2), 4, 16, tabB[:, j, 1, :], tabB[:, j, 0, :],
                 qtok[:, half * 4:(half + 1) * 4, 64:96].rearrange("p h (t f) -> p h t f", t=2),
                 [PB[6 + half], tabB, sm8], [qtok], rt8d, scl=sm8[:, 5:6])
        RED(qn2b[:, n, :], sq8[:, 0:768].rearrange("p (h x) -> p h x", x=96), ALU.add, [sq8], [qn2b])

    def st8e(j):
        n = j - 1
        for h in range(8):
            TR(pbf(0)[0:96, h * 128:(h + 1) * 128], qtok[:, h, :], identb, [qtok, cstb], [PB[0]])
        evac(qst[n % 2].ap, pbf(0)[0:96, :].rearrange("p (h t) -> p h t", t=128), [PB[0]], [qst[n % 2]])
        DMA("sp", QT_s[0:96, :, n * 128:(n + 1) * 128], qst[n % 2].ap, [qst[n % 2]], [tk_QT])

    has_wa = lambda j: 2 <= j <= NOWN + 1 and "wattn" not in SKIP
    def m2(f, g_):
        def h(j):
            f(j)
            g_(j)
        return h
    stages = [
        (0, sA0, lambda j: True),
        (1, sA1, lambda j: True),
        (2, sA2, lambda j: True),
        (3, sA3, lambda j: True),
        (4, sA4, is_own_),
        (5, m2(sA5, sA6), lambda j: True),
        (6, m2(sA7, sA8), is_key_),
        (6, lambda j: interleave2((lambda: st7(j)), (lambda: st8a(j)) if is_own_(j) else None), is_win_),
        (7, sA9, is_key_),
        (7, st8b, is_own_),
        (8, sA10, is_key_),
        (8, st8c, is_own_),
        (9, sA11, is_key_),
        (9, lambda j: interleave2((lambda: st8d(j)) if is_own_(j) else None,
                                  (lambda: wa_a(j - 2)) if has_wa(j) else None),
         lambda j: is_own_(j) or has_wa(j)),
        (10, m2(sA12, sA13), is_key_),
        (10, st8e, is_own_),
        (11, m2(sA14, sA15), is_key_),
        (11, wa_pair, lambda j: has_wa(j) or has_wa(j + 1)),
        (12, sA16, is_key_),
        (12, lambda j: wa_d(j - 2), has_wa),
    ]
    maxoff = max(o for o, _, _ in stages)
    for step in range(NJ + maxoff):
        for off, fn, pred in sorted(stages, key=lambda x: -x[0]):
            j = step - off
            if 0 <= j < NJ and pred(j):
                fn(j)
        if STOPAT == "S%d" % step:
            S.barrier()
            S.emit(nc, es, final=True)
            return nc


    if dbg:
        d_tabB = dout("d_tabB", [128, NJ * 32])
        DMA("sp", d_tabB, tabB.ap.rearrange("p n t f -> p (n t f)"), [tabB], [Tok()])
    S.barrier()
    S.emit(nc, es)
    esA.close()
    esX = ExitStack()
    nmT = sb("nmT", [8, NQ], BF16, esX)
    nm = sb("nm", [128, NOWN, 8], BF16, esX)
    wsm = sb("wsm2", [128, 64], F32, esX)
    sqs = sb("sqs2", [128, NOWN * 8], F32, esX)
    pmax_bcast(kmax2.ap, kmax2.t, 8, wsm[:, 56:64], wsm.t, 7, wsm)
    for n in range(NOWN):
        TTo("dve", sqs[:, n * 8:(n + 1) * 8], qn2b[:, n, :], wsm[:, 56:64], ALU.mult, [qn2b, wsm], [sqs])
    ACT(sqs[:, 0:NOWN * 8], sqs[:, 0:NOWN * 8], AF.Ln, [sqs], [sqs])
    ACT(sqs[:, 0:NOWN * 8], sqs[:, 0:NOWN * 8], AF.Exp, [sqs], [sqs], scale=0.5)
    TSo("dve", nm.ap.rearrange("p n h -> p (n h)"), sqs[:, 0:NOWN * 8], -1.0, None, ALU.mult, None, [sqs], [nm])
    for n0 in range(0, NOWN, 8):
        nn = min(8, NOWN - n0)
        for i in range(nn):
            TR(pbf(0)[0:8, i * 128:(i + 1) * 128], nm[:, n0 + i, :], identb, [nm, cstb], [PB[0]])
        evac(nmT[:, n0 * 128:(n0 + nn) * 128], pbf(0)[0:8, 0:nn * 128], [PB[0]], [nmT])
    DMA("sp", QT_s[96], nmT.ap, [nmT], [tk_QT])

    if dbg:
        d_QT = dout("d_QT", [97, 8 * NQ], BF16)
        DMA("sp", d_QT, QT_s.rearrange("p h n -> p (h n)"), [tk_QT], [Tok()])
        d_KnT = dout("d_KnT", [512, NK * 128], BF16)
        DMA("sp", d_KnT, KnT_s, [tk_KnT], [Tok()])
        d_KrT = dout("d_KrT", [32, NK * 128], BF16)
        DMA("sp", d_KrT, KrT_s, [tk_KrT], [Tok()])
        d_V = dout("d_V", [8 * 128, NK * 72], BF16)
        DMA("sp", d_V, V_s.rearrange("h p f -> (h p) f"), [tk_V], [Tok()])
        if "wattn" not in SKIP:
            d_mixa = dout("d_mixa", [NQ, 512], BF16)
            DMA("sp", d_mixa, mixa_s, [tk_mixa], [Tok()])
        d_h = dout("d_h", [NQ, 1024], F32)
        DMA("sp", d_h, h_s, [tk_h], [Tok()])

    S.barrier()
    S.emit(nc, es, final=(stage == "A"))
    esX.close()
    esT.close()
    print("phase A ops", len(S.ops), "standalone waits", S.nwait)

    if stage == "A":
        es.close()
        return nc

    esB = ExitStack()
    Bt = lambda name, shape, dt: sb(name, shape, dt, esB)
    KT = [Bt("KT%d" % i, [97, NK * 128], BF16) for i in range(2)]
    Vh = [Bt("Vh%d" % i, [128, NK, 72], BF16) for i in range(2)]
    QTh = [Bt("QTh%d" % i, [97, NQ], BF16) for i in range(2)]
    OsbB = [Bt("OsbB%d" % i, [65, 512], F32) for i in range(2)]
    obt = [Bt("obt%d" % i, [128, 4, 64], F32) for i in range(2)]
    wsB = [Bt("wsB%d" % i, [128, 8], F32) for i in range(2)]
    for kt_ in KT:
        OP("pool", "memset", [], [kt_], kt_[64:97, :], 1.0)
    NQB = NOWN // 4

    def load_head(h):
        hb_ = h % 2
        half = (NK * 128) // 2
        DMA("sp", KT[hb_][0:64, 0:half], KnT_s[h * 64:(h + 1) * 64, 0:half], [tk_KnT], [KT[hb_]])
        DMA("pool", KT[hb_][0:64, half:], KnT_s[h * 64:(h + 1) * 64, half:], [tk_KnT], [KT[hb_]])
        DMA("sp", KT[hb_][64:96, :], KrT_s, [tk_KrT], [KT[hb_]])
        DMA("pool", Vh[hb_].ap.rearrange("p c x -> p (c x)"), V_s[h], [tk_V], [Vh[hb_]])
        DMA("sp", QTh[hb_].ap, QT_s[:, h, :], [tk_QT], [QTh[hb_]])

    PT = [Bt("PT%d" % i, [128, 512], BF16) for i in range(4)]
    units = [(h, qb, c) for h in range(8) for qb in range(NQB) for c in range(NK)]
    NU = len(units)
    LA = 2
    pending = []

    def epilogue(h, qb, blk):
        ob_ = PB[4 + blk % 2]
        os_ = OsbB[blk % 2]
        bt = PB[6 + blk % 2]
        ot = obt[blk % 2]
        ws = wsB[blk % 2]
        CP("dve", os_.ap, ob_[0:65, :], [ob_], [os_])

        def part2():
            btv = bt[:, 0:260].rearrange("p (i x) -> p i x", x=65)
            for i in range(4):
                TR(bt[:, i * 65:(i + 1) * 65], os_[:, i * 128:(i + 1) * 128], identf[0:65, 0:65], [os_, identf], [bt])
            OP("dve", "reciprocal", [bt], [ws], ws[:, 0:4].unsqueeze(2), btv[:, :, 64:65])
            TTo("dve", ot.ap, btv[:, :, 0:64], ws[:, 0:4].unsqueeze(2).to_broadcast([128, 4, 64]), ALU.mult,
                [bt, ws], [ot])
            DMA("pool", outb_s[qb * 512:(qb + 1) * 512, h * 64:(h + 1) * 64].rearrange("(i p) d -> p i d", p=128),
                ot.ap, [ot], [tk_outb])
        return part2

    for i in range(NU + LA):
        while pending and pending[0][0] <= i:
            pending.pop(0)[1]()
        if i < NU:
            h, qb, c = units[i]
            if qb == 0 and c == 0 and h == 0:
                load_head(0)
            hb_ = h % 2
            bs = PB[i % 3]
            MM(bs.ap, KT[hb_][0:97, c * 128:(c + 1) * 128], QTh[hb_][0:97, qb * 512:(qb + 1) * 512], True, True,
               [KT[hb_], QTh[hb_]], [bs])
            ACT(PT[i % 4].ap, bs.ap, AF.Exp, [bs], [PT[i % 4]])
        ip = i - LA
        if ip >= 0:
            h, qb, c = units[ip]
            if qb == 0 and c == 0 and h + 1 < 8:
                load_head(h + 1)
            hb_ = h % 2
            blk = h * NQB + qb
            ob_ = PB[4 + blk % 2]
            MM(ob_[0:65, :], Vh[hb_][:, c, 0:65], PT[ip % 4].ap, c == 0, c == NK - 1, [Vh[hb_], PT[ip % 4]], [ob_])
            if c == NK - 1:
                pending.append((i + 6, epilogue(h, qb, blk)))
    for _, f in pending:
        f()

    if dbg:
        d_outb = dout("d_outb", [NQ, 512], F32)
        DMA("sp", d_outb, outb_s, [tk_outb], [Tok()])

    S.barrier()
    S.emit(nc, es, final=(stage == "AB"))
    esB.close()
    print("phase B ops", len(S.ops), "standalone waits", S.nwait)
    if stage == "AB":
        es.close()
        return nc

    NSB = 2 if NOWN >= 8 else 1
    SBT = NOWN // NSB
    NT = SBT * 128
    cmb_s = dscr("cmb_s", [NSB, 32, NT], F32)
    tk_cmb = Tok("cmb_s")
    tk_y = Tok("y")
    esC = ExitStack()
    C = lambda name, shape, dt: sb(name, shape, dt, esC)
    Gob = C("Gob", [128, 512], F32)
    G_at = C("G_at", [128, 1024], F32)
    B_at = C("B_at", [128, 1024], F32)
    G_ff = C("G_ff", [128, 1024], F32)
    B_ff = C("B_ff", [128, 1024], F32)
    brB = C("brB", [128, 36], F32)
    wr_f = C("wr_f", [128, 8, 36], F32)
    yacc = C("yacc", [128, SBT, 1024], F32)
    h2T = C("h2T", [128, 8, NT], BF16)
    cmbT = C("cmbT", [32, NT], F32)
    vload(Gob, "ob_g")
    vload(G_at, "ln_attn_g")
    vload(B_at, "ln_attn_b")
    vload(G_ff, "ln_ffn_g")
    vload(B_ff, "ln_ffn_b")
    vload(brB, "b_router")
    TSo("dve", Gob.ap, Gob.ap, math.sqrt(512.0), None, ALU.mult, None, [Gob], [Gob])
    DMA("sp", wr_f.ap, w_router.rearrange("(c p) n -> p c n", p=128), [], [wr_f])

    def layer_norm_tile(src, st_, Gt, Bt_, dst):
        OP("dve", "bn_stats", [src], [st_], st_[:, 0:6], src[:, 0:512])
        OP("dve", "bn_stats", [src], [st_], st_[:, 6:12], src[:, 512:1024])
        OP("dve", "bn_aggr", [st_], [st_], st_[:, 12:14], st_[:, 0:12])
        RSQ(st_[:, 14:15], st_[:, 13:14], LN_EPS, [st_], [st_])
        STT("dve", st_[:, 15:16], st_[:, 12:13], -1.0, st_[:, 14:15], ALU.mult, ALU.mult, [st_], [st_])
        ACT(src.ap, src.ap, AF.Identity, [src, st_], [src], bias=st_[:, 15:16], scale=st_[:, 14:15])
        TTo("pool", src.ap, src.ap, Gt.ap, ALU.mult, [src, Gt], [src])
        TTo("dve", dst.ap, src.ap, Bt_.ap, ALU.add, [src, Bt_], [dst])

    for sbi in range(NSB):
        es1 = ExitStack()
        mk1 = lambda name, shape, dt: sb("%s_s%d" % (name, sbi), shape, dt, es1)
        woutb = mk1("woutb", [128, 8, 1024], BF16)
        mix = [mk1("mix%d" % i, [128, 1024], BF16) for i in range(2)]
        obf = [mk1("obf%d" % i, [128, 512], F32) for i in range(6)]
        hin = [mk1("hin%d" % i, [128, 1024], F32) for i in range(2)]
        rr = [mk1("rr%d" % i, [128, 1024], F32) for i in range(2)]
        rn = [mk1("rn%d" % i, [128, 1024], F32) for i in range(2)]
        h2 = [mk1("h2_%d" % i, [128, 1024], F32) for i in range(2)]
        h2Tf = mk1("h2Tf", [128, 8, 128], F32)
        mixT = mk1("mixT", [128, 8, 128], BF16)
        sq1 = [mk1("sq1_%d" % i, [128, 512], F32) for i in range(2)]
        sta = [mk1("sta_%d" % i, [128, 4], F32) for i in range(4)]
        stb = [mk1("stb_%d" % i, [128, 16], F32) for i in range(2)]
        rt = [mk1("rt%d" % i, [128, 256], F32) for i in range(4)]
        wst1 = rr
        for c in range(8):
            DMA("sp", wst1[c % 2].ap, w_out[c * 128:(c + 1) * 128, :], [], [wst1[c % 2]])
            CP(("dve", "pool")[c % 2], woutb[:, c, :], wst1[c % 2].ap, [wst1[c % 2]], [woutb])

        def rows_(l):
            n = sbi * SBT + l
            return slice(n * 128, (n + 1) * 128)

        def c0(l):
            DMA("sp", obf[l % 6].ap, outb_s[rows_(l), :], [tk_outb], [obf[l % 6]])

        def c1(l):
            ACT(sq1[l % 2].ap, obf[l % 6].ap, AF.Square, [obf[l % 6]], [sq1[l % 2]])

        def c2(l):
            st_ = sta[l % 4]
            RED(st_[:, 0:1], sq1[l % 2].ap, ALU.add, [sq1[l % 2]], [st_])
            TSo("dve", st_[:, 1:2], st_[:, 0:1], 512.0 * RMS_EPS, None, ALU.add, None, [st_], [st_])

        def c3(l):
            st_ = sta[l % 4]
            ACT(st_[:, 1:2], st_[:, 1:2], AF.Ln, [st_], [st_])
            ACT(st_[:, 1:2], st_[:, 1:2], AF.Exp, [st_], [st_], scale=-0.5)
            DMA("sp", mix[l % 2][:, 0:512], mixa_s[rows_(l), :], [tk_mixa], [mix[l % 2]])

        def c4(l):
            mx = mix[l % 2]
            STT("dve", mx[:, 512:1024], obf[l % 6].ap, sta[l % 4][:, 1:2], Gob.ap, ALU.mult, ALU.mult,
                [obf[l % 6], sta[l % 4], Gob], [mx])
            DMA("pool", hin[l % 2].ap, h_s[rows_(l), :], [tk_h], [hin[l % 2]])

        def c5(l):
            mx = mix[l % 2]
            for c in range(8):
                TR(pbf(0)[:, c * 128:(c + 1) * 128], mx[:, c * 128:(c + 1) * 128], identb, [mx, cstb], [PB[0]])
            CP("act", mixT.ap, pbf(0).rearrange("p (c t) -> p c t", t=128), [PB[0]], [mixT])

        def c6(l):
            hi_, r_ = hin[l % 2], rr[l % 2]
            for half in range(2):
                for c in range(8):
                    MM(PB[1 + half].ap, mixT[:, c, :], woutb[:, c, half * 512:(half + 1) * 512], c == 0, c == 7,
                       [mixT, woutb], [PB[1 + half]])
            for half in range(2):
                cs = slice(half * 512, (half + 1) * 512)
                STT("dve", r_[:, cs], hi_[:, cs], ALPHA, PB[1 + half].ap, ALU.mult, ALU.add, [hi_, PB[1 + half]], [r_])

        def c7(l):
            r_, st_ = rr[l % 2], stb[l % 2]
            OP("dve", "bn_stats", [r_], [st_], st_[:, 0:6], r_[:, 0:512])
            OP("dve", "bn_stats", [r_], [st_], st_[:, 6:12], r_[:, 512:1024])
            OP("dve", "bn_aggr", [st_], [st_], st_[:, 12:14], st_[:, 0:12])
            TSo("dve", st_[:, 14:15], st_[:, 13:14], LN_EPS, None, ALU.add, None, [st_], [st_])

        def c8(l):
            r_, st_, rn_ = rr[l % 2], stb[l % 2], rn[l % 2]
            ACT(st_[:, 14:15], st_[:, 14:15], AF.Ln, [st_], [st_])
            ACT(st_[:, 14:15], st_[:, 14:15], AF.Exp, [st_], [st_], scale=-0.5)
            ACT(st_[:, 15:16], st_[:, 12:13], AF.Identity, [st_], [st_], scale=st_[:, 14:15])
            OP("act", "mul", [st_], [st_], st_[:, 15:16], st_[:, 15:16], -1.0)
            ACT(rn_.ap, r_.ap, AF.Identity, [r_, st_], [rn_], bias=st_[:, 15:16], scale=st_[:, 14:15])

        def c9(l):
            rn_ = rn[l % 2]
            TTo("pool", rn_.ap, rn_.ap, G_at.ap, ALU.mult, [rn_, G_at], [rn_])

        def c10(l):
            TTo("dve", h2[l % 2].ap, rn[l % 2].ap, B_at.ap, ALU.add, [rn[l % 2], B_at], [h2[l % 2]])

        def c11(l):
            h2_ = h2[l % 2]
            OP("act", "mul", [h2_], [yacc], yacc[:, l, :], h2_.ap, ALPHA)
            for c in range(8):
                TR(PB[3 + c // 4][:, (c % 4) * 128:(c % 4 + 1) * 128], h2_[:, c * 128:(c + 1) * 128], identf.ap,
                   [h2_, identf], [PB[3 + c // 4]])
            for hh in range(2):
                pv = PB[3 + hh].ap.rearrange("p (c t) -> p c t", t=128)
                CP("act", h2T[:, hh * 4:(hh + 1) * 4, l * 128:(l + 1) * 128], pv, [PB[3 + hh]], [h2T])
                CP("act", h2Tf[:, hh * 4:(hh + 1) * 4, :], pv, [PB[3 + hh]], [h2Tf])

        def c12(l):
            rt_ = rt[l % 4]
            for c in range(8):
                MM(PB[5][:, 0:36], h2Tf[:, c, :], wr_f[:, c, :], c == 0, c == 7, [h2Tf, wr_f], [PB[5]])
            TTo("dve", rt_[:, 0:36], PB[5][:, 0:36], brB.ap, ALU.add, [PB[5], brB], [rt_])

        def c13(l):
            rt_ = rt[l % 4]
            RED(rt_[:, 40:41], rt_[:, 0:4], ALU.max, [rt_], [rt_])
            TSo("dve", rt_[:, 44:48], rt_[:, 0:4], rt_[:, 40:41], None, ALU.is_equal, None, [rt_], [rt_])
            TSo("dve", rt_[:, 48:52], rt_[:, 44:48], 1.0, 1e30, ALU.subtract, ALU.mult, [rt_], [rt_])
            elm = rt_[:, 64:96]
            TTo("dve", elm.rearrange("p (g j) -> p g j", j=8), rt_[:, 4:36].rearrange("p (g j) -> p g j", j=8),
                rt_[:, 48:52].unsqueeze(2).to_broadcast([128, 4, 8]), ALU.add, [rt_], [rt_])
            RED(rt_[:, 41:42], elm, ALU.max, [rt_], [rt_])
            TSo("dve", rt_[:, 96:128], elm, rt_[:, 41:42], None, ALU.is_equal, None, [rt_], [rt_])
            STT("dve", rt_[:, 128:160], rt_[:, 96:128], -1e30, elm, ALU.mult, ALU.add, [rt_], [rt_])
            RED(rt_[:, 42:43], rt_[:, 128:160], ALU.max, [rt_], [rt_])
            TSo("dve", rt_[:, 160:192], rt_[:, 128:160], rt_[:, 42:43], None, ALU.is_equal, None, [rt_], [rt_])
            TSo("dve", rt_[:, 43:44], rt_[:, 40:41], -1.0, None, ALU.mult, None, [rt_], [rt_])
            TTo("dve", rt_[:, 57:58], rt_[:, 42:43], rt_[:, 41:42], ALU.subtract, [rt_], [rt_])

        def c14(l):
            rt_ = rt[l % 4]
            ACT(rt_[:, 52:56], rt_[:, 0:4], AF.Exp, [rt_], [rt_], bias=rt_[:, 43:44], scale=1.0)
            ACT(rt_[:, 58:59], rt_[:, 57:58], AF.Exp, [rt_], [rt_])

        def c15(l):
            rt_ = rt[l % 4]
            RED(rt_[:, 56:57], rt_[:, 52:56], ALU.add, [rt_], [rt_])
            STT("dve", rt_[:, 59:60], rt_[:, 58:59], 1.0, rt_[:, 56:57], ALU.add, ALU.mult, [rt_], [rt_])
            OP("dve", "reciprocal", [rt_], [rt_], rt_[:, 60:61], rt_[:, 59:60])
            TTo("dve", rt_[:, 61:62], rt_[:, 60:61], rt_[:, 58:59], ALU.mult, [rt_], [rt_])
            TSo("dve", rt_[:, 192:224], rt_[:, 96:128], rt_[:, 60:61], None, ALU.mult, None, [rt_], [rt_])
            STT("dve", rt_[:, 192:224], rt_[:, 160:192], rt_[:, 61:62], rt_[:, 192:224], ALU.mult, ALU.add,
                [rt_], [rt_])
            TR(PB[6][0:32, 0:128], rt_[:, 192:224], identf.ap, [rt_, identf], [PB[6]])
            CP("dve", cmbT[:, l * 128:(l + 1) * 128], PB[6][0:32, 0:128], [PB[6]], [cmbT])

        cstages = [c0, c1, c2, c3, c4, c5, c6, c7, c8, c9, c10, c11, c12, c13, c14, c15]
        for step in range(SBT + len(cstages) - 1):
            for off in range(len(cstages) - 1, -1, -1):
                l = step - off
                if 0 <= l < SBT:
                    cstages[off](l)

        DMA("sp", cmb_s[sbi], cmbT.ap, [cmbT], [tk_cmb])
        if dbg and sbi == 0:
            d_h2T = dout("d_h2T", [128, 8 * NT], BF16)
            DMA("sp", d_h2T, h2T.ap.rearrange("p c n -> p (c n)"), [h2T], [Tok()])
            d_cmbT = dout("d_cmbT", [32, NT], F32)
            DMA("sp", d_cmbT, cmbT.ap, [cmbT], [Tok()])
        S.barrier()
        S.emit(nc, es)
        es1.close()
        es2 = ExitStack()
        mk2 = lambda name, shape, dt: sb("%s_s%d" % (name, sbi), shape, dt, es2)
        wstg = [mk2("wstg%d" % i, [128, 2048], F32) for i in range(3)]
        wg = [mk2("wg%d" % i, [128, 8, 256], BF16) for i in range(2)]
        wu = [mk2("wu%d" % i, [128, 8, 256], BF16) for i in range(2)]
        wd = [mk2("wd%d" % i, [128, 2, 1024], BF16) for i in range(2)]
        cB = [mk2("cB%d" % i, [128, NT], F32) for i in range(2)]
        sg = mk2("sg", [128, 1024], F32)
        t1 = mk2("t1", [128, 1024], F32)
        hid = [mk2("hid%d" % i, [128, 2, 512], BF16) for i in range(2)]
        NBLK = SBT // 4

        def load_expert(e):
            sl = e % 2
            DMA("sp", wstg[0].ap.rearrange("p (c f) -> p c f", f=256), w_gate[e].rearrange("(c p) f -> p c f", p=128),
                [], [wstg[0]])
            CP("act", wg[sl].ap.rearrange("p c f -> p (c f)"), wstg[0].ap, [wstg[0]], [wg[sl]])
            DMA("sp", wstg[1].ap.rearrange("p (c f) -> p c f", f=256), w_up[e].rearrange("(c p) f -> p c f", p=128),
                [], [wstg[1]])
            CP("act", wu[sl].ap.rearrange("p c f -> p (c f)"), wstg[1].ap, [wstg[1]], [wu[sl]])
            DMA("sp", wstg[2].ap.rearrange("p (c f) -> p c f", f=1024), w_down[e].rearrange("(c p) f -> p c f", p=128),
                [], [wstg[2]])
            CP("act", wd[sl].ap.rearrange("p c f -> p (c f)"), wstg[2].ap, [wstg[2]], [wd[sl]])
            DMA("sp", cB[sl].ap, cmb_s[sbi, e:e + 1, :].to_broadcast([128, NT]), [tk_cmb], [cB[sl]])

        eunits = [(e, b) for e in range(32) for b in range(NBLK)]
        dcount = [0]

        def down_pairs(e, b, ui):
            sl = e % 2
            hd = hid[ui % 2]
            outl = []
            for i in range(4):
                for half in range(2):
                    def f(i=i, half=half):
                        bank = PB[4 + dcount[0] % 4]
                        dcount[0] += 1
                        for ff in range(2):
                            MM(bank.ap, hd[:, ff, i * 128:(i + 1) * 128], wd[sl][:, ff, half * 512:(half + 1) * 512],
                               ff == 0, ff == 1, [hd, wd[sl]], [bank])
                        ya = yacc[:, b * 4 + i, half * 512:(half + 1) * 512]
                        TTo("dve", ya, ya, bank.ap, ALU.add, [yacc, bank], [yacc])
                    outl.append(f)
            return outl

        def gu(e, b, ui, inter):
            sl = e % 2
            T0 = b * 512
            nmm = 0
            for which, wt in ((0, wg), (1, wu)):
                for f in range(2):
                    bank = PB[which * 2 + f]
                    for c in range(8):
                        MM(bank.ap, wt[sl][:, c, f * 128:(f + 1) * 128], h2T[:, c, T0:T0 + 512], c == 0, c == 7,
                           [wt[sl], h2T], [bank])
                        nmm += 1
                        if nmm > 16 and nmm % 2 == 0 and inter:
                            inter.pop(0)()
            hd = hid[ui % 2]
            for f in range(2):
                fs = slice(f * 512, (f + 1) * 512)
                ACT(sg[:, fs], PB[f].ap, AF.Silu, [PB[f]], [sg])
                TTo("dve", t1[:, fs], sg[:, fs], PB[2 + f].ap, ALU.mult, [sg, PB[2 + f]], [t1])
                TTo("pool", hd[:, f, :], t1[:, fs], cB[sl][:, T0:T0 + 512], ALU.mult, [t1, cB[sl]], [hd])

        for ui in range(len(eunits) + 1):
            inter = []
            if ui >= 1:
                e, b = eunits[ui - 1]
                inter = down_pairs(e, b, ui - 1)
            if ui < len(eunits):
                e, b = eunits[ui]
                if b == 0 and e == 0:
                    load_expert(0)
                gu(e, b, ui, inter)
            for f in inter:
                f()
            if ui < len(eunits):
                e, b = eunits[ui]
                if b == 0 and e + 1 < 32:
                    load_expert(e + 1)
        S.barrier()
        S.emit(nc, es)
        es2.close()
        es3 = ExitStack()
        yo = [sb("yo%d_s%d" % (i, sbi), [128, 1024], F32, es3) for i in range(2)]
        yi = [sb("yi%d_s%d" % (i, sbi), [128, 1024], F32, es3) for i in range(2)]
        st3 = [sb("st3_%d_s%d" % (i, sbi), [128, 16], F32, es3) for i in range(2)]
        for l in range(SBT):
            n = sbi * SBT + l
            s = l % 2
            CP("act", yi[s].ap, yacc[:, l, :], [yacc], [yi[s]])
            layer_norm_tile(yi[s], st3[s], G_ff, B_ff, yo[s])
            DMA("sp", y_out[n * 128:(n + 1) * 128, :], yo[s].ap, [yo[s]], [tk_y])
        S.barrier()
        S.emit(nc, es, final=(sbi == NSB - 1))
        es3.close()
    esC.close()
    es.close()
    print("total ops", len(S.ops), "standalone waits", S.nwait)
    return nc

COMPUTE = ("pe", "act", "dve", "pool")
KD = 8
EPOCH = 12000


class Tok:
    __slots__ = ("n", "lw", "rd", "rdma", "x")

    def __init__(s, n="", x=False):
        s.n = n
        s.x = x
        s.lw = -1
        s.rd = {}
        s.rdma = []


class Sched:
    def __init__(s):
        s.ops = []
        s.lastc = {}
        s.bar = None
        s.bar_idx = 0
        s.bar_done = set()
        s.dmas_since_bar = []
        s.emitted = 0
        s.dq = {}
        s.semof = {}
        s.pos = {}
        s.cpos = {e: 0 for e in COMPUTE}
        s.cnt = {e: 0 for e in COMPUTE}
        s.seen = {}
        s.sems = {}
        s.final = {}
        s.nwait = 0

    def add(s, eng, fn, reads=(), writes=(), dma=False):
        i = len(s.ops)
        deps = set()
        xr = [t for t in reads if t.x]
        if xr:
            reads = [t for t in reads if not t.x]
            writes = list(writes) + [t for t in xr if t not in writes]
        for t in reads:
            if t.lw >= 0:
                deps.add(t.lw)
        for t in writes:
            if t.lw >= 0:
                deps.add(t.lw)
            deps.update(t.rd.values())
            deps.update(t.rdma)
        bi = s.bar_idx
        deps = set(d for d in deps if d >= bi)
        if s.bar is not None and eng not in s.bar_done:
            deps.update(s.bar)
            s.bar_done.add(eng)
        s.ops.append([eng, fn, deps, dma])
        for t in reads:
            if dma:
                t.rdma.append(i)
            else:
                t.rd[eng] = i
        for t in writes:
            t.lw = i
            t.rd = {}
            t.rdma = []
        if dma:
            s.dmas_since_bar.append(i)
        else:
            s.lastc[eng] = i
        return i

    def barrier(s):
        b = set(s.dmas_since_bar)
        b.update(s.lastc.values())
        if s.bar is not None and len(s.bar_done) < 5:
            b.update(s.bar)
        s.bar = b
        s.bar_idx = len(s.ops)
        s.bar_done = set()
        s.dmas_since_bar = []

    def emit(s, nc, stack, final=False):
        ops = s.ops
        lo, n = s.emitted, len(ops)
        semof, pos = s.semof, s.pos
        for i in range(lo, n):
            eng, fn, deps, dma = ops[i]
            if dma:
                lst = s.dq.setdefault(eng, [])
                k = len(lst)
                if k >= KD:
                    deps.add(lst[k - KD])
                lst.append(i)
                semof[i] = (("d", eng, k % KD), 16 * (k // KD + 1))
            else:
                pos[i] = s.cpos[eng]
                s.cpos[eng] += 1
        hasdep = set()
        for i in range(lo, n):
            hasdep.update(ops[i][2])
        hasdep.update(s.lastc.values())
        for i in range(lo, n):
            eng, fn, deps, dma = ops[i]
            if not dma and i in hasdep:
                c = s.cnt[eng]
                s.cnt[eng] = c + 1
                semof[i] = (("c", eng, c // EPOCH), c % EPOCH + 1)
        waits = {}
        for i in range(lo, n):
            eng, fn, deps, dma = ops[i]
            need = {}
            for d in deps:
                de, _, _, ddma = ops[d]
                if not ddma and de == eng and not dma:
                    if eng == "pe":
                        continue
                    if pos[i] - pos[d] > 3:
                        continue
                key, val = semof[d]
                if need.get(key, 0) < val:
                    need[key] = val
            sn = s.seen.setdefault(eng, {})
            w = []
            for key, val in need.items():
                if sn.get(key, 0) >= val:
                    continue
                sn[key] = val
                w.append((key, val))
            waits[i] = w
        sems = s.sems
        for i in range(lo, n):
            x = semof.get(i)
            if x is None:
                continue
            if x[0] not in sems:
                sems[x[0]] = stack.enter_context(nc.semaphore("s_%s_%s_%d" % x[0]))
            if x[0][0] == "d" and s.final.get(x[0], 0) < x[1]:
                s.final[x[0]] = x[1]
        with nc.Block() as block:
            def run(engname, eobj, tail=False):
                for i in range(lo, n):
                    o = ops[i]
                    if o[0] != engname:
                        continue
                    w = waits[i]
                    for key, val in w[:-1]:
                        eobj.wait_ge(sems[key], val)
                        s.nwait += 1
                    ins = o[1](eobj)
                    if w:
                        ins._wait_ge(sems[w[-1][0]], w[-1][1])
                    x = semof.get(i)
                    if x is not None:
                        ins.then_inc(sems[x[0]], 16 if x[0][0] == "d" else 1)
                    o[1] = None
                if tail:
                    for key, val in s.final.items():
                        eobj.wait_ge(sems[key], val)

            @block.sync
            def _(e):
                run("sp", e, tail=final)

            @block.scalar
            def _(e):
                run("act", e)

            @block.vector
            def _(e):
                run("dve", e)

            @block.gpsimd
            def _(e):
                run("pool", e)

            @block.tensor
            def _(e):
                run("pe", e)
        s.emitted = n


N_CORES = 8
NOWN_FULL = 32
NOTH_FULL = 96
_NC_CACHE = {}


def _host_prep(inputs, c):
    bf = ml_dtypes.bfloat16
    NOWN, NOTH = NOWN_FULL, NOTH_FULL
    seq_tiles = NOWN + NOTH
    b = c // 4
    qd = c % 4
    first = qd == 0
    last = qd == 3
    own0 = qd * NOWN
    x = inputs["x"][b]
    pos = inputs["positions"][b]
    own = list(range(own0, own0 + NOWN))
    others = [t for t in range(seq_tiles) if t not in own]
    P = own0 - 1 if not first else own0
    N = own0 + NOWN if not last else own0 + NOWN - 1
    order = [P] + own + [N] + others
    xr = np.concatenate([x[t * 128:(t + 1) * 128] for t in order], 0)
    posr = np.ascontiguousarray(np.stack([pos[t * 128:(t + 1) * 128] for t in order], 1).astype(np.int32))
    jj = np.arange(128)[:, None]
    ii = np.arange(128)[None, :]
    mP = np.where(jj >= ii, 0.0, NEGB).astype(np.float32)
    mN = np.where(jj <= ii, 0.0, NEGB).astype(np.float32)
    full = np.full((128, 128), NEGB, np.float32)
    ms = [mP, mN, full if first else mP, full if last else mN]
    cst_b = np.concatenate([np.eye(128, dtype=np.float32)] + [np.tile(m, (1, 4)) for m in ms], 1).astype(bf)
    return dict(xr=xr, posr=posr, cst_b=cst_b)


def kernel(**inputs):
    inputs = {k: np.asarray(v) for k, v in inputs.items()}
    g = lambda k: np.ascontiguousarray(inputs[k], dtype=np.float32)
    invA = (1.0 / (np.float32(10000.0) ** (np.arange(0, 64, 2, dtype=np.float32) / np.float32(64)))).astype(np.float32)
    invB = (1.0 / (np.float32(10000.0) ** (np.arange(0, 32, 2, dtype=np.float32) / np.float32(32)))).astype(np.float32)
    parts = dict(ln_emb_g=g("ln_emb_g"), ln_emb_b=g("ln_emb_b"), q_g=g("q_a_norm_g")[0], kv_g=g("kv_a_norm_g")[0],
                 oa_g=g("out_norm_a_g")[0], ob_g=g("out_norm_b_g")[0], ln_attn_g=g("ln_attn_g")[0],
                 ln_attn_b=g("ln_attn_b")[0], ln_ffn_g=g("ln_ffn_g")[0], ln_ffn_b=g("ln_ffn_b")[0], sink=g("a_sink")[0],
                 b_router=np.concatenate([g("b_group")[0], g("b_expert")[0]]), invA=invA, invB=invB)
    vecs = np.zeros((1, NVEC), np.float32)
    for k, (o, l) in VOFF.items():
        vecs[0, o:o + l] = parts[k]
    shared = dict(w_in=g("w_in")[0], w_q_b=g("w_q_b")[0], w_kv_b=g("w_kv_b")[0], w_out=g("w_out")[0],
                  w_router=np.ascontiguousarray(np.concatenate([g("w_group")[0], g("w_expert")[0]], 1)),
                  w_gate=g("w_gate")[0], w_up=g("w_up")[0], w_down=g("w_down")[0], vecs=vecs,
                  cst_f=np.eye(128, dtype=np.float32))
    in_maps = []
    for c in range(N_CORES):
        m = dict(shared)
        m.update(_host_prep(inputs, c))
        in_maps.append(m)
    if "nc" not in _NC_CACHE:
        _NC_CACHE["nc"] = build(NOWN_FULL, NOTH_FULL, stage="ABC", dbg=False)
    nc = _NC_CACHE["nc"]
    res = run_bass_kernel_spmd(nc, in_maps, core_ids=list(range(N_CORES)))
    B, Sq, D = inputs["x"].shape
    out = np.zeros((B, Sq, D), np.float32)
    for c in range(N_CORES):
        b = c // 4
        r0 = (c % 4) * NOWN_FULL * 128
        out[b, r0:r0 + NOWN_FULL * 128] = np.asarray(res.results[c]["y"], dtype=np.float32)
    return out
```
